# Optimizing a Trainium2 kernel written in Bass

```python
import math
import jax, jax.numpy as jnp
from jax import lax
import numpy as np

D_MODEL = 1024
BATCH = 32
SEQ = 2048
DEPTH = 1

MEM_LEN = 256
SSM_WIDTH = 512
SSM_GROUP = 16
SSM_GROUPS = SSM_WIDTH // SSM_GROUP
SSM_STATE = 64
DSA_HEADS = 8
DSA_HEAD_DIM = 64
IDX_HEADS = 4
IDX_DIM = 64
IDX_TOPK_MAX = 256
Q_BLOCK = 128
MEM_HEADS = 4
MEM_HEAD_DIM = 128
N_BRANCHES = 3
ROPE_THETA = 10000.0
N_GROUPS = 4
EXPERTS_PER_GROUP = 8
N_EXPERTS = N_GROUPS * EXPERTS_PER_GROUP
TOP_K_EXPERTS = 2
EXPERT_FF = 512
EPS = 1e-6
IN_SPLITS = (SSM_WIDTH, DSA_HEADS * DSA_HEAD_DIM, DSA_HEAD_DIM, DSA_HEAD_DIM, IDX_HEADS * IDX_DIM, IDX_DIM, IDX_HEADS, MEM_HEADS * MEM_HEAD_DIM, N_BRANCHES * D_MODEL)
IN_COLS = sum(IN_SPLITS)

kernel_name = 'hybrid_s5_dsa_memxattn_hmoe'

F32 = jnp.float32


def rms_norm(x, g):
    xf = x.astype(F32)
    y = xf * lax.rsqrt(jnp.mean(xf * xf, axis=-1, keepdims=True) + EPS)
    return (y * g.astype(F32)).astype(x.dtype)


def rope_tables(positions, dim):
    inv = 1.0 / (ROPE_THETA ** (jnp.arange(0, dim, 2, dtype=F32) / dim))
    ang = positions.astype(F32)[..., None] * inv
    return jnp.cos(ang), jnp.sin(ang)


def apply_rope(x, cos, sin):
    extra = x.ndim - cos.ndim
    cos = cos.reshape(cos.shape[:2] + (1,) * extra + cos.shape[2:])
    sin = sin.reshape(sin.shape[:2] + (1,) * extra + sin.shape[2:])
    x1, x2 = jnp.split(x.astype(F32), 2, axis=-1)
    out = jnp.concatenate([x1 * cos - x2 * sin, x2 * cos + x1 * sin], axis=-1)
    return out.astype(x.dtype)


def _ssm_combine(e1, e2):
    a1, b1 = e1
    a2, b2 = e2
    return a1 * a2, a2 * b1 + b2


def s5_branch(u, lam_re, lam_im, log_dt, b_re, b_im, c_re, c_im, d_skip, w_glu):
    bsz, seq, _ = u.shape
    uf = u.astype(F32).reshape(bsz, seq, SSM_GROUPS, SSM_GROUP)
    lam = lax.complex(lam_re.astype(F32), lam_im.astype(F32))
    dt = jnp.exp(log_dt.astype(F32))[:, None]
    lam_bar = jnp.exp(lam * dt)
    b = lax.complex(b_re.astype(F32), b_im.astype(F32))
    b_bar = ((lam_bar - 1.0) / lam)[..., None] * b
    bu = jnp.einsum('bsgh,gph->bsgp', uf.astype(jnp.complex64), b_bar)
    a = jnp.broadcast_to(lam_bar, (1, seq) + lam_bar.shape)
    _, states = lax.associative_scan(_ssm_combine, (a, bu), axis=1)
    c = lax.complex(c_re.astype(F32), c_im.astype(F32))
    y = jnp.real(jnp.einsum('bsgp,ghp->bsgh', states, c))
    y = y + d_skip.astype(F32).reshape(SSM_GROUPS, SSM_GROUP) * uf
    y = jax.nn.gelu(y.reshape(bsz, seq, SSM_WIDTH))
    y = y * jax.nn.sigmoid(y @ w_glu.astype(F32))
    return y.astype(u.dtype)


def dsa_branch(q, k, v, qi, ki, wi, pos):
    bsz, seq = pos.shape
    topk = min(IDX_TOPK_MAX, seq // 4)
    nb = seq // Q_BLOCK
    kif = ki.astype(F32)
    gather = jax.vmap(lambda src, idx: src[idx])

    def to_blocks(t):
        t = t.reshape((bsz, nb, Q_BLOCK) + t.shape[2:])
        return jnp.moveaxis(t, 1, 0)

    def block_fn(args):
        qb, qib, wib, posb = args
        sc = jnp.einsum('bqhd,bsd->bqhs', qib.astype(F32), kif) * (IDX_DIM ** -0.5)
        iscore = jnp.einsum('bqh,bqhs->bqs', wib.astype(F32), jax.nn.relu(sc))
        causal = pos[:, None, :] <= posb[:, :, None]
        iscore = jnp.where(causal, iscore, -jnp.inf)
        _, idx = lax.top_k(iscore, topk)
        ks = gather(k, idx).astype(F32)
        vs = gather(v, idx).astype(F32)
        ps = gather(pos, idx)
        s = jnp.einsum('bqhd,bqkd->bqhk', qb.astype(F32), ks) * (DSA_HEAD_DIM ** -0.5)
        valid = (ps <= posb[..., None])[:, :, None, :]
        p = jax.nn.softmax(jnp.where(valid, s, -jnp.inf), axis=-1)
        o = jnp.einsum('bqhk,bqkd->bqhd', p, vs)
        return o.astype(qb.dtype)

    out = lax.map(block_fn, (to_blocks(q), to_blocks(qi), to_blocks(wi), to_blocks(pos)))
    out = jnp.moveaxis(out, 0, 1).reshape(bsz, seq, DSA_HEADS * DSA_HEAD_DIM)
    return out


def memory_branch(qm, km, vm):
    bsz, seq = qm.shape[:2]
    s = jnp.einsum('bshd,bmhd->bhsm', qm.astype(F32), km.astype(F32)) * (MEM_HEAD_DIM ** -0.5)
    p = jax.nn.softmax(s, axis=-1)
    o = jnp.einsum('bhsm,bmhd->bshd', p, vm.astype(F32))
    return o.reshape(bsz, seq, MEM_HEADS * MEM_HEAD_DIM).astype(qm.dtype)


def hier_moe(h, w_group, b_group, w_expert, b_expert, w_gate, w_up, w_down):
    bsz, seq, d = h.shape
    t = h.reshape(-1, d)
    hf = t.astype(F32)
    g_prob = jax.nn.softmax(hf @ w_group.astype(F32) + b_group.astype(F32), axis=-1)
    g_idx = jnp.argmax(g_prob, axis=-1)
    g_gate = jnp.take_along_axis(g_prob, g_idx[:, None], axis=-1)[:, 0]
    e_logits = (hf @ w_expert.astype(F32)).reshape(-1, N_GROUPS, EXPERTS_PER_GROUP) + b_expert.astype(F32)
    e_logits = jnp.take_along_axis(e_logits, g_idx[:, None, None], axis=1)[:, 0]
    e_prob = jax.nn.softmax(e_logits, axis=-1)
    top_p, top_i = lax.top_k(e_prob, TOP_K_EXPERTS)
    top_w = g_gate[:, None] * top_p / jnp.sum(top_p, axis=-1, keepdims=True)
    global_i = g_idx[:, None] * EXPERTS_PER_GROUP + top_i
    combine = jnp.sum(jax.nn.one_hot(global_i, N_EXPERTS, dtype=F32) * top_w[..., None], axis=1)
    out = jnp.zeros(hf.shape, F32)
    for e in range(N_EXPERTS):
        hid = jax.nn.silu(t @ w_gate[e]) * (t @ w_up[e])
        out = out + combine[:, e:e + 1] * (hid @ w_down[e]).astype(F32)
    return out.reshape(bsz, seq, d).astype(h.dtype)


def hybrid_layer(x, mem, positions, g_mix, g_mem, w_in, lam_re, lam_im, log_dt, b_re, b_im, c_re, c_im, d_skip, w_glu, g_q, g_k, g_qm, g_km, w_mem_kv, w_up_ssm, w_up_dsa, w_up_mem, w_out, g_ffn, w_group, b_group, w_expert, b_expert, w_gate, w_up, w_down):
    bsz, seq, _ = x.shape
    mlen = mem.shape[1]
    h = rms_norm(x, g_mix)
    proj = h @ w_in
    offs = [int(o) for o in np.cumsum(IN_SPLITS)[:-1]]
    u_ssm, q, k, v, qi, ki, wi, qm, gates = jnp.split(proj, offs, axis=-1)
    cos_a, sin_a = rope_tables(positions, DSA_HEAD_DIM)
    cos_i, sin_i = rope_tables(positions, IDX_DIM)
    q = apply_rope(rms_norm(q.reshape(bsz, seq, DSA_HEADS, DSA_HEAD_DIM), g_q), cos_a, sin_a)
    k = apply_rope(rms_norm(k, g_k), cos_a, sin_a)
    qi = apply_rope(qi.reshape(bsz, seq, IDX_HEADS, IDX_DIM), cos_i, sin_i)
    ki = apply_rope(ki, cos_i, sin_i)
    wi = wi * (IDX_HEADS ** -0.5)
    qm = rms_norm(qm.reshape(bsz, seq, MEM_HEADS, MEM_HEAD_DIM), g_qm)
    km, vm = jnp.split(rms_norm(mem, g_mem) @ w_mem_kv, 2, axis=-1)
    km = rms_norm(km.reshape(bsz, mlen, MEM_HEADS, MEM_HEAD_DIM), g_km)
    vm = vm.reshape(bsz, mlen, MEM_HEADS, MEM_HEAD_DIM)

    y_ssm = s5_branch(u_ssm, lam_re, lam_im, log_dt, b_re, b_im, c_re, c_im, d_skip, w_glu)
    y_dsa = dsa_branch(q, k, v, qi, ki, wi, positions)
    y_mem = memory_branch(qm, km, vm)

    g_s, g_d, g_m = jnp.split(jax.nn.sigmoid(gates), N_BRANCHES, axis=-1)
    merged = g_s * (y_ssm @ w_up_ssm) + g_d * (y_dsa @ w_up_dsa) + g_m * (y_mem @ w_up_mem)
    x = x + merged @ w_out
    x = x + hier_moe(rms_norm(x, g_ffn), w_group, b_group, w_expert, b_expert, w_gate, w_up, w_down)
    return x


def setup_inputs(seed: int = 0) -> dict:
    key = jax.random.key(seed)
    ks = iter(jax.random.split(key, 48))
    L = DEPTH

    def nrm(shape, scale):
        return jax.random.normal(next(ks), shape, F32) * scale

    def gain(shape):
        return 1.0 + 0.02 * jax.random.normal(next(ks), shape, F32)

    x = nrm((BATCH, SEQ, D_MODEL), 1.0)
    mem = nrm((BATCH, MEM_LEN, D_MODEL), 1.0)
    offset = jax.random.randint(next(ks), (BATCH, 1), 0, 1024, dtype=jnp.int32)
    positions = offset + jnp.arange(SEQ, dtype=jnp.int32)[None, :]
    n = jnp.arange(SSM_STATE, dtype=F32)
    lam_re = -0.5 + nrm((L, SSM_GROUPS, SSM_STATE), 0.01)
    lam_im = jnp.pi * n + nrm((L, SSM_GROUPS, SSM_STATE), 0.01)
    log_dt = jax.random.uniform(next(ks), (L, SSM_GROUPS), F32, math.log(1e-3), math.log(1e-1))
    return {
        'x': x,
        'mem': mem,
        'positions': positions,
        'g_mix': gain((L, D_MODEL)),
        'g_mem': gain((L, D_MODEL)),
        'w_in': nrm((L, D_MODEL, IN_COLS), D_MODEL ** -0.5),
        'lam_re': lam_re,
        'lam_im': lam_im,
        'log_dt': log_dt,
        'b_re': nrm((L, SSM_GROUPS, SSM_STATE, SSM_GROUP), (2 * SSM_GROUP) ** -0.5),
        'b_im': nrm((L, SSM_GROUPS, SSM_STATE, SSM_GROUP), (2 * SSM_GROUP) ** -0.5),
        'c_re': nrm((L, SSM_GROUPS, SSM_GROUP, SSM_STATE), SSM_STATE ** -0.5),
        'c_im': nrm((L, SSM_GROUPS, SSM_GROUP, SSM_STATE), SSM_STATE ** -0.5),
        'd_skip': nrm((L, SSM_WIDTH), 1.0),
        'w_glu': nrm((L, SSM_WIDTH, SSM_WIDTH), SSM_WIDTH ** -0.5),
        'g_q': gain((L, DSA_HEAD_DIM)),
        'g_k': gain((L, DSA_HEAD_DIM)),
        'g_qm': gain((L, MEM_HEAD_DIM)),
        'g_km': gain((L, MEM_HEAD_DIM)),
        'w_mem_kv': nrm((L, D_MODEL, 2 * MEM_HEADS * MEM_HEAD_DIM), D_MODEL ** -0.5),
        'w_up_ssm': nrm((L, SSM_WIDTH, D_MODEL), SSM_WIDTH ** -0.5),
        'w_up_dsa': nrm((L, DSA_HEADS * DSA_HEAD_DIM, D_MODEL), (DSA_HEADS * DSA_HEAD_DIM) ** -0.5),
        'w_up_mem': nrm((L, MEM_HEADS * MEM_HEAD_DIM, D_MODEL), (MEM_HEADS * MEM_HEAD_DIM) ** -0.5),
        'w_out': nrm((L, D_MODEL, D_MODEL), D_MODEL ** -0.5),
        'g_ffn': gain((L, D_MODEL)),
        'w_group': nrm((L, D_MODEL, N_GROUPS), D_MODEL ** -0.5),
        'b_group': nrm((L, N_GROUPS), 0.01),
        'w_expert': nrm((L, D_MODEL, N_EXPERTS), D_MODEL ** -0.5),
        'b_expert': nrm((L, N_GROUPS, EXPERTS_PER_GROUP), 0.01),
        'w_gate': nrm((L, N_EXPERTS, D_MODEL, EXPERT_FF), D_MODEL ** -0.5),
        'w_up': nrm((L, N_EXPERTS, D_MODEL, EXPERT_FF), D_MODEL ** -0.5),
        'w_down': nrm((L, N_EXPERTS, EXPERT_FF, D_MODEL), EXPERT_FF ** -0.5),
    }


def reference(x, mem, positions, g_mix, g_mem, w_in, lam_re, lam_im, log_dt, b_re, b_im, c_re, c_im, d_skip, w_glu, g_q, g_k, g_qm, g_km, w_mem_kv, w_up_ssm, w_up_dsa, w_up_mem, w_out, g_ffn, w_group, b_group, w_expert, b_expert, w_gate, w_up, w_down):
    for l in range(DEPTH):
        x = hybrid_layer(x, mem, positions, g_mix[l], g_mem[l], w_in[l], lam_re[l], lam_im[l], log_dt[l], b_re[l], b_im[l], c_re[l], c_im[l], d_skip[l], w_glu[l], g_q[l], g_k[l], g_qm[l], g_km[l], w_mem_kv[l], w_up_ssm[l], w_up_dsa[l], w_up_mem[l], w_out[l], g_ffn[l], w_group[l], b_group[l], w_expert[l], b_expert[l], w_gate[l], w_up[l], w_down[l])
    return x
```

```python
import math
from os import environ as _os_env
import numpy as np
from contextlib import ExitStack
import concourse.bass as bass
import concourse.mybir as mybir
from concourse.bass_utils import run_bass_kernel_spmd

F32 = mybir.dt.float32
BF16 = mybir.dt.bfloat16
I32 = mybir.dt.int32
AF = mybir.ActivationFunctionType
ALU = mybir.AluOpType
AX = mybir.AxisListType

NCORES = 8
MOE_SCALE = float(_os_env.get("MOE_SCALE", "0.8"))
QA = "pool"
import os as _os
MOE_SUB = int(_os.environ.get("MOE_SUB", "9"))
D = 1024
SEQ = 2048
MEM = 256
INC = 5060
TWO_PI = 2.0 * math.pi


class Buf:
    __slots__ = ("name", "w", "r")

    def __init__(self, name=""):
        self.name = name
        self.w = None
        self.r = {}


class T:
    def __init__(self, t, name=""):
        self.t = t
        self.b = Buf(name)

    def __getitem__(self, idx):
        return self.t[idx]


def _b(x):
    return x.b if isinstance(x, T) else x


class FW:
    NDMA = 32

    def __init__(self, nc, es):
        self.nc = nc
        self.es = es
        self.eng = {"pe": nc.tensor, "dve": nc.vector, "act": nc.scalar, "pool": nc.gpsimd, "sp": nc.sync}
        self.sem = {k: es.enter_context(nc.semaphore("s_" + k)) for k in self.eng}
        self.cnt = {k: 0 for k in self.eng}
        self.seen = {k: {} for k in self.eng}
        self.dsem = [es.enter_context(nc.semaphore("d%d" % i)) for i in range(self.NDMA)]
        self.dcnt = [0] * self.NDMA
        self.dnext = 0
        self.ninstr = 0
        self.nwaits = 0

    def sb(self, name, shape, dt, es=None):
        self.nalloc = getattr(self, "nalloc", 0) + 1
        return T((es or self.es).enter_context(self.nc.sbuf_tensor("%s_%d" % (name, self.nalloc), list(shape), dt)), name)

    def _wait(self, e, ev):
        sem, val, _ = ev
        key = id(sem)
        if self.seen[e].get(key, 0) >= val:
            return
        self.eng[e].wait_ge(sem, val)
        self.seen[e][key] = val
        self.nwaits += 1

    def _deps(self, e, reads, writes, pe_acc=False):
        for b in reads:
            if b.w is not None:
                self._wait(e, b.w)
        for b in writes:
            if b.w is not None and not (pe_acc and b.w[2] == "pe" and e == "pe"):
                self._wait(e, b.w)
            for ev in b.r.values():
                self._wait(e, ev)

    def _mark(self, ev, reads, writes):
        key = id(ev[0])
        for b in reads:
            old = b.r.get(key)
            if old is None or old[1] < ev[1]:
                b.r[key] = ev
        for b in writes:
            b.w = ev
            b.r = {}

    def op(self, e, fn, reads=(), writes=(), pe_acc=False):
        reads = [_b(x) for x in reads]
        writes = [_b(x) for x in writes]
        self._deps(e, reads, writes, pe_acc)
        ins = fn(self.eng[e])
        self.cnt[e] += 1
        ins.then_inc(self.sem[e], 1)
        self._mark((self.sem[e], self.cnt[e], e), reads, writes)
        self.ninstr += 1

    def dma(self, out, in_, reads=(), writes=(), q="sp", **kw):
        reads = [_b(x) for x in reads]
        writes = [_b(x) for x in writes]
        i = self.dnext
        self.dnext = (self.dnext + 1) % self.NDMA
        sem = self.dsem[i]
        if self.dcnt[i] > 0:
            self._wait(q, (sem, self.dcnt[i], "dma"))
        self._deps(q, reads, writes)
        ins = self.eng[q].dma_start(out=out, in_=in_, **kw)
        self.dcnt[i] += 16
        ins.then_inc(sem, 16)
        self._mark((sem, self.dcnt[i], "dma"), reads, writes)
        self.ninstr += 1

    def idma(self, out, out_off, in_, in_off, bound, reads=(), writes=()):
        q = "pool"
        reads = [_b(x) for x in reads]
        writes = [_b(x) for x in writes]
        i = self.dnext
        self.dnext = (self.dnext + 1) % self.NDMA
        sem = self.dsem[i]
        if self.dcnt[i] > 0:
            self._wait(q, (sem, self.dcnt[i], "dma"))
        self._deps(q, reads, writes)
        ins = self.nc.gpsimd.indirect_dma_start(out=out, out_offset=out_off, in_=in_, in_offset=in_off)
        self.dcnt[i] += 16
        ins.then_inc(sem, 16)
        self._mark((sem, self.dcnt[i], "dma"), reads, writes)
        self.ninstr += 1

    def barrier(self, engines=None):
        for e in (engines or list(self.eng)):
            for i in range(self.NDMA):
                if self.dcnt[i] > 0:
                    self._wait(e, (self.dsem[i], self.dcnt[i], "dma"))
            for k in self.eng:
                if k != e and self.cnt[k] > 0:
                    self._wait(e, (self.sem[k], self.cnt[k], k))


C_U, C_Q, C_KVI, C_QM, C_G0 = 0, 1, 2, 3, 4
C_GLU, C_UPS, C_UPD, C_UPM, C_OUT0, C_OUT1, C_MKV0, C_MKV1, C_EXP = 10, 11, 12, 13, 14, 15, 16, 17, 18
NCH = C_EXP + 96
NSLOT = 2
NTILE = 160


class _Stop(Exception):
    pass


def build_program(NB=4, NST=4, NEXP=32, dbg=(), upto=99):
    nc = bass.Bass("TRN2", target_bir_lowering=False)
    dbg = set(dbg)

    def stage(k):
        if upto >= k:
            with ExitStack() as e_:
                yield e_

    def din(name, shape, dt=F32):
        return nc.dram_tensor(name, list(shape), dt, kind="ExternalInput").ap()

    x_d = din("x", [NB, SEQ, D])
    mem_d = din("mem", [NB, MEM, D])
    pos_d = din("positions", [NB, SEQ], I32)
    g_mix_d = din("g_mix", [1, D]); g_mem_d = din("g_mem", [1, D]); g_ffn_d = din("g_ffn", [1, D])
    w_in_d = din("w_in", [D, INC])
    lam_re_d = din("lam_re", [32, 64]); lam_im_d = din("lam_im", [32, 64]); log_dt_d = din("log_dt", [1, 32])
    b_re_d = din("b_re", [32, 64, 16]); b_im_d = din("b_im", [32, 64, 16])
    c_re_d = din("c_re", [512, 64]); c_im_d = din("c_im", [512, 64])
    d_skip_d = din("d_skip", [1, 512])
    w_glu_d = din("w_glu", [512, 512])
    g_q_d = din("g_q", [1, 64]); g_k_d = din("g_k", [1, 64]); g_qm_d = din("g_qm", [1, 128]); g_km_d = din("g_km", [1, 128])
    w_mem_kv_d = din("w_mem_kv", [D, D])
    w_up_ssm_d = din("w_up_ssm", [512, D]); w_up_dsa_d = din("w_up_dsa", [512, D]); w_up_mem_d = din("w_up_mem", [512, D])
    w_out_d = din("w_out", [D, D])
    w_group_d = din("w_group", [D, 4]); b_group_d = din("b_group", [1, 4])
    w_expert_d = din("w_expert", [D, 32]); b_expert_d = din("b_expert", [1, 32])
    w_gate_d = din("w_gate", [32, D, 512]); w_upx_d = din("w_up", [32, D, 512]); w_down_d = din("w_down", [32, 512, D])
    ident_d = din("c_ident", [128, 128]); causal_d = din("c_causal", [128, 128]); jj_d = din("c_jj", [128, 128])
    invf_d = din("c_invf", [1, 32]); par_d = din("c_par", [128, 2])
    ltri_d = din("c_ltri", [128, 128]); thr_d = din("c_thr", [1, 64]); ltc_d = din("c_lt", [1, 1024])
    iot_d = din("c_iot", [1, NTILE]); pcol_d = din("c_pcol", [128, 1])

    out_d = nc.dram_tensor("out", [NB, SEQ, D], F32, kind="ExternalOutput").ap()
    wsc = nc.dram_tensor("wsc", [NCH, 128, 4096], BF16, kind="Internal").ap()
    HN = nc.dram_tensor("hn_scr", [NB * SEQ, D], BF16, kind="Internal").ap()
    XM = nc.dram_tensor("xm_scr", [NB * SEQ, D], F32, kind="Internal").ap()
    HS = nc.dram_tensor("hs_scr", [NTILE * 128, D], BF16, kind="Internal").ap()
    YS = nc.dram_tensor("ys_scr", [NTILE * 128, D], F32, kind="Internal").ap()
    HNb, XMb, HSb, YSb = Buf("HN"), Buf("XM"), Buf("HS"), Buf("YS")
    dbg_t = {}

    def dbg_out(name, shape, dt=F32):
        dbg_t[name] = nc.dram_tensor("dbg_" + name, list(shape), dt, kind="ExternalOutput").ap()
        return dbg_t[name]

    with ExitStack() as es:
        fw = FW(nc, es)
        op = fw.op

        def TT(e, out, in0, in1, o, R, W):
            op(e, lambda E: E.tensor_tensor(out=out, in0=in0, in1=in1, op=o), R, W)

        def TS(e, out, in0, s1, s2, o0, o1, R, W):
            if o1 is None:
                op(e, lambda E: E.tensor_scalar(out=out, in0=in0, scalar1=s1, scalar2=None, op0=o0), R, W)
            else:
                op(e, lambda E: E.tensor_scalar(out=out, in0=in0, scalar1=s1, scalar2=s2, op0=o0, op1=o1), R, W)

        def STT(out, in0, sc, in1, o0, o1, R, W):
            op("dve", lambda E: E.scalar_tensor_tensor(out=out, in0=in0, scalar=sc, in1=in1, op0=o0, op1=o1), R, W)

        def ACT(out, in_, func, R, W, scale=None, bias=None, accum=None):
            kw = {}
            if scale is not None:
                kw["scale"] = scale
            if bias is not None:
                kw["bias"] = bias
            if accum is not None:
                kw["accum_out"] = accum
            op("act", lambda E: E.activation(out=out, in_=in_, func=func, **kw), R, W)

        def CP(e, out, in_, R, W):
            if e == "act":
                op("act", lambda E: E.copy(out=out, in_=in_), R, W)
            else:
                op(e, lambda E: E.tensor_copy(out=out, in_=in_), R, W)

        def MM(out, lhsT, rhs, start, stop, R, W):
            op("pe", lambda E: E.matmul(out, lhsT=lhsT, rhs=rhs, start=start, stop=stop), R, W, pe_acc=True)

        def TR(out, in_, idn, R, W):
            op("pe", lambda E: E.transpose(out=out, in_=in_, identity=idn), R, W, pe_acc=True)

        def MSET(e, out, val, W):
            op(e, lambda E: E.memset(out, val), (), W)

        def rstd_of(ssq, dim, R):
            TS("dve", ssq, ssq, 1.0 / dim, 1e-6, ALU.mult, ALU.add, R, R)
            ACT(ssq, ssq, AF.Sqrt, R, R)
            op("dve", lambda E: E.reciprocal(out=ssq, in_=ssq), R, R)

        pbank = [T(es.enter_context(nc.psum_tensor("pb%d" % i, [128, 512], F32)), "pb%d" % i) for i in range(8)]
        ps_state = {"i": 0, "ring": list(range(8))}

        def PS():
            r = ps_state["ring"]
            ps_state["i"] = (ps_state["i"] + 1) % len(r)
            return pbank[r[ps_state["i"]]]

        sb = fw.sb
        esP1 = ExitStack()

        def sbp(name, shape, dt):
            return fw.sb(name, shape, dt, esP1)

        ident = sb("ident", [128, 128], F32)
        identb = sb("identb", [128, 128], BF16)
        gffnT = sb("gffnT", [128, 8], F32)
        ring = [sb("ring%d" % i, [128, 4096], BF16) for i in range(NSLOT)]
        ltri = sb("ltri", [128, 128], F32); ones = sb("ones", [128, 128], F32)
        thr = sb("thr", [128, 64], F32); iot = sb("iot", [128, NTILE], F32); pcol = sb("pcol", [128, 1], F32)
        NTT = NB * NST * 4
        OH1 = sb("OH1", [128, NTT, 32], BF16); OH2 = sb("OH2", [128, NTT, 32], BF16)
        RNK = sb("RNK", [128, NTT, 2], F32)
        W12 = sb("W12", [128, NTT, 2], F32)
        run = sb("run", [128, 32], F32)
        dpi = [sb("dpi%d" % c, [128, NTT], I32) for c in range(2)]
        widx = sb("widx", [128, NTILE, 3], I32)
        irep = sbp("irep", [128, 512], BF16)
        causal = sbp("causal", [128, 128], F32)
        causalb = sbp("causalb", [128, 128], BF16)
        zerob = sbp("zerob", [128, 128], BF16)
        jj = sbp("jj", [128, 128], F32)
        invf = sbp("invf", [128, 32], F32)
        par = sbp("par", [128, 2], F32)
        gmixT = sbp("gmixT", [128, 8], F32)
        gffnT64 = sbp("gffnT64", [64, 16], F32)
        gmemT = sbp("gmemT", [128, 8], F32)
        gq = sbp("gq", [128, 64], F32); gk = sbp("gk", [128, 64], F32)
        gqm = sbp("gqm", [128, 128], F32); gkm = sbp("gkm", [128, 128], F32)
        wr32 = sbp("wr32", [128, 8, 36], F32)
        rbias = sbp("rbias", [128, 36], F32)
        XsB = [[Buf("Xs_%d_%d" % (t_, h_)) for h_ in range(2)] for t_ in range(4)]
        XsAll = [XsB[t_][h_] for t_ in range(4) for h_ in range(2)]
        cosb = sbp("cosb", [128, 16, 32], F32); sinb = sbp("sinb", [128, 16, 32], F32)
        kT = sbp("kT", [64, SEQ], BF16)
        kiT = sbp("kiT", [64, SEQ], F32)
        vaug = sbp("vaug", [128, 16, 65], BF16)
        kmT = sbp("kmT", [128, 4, MEM], BF16)
        vmaug = sbp("vmaug", [128, 2, 4, 129], BF16)
        s5_mag = sbp("s5_mag", [128, 16], F32)
        s5_c128 = sbp("s5_c128", [128, 16], F32); s5_s128 = sbp("s5_s128", [128, 16], F32); s5_s128n = sbp("s5_s128n", [128, 16], F32)
        tabc = sbp("tabc", [128, 16, 128], F32); tabs = sbp("tabs", [128, 16, 128], F32)
        BTre = sbp("BTre", [128, 16, 128], BF16); BTim = sbp("BTim", [128, 16, 128], BF16)
        Cwre = sbp("Cwre", [128, 16, 64], BF16); Cwim = sbp("Cwim", [128, 16, 64], BF16)
        dsk = sbp("dsk", [128, 4], F32)
        carr = sbp("carr", [128, 16, 2], F32)

        chunk_buf = [Buf("ch%d" % i) for i in range(NCH)]

        class WRing:
            def __init__(self, slots, q="sp"):
                self.q = q
                self.slots = slots
                self.plan = []
                self.next_issue = 0
                self.next_req = 0
                self.free = list(range(len(slots)))
                self.slot_of = {}

            def pump(self):
                while self.free and self.next_issue < len(self.plan):
                    cid = self.plan[self.next_issue]
                    s_ = self.free.pop(0)
                    fw.dma(self.slots[s_][:], wsc[cid], reads=[chunk_buf[cid]], writes=[self.slots[s_]], q=self.q)
                    self.slot_of[self.next_issue] = s_
                    self.next_issue += 1

            def get(self, cid, kc, n):
                k = self.next_req
                assert self.plan[k] == cid, (k, self.plan[k], cid)
                self.pump()
                assert k in self.slot_of, "ring overflow"
                self.next_req += 1
                s_ = self.slot_of[k]
                return self.slots[s_], self.slots[s_][:, 0:kc * n].rearrange("p (k n) -> p k n", k=kc), (k, s_)

            def rel(self, h):
                self.free.append(h[1])
                self.pump()

        ringA = WRing(ring[0:2], q=QA)

        psr = {"A": {"ring": [0, 1, 2, 3, 4, 5, 6, 7], "i": 0}}

        def PSs(s_):
            r_ = psr[s_]
            r_["i"] = (r_["i"] + 1) % len(r_["ring"])
            return pbank[r_["ring"][r_["i"]]]

        def ps_reserve(s_):
            r_ = psr[s_]
            bk = r_["ring"].pop(0)
            r_["i"] = 0
            return bk

        def ps_release(s_, bk):
            psr[s_]["ring"].append(bk)

        try:
            fw.dma(ident[:], ident_d, writes=[ident])
            fw.dma(causal[:], causal_d, writes=[causal])
            fw.dma(jj[:], jj_d, writes=[jj])
            fw.dma(invf[:], invf_d.partition_broadcast(128), writes=[invf])
            fw.dma(par[:], par_d, writes=[par])
            fw.dma(ltri[:], ltri_d, writes=[ltri])
            fw.dma(thr[:], thr_d.partition_broadcast(128), writes=[thr])
            fw.dma(iot[:], iot_d.partition_broadcast(128), writes=[iot])
            fw.dma(pcol[:], pcol_d, writes=[pcol])
            MSET("dve", ones[:], 1.0, [ones])
            MSET("dve", run[:], 0.0, [run])
            with nc.allow_non_contiguous_dma(reason="tiny gain loads"):
                fw.dma(gmixT[:], g_mix_d.rearrange("o (k p) -> p (o k)", p=128), writes=[gmixT])
                fw.dma(gffnT[:], g_ffn_d.rearrange("o (k p) -> p (o k)", p=128), writes=[gffnT])
                fw.dma(gffnT64[:], g_ffn_d.rearrange("o (k p) -> p (o k)", p=64), writes=[gffnT64])
                fw.dma(gmemT[:], g_mem_d.rearrange("o (k p) -> p (o k)", p=128), writes=[gmemT])
            fw.dma(gq[:], g_q_d.partition_broadcast(128), writes=[gq])
            fw.dma(gk[:], g_k_d.partition_broadcast(128), writes=[gk])
            fw.dma(gqm[:], g_qm_d.partition_broadcast(128), writes=[gqm])
            fw.dma(gkm[:], g_km_d.partition_broadcast(128), writes=[gkm])
            fw.dma(wr32[:, :, 0:4], w_group_d.rearrange("(k p) n -> p k n", p=128), writes=[wr32])
            fw.dma(wr32[:, :, 4:36], w_expert_d.rearrange("(k p) n -> p k n", p=128), writes=[wr32])
            fw.dma(rbias[:, 0:4], b_group_d.partition_broadcast(128), writes=[rbias])
            fw.dma(rbias[:, 4:36], b_expert_d.partition_broadcast(128), writes=[rbias])
            CP("dve", identb[:], ident[:], [ident], [identb])
            for r in range(4):
                CP("dve", irep[:, r * 128:(r + 1) * 128], ident[:], [ident], [irep])
            TS("dve", causalb[:], causal[:], -1.0, -30000.0, ALU.is_lt, ALU.mult, [causal], [causalb])
            MSET("dve", vaug[:, :, 64:65], 1.0, [vaug])
            MSET("dve", zerob[:], 0.0, [zerob])
            MSET("dve", vmaug[:, :, :, 128:129], 1.0, [vmaug])

            def w_in_cols(c0, c1):
                return w_in_d.rearrange("(k p) n -> p k n", p=128)[:, :, c0:c1], 8, c1 - c0

            srcs = {C_U: w_in_cols(0, 512), C_Q: w_in_cols(512, 1024), C_KVI: w_in_cols(1024, 1476), C_QM: w_in_cols(1476, 1988)}
            for i in range(6):
                srcs[C_G0 + i] = w_in_cols(1988 + 512 * i, 1988 + 512 * (i + 1))
            srcs[C_GLU] = (w_glu_d.rearrange("(k p) n -> p k n", p=128), 4, 512)
            srcs[C_UPS] = (w_up_ssm_d.rearrange("(k p) n -> p k n", p=128), 4, 1024)
            srcs[C_UPD] = (w_up_dsa_d.rearrange("(k p) n -> p k n", p=128), 4, 1024)
            srcs[C_UPM] = (w_up_mem_d.rearrange("(k p) n -> p k n", p=128), 4, 1024)
            srcs[C_OUT0] = (w_out_d.rearrange("(k p) n -> p k n", p=128)[:, :, 0:512], 8, 512)
            srcs[C_OUT1] = (w_out_d.rearrange("(k p) n -> p k n", p=128)[:, :, 512:1024], 8, 512)
            srcs[C_MKV0] = (w_mem_kv_d.rearrange("(k p) n -> p k n", p=128)[:, :, 0:512], 8, 512)
            srcs[C_MKV1] = (w_mem_kv_d.rearrange("(k p) n -> p k n", p=128)[:, :, 512:1024], 8, 512)
            for e in range(32):
                srcs[C_EXP + 3 * e] = (w_gate_d[e].rearrange("(k p) n -> p k n", p=128), 8, 512)
                srcs[C_EXP + 3 * e + 1] = (w_upx_d[e].rearrange("(k p) n -> p k n", p=128), 8, 512)
                srcs[C_EXP + 3 * e + 2] = (w_down_d[e].rearrange("(k p) n -> p k n", p=128), 4, 1024)

            with ExitStack() as es0:
                st32 = [sb("st32_%d" % i, [128, 4096], F32, es0) for i in range(3)]
                st16 = [sb("st16_%d" % i, [128, 4096], BF16, es0) for i in range(3)]
                cast_eng = ["pool", "dve", "act"]
                z16 = sb("z16", [128, 4096], BF16, es0)
                MSET("pool", z16[:], 0.0, [z16])
                hs_z = HS.rearrange("(a p k) n -> a p (k n)", p=128, k=4)
                for a_ in range(NTILE // 4):
                    fw.dma(hs_z[a_], z16[:], reads=[z16], writes=[HSb])
                order = list(range(C_EXP))
                for n_, cid in enumerate(order):
                    src, kc, n = srcs[cid]
                    a = st32[n_ % 3]; bq = st16[n_ % 3]
                    fw.dma(a[:, 0:kc * n].rearrange("p (k n) -> p k n", k=kc), src, writes=[a])
                    CP(cast_eng[n_ % 3], bq[:, 0:kc * n], a[:, 0:kc * n], [a], [bq])
                    fw.dma(wsc[cid][:, 0:kc * n], bq[:, 0:kc * n], reads=[bq], writes=[chunk_buf[cid]], q="act")

                fw.barrier()
            fw.barrier()
            with ExitStack() as es0:
                lre = sb("lre", [128, 16], F32, es0); lim = sb("lim", [128, 16], F32, es0); dtv = sb("dtv", [128, 16], F32, es0)
                with nc.allow_non_contiguous_dma(reason="tiny param loads"):
                    fw.dma(lre[:], lam_re_d.rearrange("(c g) p -> (g p) c", g=2), writes=[lre])
                    fw.dma(lim[:], lam_im_d.rearrange("(c g) p -> (g p) c", g=2), writes=[lim])
                    ldt2 = log_dt_d.rearrange("o (c g) -> o g c", g=2)
                    for gl in range(2):
                        fw.dma(dtv[gl * 64:(gl + 1) * 64, :], ldt2[:, gl, :].partition_broadcast(64), writes=[dtv])
                    fw.dma(dsk[:], d_skip_d.rearrange("o (c p) -> p (o c)", p=128), writes=[dsk])
                ACT(dtv[:], dtv[:], AF.Exp, [dtv], [dtv])
                are = sb("are", [128, 16], F32, es0); aim = sb("aim", [128, 16], F32, es0)
                TT("dve", are[:], lre[:], dtv[:], ALU.mult, [lre, dtv], [are])
                TT("dve", aim[:], lim[:], dtv[:], ALU.mult, [lim, dtv], [aim])
                ACT(s5_mag[:], are[:], AF.Exp, [are], [s5_mag])

                def wrap_pi(xt, shape, tmp_i, tmp_f):
                    TS("dve", tmp_f[:], xt[:], 1.0 / TWO_PI, None, ALU.mult, None, [xt], [tmp_f])
                    CP("dve", tmp_i[:], tmp_f[:], [tmp_f], [tmp_i])
                    CP("dve", tmp_f[:], tmp_i[:], [tmp_i], [tmp_f])
                    c1 = 6.28125
                    c2 = TWO_PI - c1
                    STT(xt[:], tmp_f[:], -c1, xt[:], ALU.mult, ALU.add, [tmp_f, xt], [xt])
                    STT(xt[:], tmp_f[:], -c2, xt[:], ALU.mult, ALU.add, [tmp_f, xt], [xt])
                    TS("dve", tmp_f[:], xt[:], math.pi, -TWO_PI, ALU.is_gt, ALU.mult, [xt], [tmp_f])
                    TT("dve", xt[:], xt[:], tmp_f[:], ALU.add, [xt, tmp_f], [xt])
                    TS("dve", tmp_f[:], xt[:], -math.pi, TWO_PI, ALU.is_lt, ALU.mult, [xt], [tmp_f])
                    TT("dve", xt[:], xt[:], tmp_f[:], ALU.add, [xt, tmp_f], [xt])

                def cos_arg(dst, src, tmp_f):
                    TS("dve", tmp_f[:], src[:], math.pi / 2, -TWO_PI, ALU.is_gt, ALU.mult, [src], [tmp_f])
                    STT(dst[:], src[:], math.pi / 2, tmp_f[:], ALU.add, ALU.add, [src, tmp_f], [dst])

                ti16 = sb("ti16", [128, 16], I32, es0); tf16 = sb("tf16", [128, 16], F32, es0)
                wrap_pi(aim, None, ti16, tf16)
                lbr = sb("lbr", [128, 16], F32, es0); lbi = sb("lbi", [128, 16], F32, es0); ca16 = sb("ca16", [128, 16], F32, es0)
                cos_arg(ca16, aim, tf16)
                ACT(lbi[:], aim[:], AF.Sin, [aim], [lbi])
                ACT(lbr[:], ca16[:], AF.Sin, [ca16], [lbr])
                TT("dve", lbr[:], lbr[:], s5_mag[:], ALU.mult, [lbr, s5_mag], [lbr])
                TT("dve", lbi[:], lbi[:], s5_mag[:], ALU.mult, [lbi, s5_mag], [lbi])
                n2 = sb("n2", [128, 16], F32, es0); t16a = sb("t16a", [128, 16], F32, es0)
                cfr = sb("cfr", [128, 16], F32, es0); cfi = sb("cfi", [128, 16], F32, es0); lb1 = sb("lb1", [128, 16], F32, es0)
                TT("dve", n2[:], lre[:], lre[:], ALU.mult, [lre], [n2])
                TT("dve", t16a[:], lim[:], lim[:], ALU.mult, [lim], [t16a])
                TT("dve", n2[:], n2[:], t16a[:], ALU.add, [n2, t16a], [n2])
                op("dve", lambda E: E.reciprocal(out=n2[:], in_=n2[:]), [n2], [n2])
                TS("dve", lb1[:], lbr[:], -1.0, None, ALU.add, None, [lbr], [lb1])
                TT("dve", cfr[:], lb1[:], lre[:], ALU.mult, [lb1, lre], [cfr])
                TT("dve", t16a[:], lbi[:], lim[:], ALU.mult, [lbi, lim], [t16a])
                TT("dve", cfr[:], cfr[:], t16a[:], ALU.add, [cfr, t16a], [cfr])
                TT("dve", cfr[:], cfr[:], n2[:], ALU.mult, [cfr, n2], [cfr])
                TT("dve", cfi[:], lbi[:], lre[:], ALU.mult, [lbi, lre], [cfi])
                TT("dve", t16a[:], lb1[:], lim[:], ALU.mult, [lb1, lim], [t16a])
                TT("dve", cfi[:], cfi[:], t16a[:], ALU.subtract, [cfi, t16a], [cfi])
                TT("dve", cfi[:], cfi[:], n2[:], ALU.mult, [cfi, n2], [cfi])
                bre = sb("bre", [128, 16, 16], F32, es0); bim = sb("bim", [128, 16, 16], F32, es0)
                fw.dma(bre[:], b_re_d.rearrange("(c g) p h -> (g p) c h", g=2), writes=[bre])
                fw.dma(bim[:], b_im_d.rearrange("(c g) p h -> (g p) c h", g=2), writes=[bim])
                bbr = sb("bbr", [128, 16, 16], F32, es0); bbi = sb("bbi", [128, 16, 16], F32, es0); tb = sb("tb", [128, 16, 16], F32, es0)
                cfr_b = cfr[:].unsqueeze(2).to_broadcast([128, 16, 16]); cfi_b = cfi[:].unsqueeze(2).to_broadcast([128, 16, 16])
                TT("dve", bbr[:], bre[:], cfr_b, ALU.mult, [bre, cfr], [bbr])
                TT("dve", tb[:], bim[:], cfi_b, ALU.mult, [bim, cfi], [tb])
                TT("dve", bbr[:], bbr[:], tb[:], ALU.subtract, [bbr, tb], [bbr])
                TT("dve", bbi[:], bim[:], cfr_b, ALU.mult, [bim, cfr], [bbi])
                TT("dve", tb[:], bre[:], cfi_b, ALU.mult, [bre, cfi], [tb])
                TT("dve", bbi[:], bbi[:], tb[:], ALU.add, [bbi, tb], [bbi])
                Xw = sb("Xw", [128, 16, 128], F32, es0)
                for (src_bb, dstBT) in ((bbr, BTre), (bbi, BTim)):
                    MSET("pool", Xw[:], 0.0, [Xw])
                    for gl in range(2):
                        for r in range(4):
                            CP("dve", Xw[gl * 64:(gl + 1) * 64, r::4, 32 * r + 16 * gl:32 * r + 16 * gl + 16],
                               src_bb[gl * 64:(gl + 1) * 64, r::4, :], [src_bb], [Xw])
                    for fc in range(4):
                        pb = PS()
                        for r in range(4):
                            MM(pb[:, r * 128:(r + 1) * 128], Xw[:, 4 * fc + r, :], ident[:], True, True, [Xw, ident], [pb])
                        CP("act", dstBT[:, 4 * fc:4 * fc + 4, :], pb[:].rearrange("p (r n) -> p r n", r=4), [pb], [dstBT])
                for (c_d, dstC, sign) in ((c_re_d, Cwre, 1.0), (c_im_d, Cwim, -1.0)):
                    MSET("dve", dstC[:], 0.0, [dstC])
                    for k in range(4):
                        cl = sb("cl_%d_%d" % (k, int(sign > 0)), [128, 64], F32, es0)
                        aw = sb("aw_%d_%d" % (k, int(sign > 0)), [128, 128], F32, es0)
                        fw.dma(cl[:], c_d[k * 128:(k + 1) * 128, :], writes=[cl])
                        TS("dve", aw[:, 0:64], cl[:], par[:, 0:1], None, ALU.mult, None, [cl, par], [aw])
                        TS("dve", aw[:, 64:128], cl[:], par[:, 1:2], None, ALU.mult, None, [cl, par], [aw])
                        pb = PS()
                        for r in range(4):
                            MM(pb[:, 32 * r:32 * r + 32], aw[:], ident[:, 32 * r:32 * r + 32], True, True, [aw, ident], [pb])
                        for r in range(4):
                            ACT(dstC[:, 4 * k + r, 32 * (r % 2):32 * (r % 2) + 32], pb[:, 32 * r:32 * r + 32], AF.Copy, [pb], [dstC], scale=sign)
                ang = sb("ang", [128, 16, 128], F32, es0); angi = sb("angi", [128, 16, 128], I32, es0); angf = sb("angf", [128, 16, 128], F32, es0)
                TT("dve", ang[:], jj[:].unsqueeze(1).to_broadcast([128, 16, 128]), aim[:].unsqueeze(2).to_broadcast([128, 16, 128]), ALU.mult, [jj, aim], [ang])
                wrap_pi(ang, None, angi, angf)
                ACT(tabs[:], ang[:], AF.Sin, [ang], [tabs])
                ang2 = sb("ang2", [128, 16, 128], F32, es0)
                cos_arg(ang2, ang, angf)
                ACT(tabc[:], ang2[:], AF.Sin, [ang2], [tabc])
                CP("dve", s5_c128[:], tabc[:, :, 127], [tabc], [s5_c128])
                CP("dve", s5_s128[:], tabs[:, :, 127], [tabs], [s5_s128])
                TS("dve", s5_s128n[:], s5_s128[:], -1.0, None, ALU.mult, None, [s5_s128], [s5_s128n])
                fw.barrier()
            fw.barrier()

            planA_st = [C_U] + [C_Q, C_KVI, C_QM] * 4 + [C_GLU] + [C_G0, C_G0 + 1, C_UPS, C_G0 + 2, C_G0 + 3, C_UPD, C_G0 + 4, C_G0 + 5, C_UPM, C_OUT0, C_OUT1]
            planB_st = [C_EXP + 3 * e + j for e in range(NEXP) for j in range(3)]
            for b in range(NB):
                ringA.plan += [C_MKV0, C_MKV1]
                for s in range(NST):
                    ringA.plan += planA_st


            def fence(tiles):
                evs = []
                for t_ in tiles:
                    bb = _b(t_)
                    if bb.w is not None:
                        evs.append(bb.w)
                    evs += list(bb.r.values())
                for e_ in fw.eng:
                    for ev in evs:
                        fw._wait(e_, ev)

            def rope(dst, src, cbx, sbx, tmp, R, W, eng="dve"):
                x1 = src[:, :, 0:32]; x2 = src[:, :, 32:64]
                TT(eng, tmp, x2, sbx, ALU.mult, R, W)
                TT(eng, dst[:, :, 0:32], x1, cbx, ALU.mult, R, W)
                TT(eng, dst[:, :, 0:32], dst[:, :, 0:32], tmp, ALU.subtract, W, W)
                TT(eng, tmp, x1, sbx, ALU.mult, R, W)
                TT(eng, dst[:, :, 32:64], x2, cbx, ALU.mult, R, W)
                TT(eng, dst[:, :, 32:64], dst[:, :, 32:64], tmp, ALU.add, W, W)

            def batch_prep(b):
                with ExitStack() as esb:
                    posi = sb("posi", [16, 128], I32, esb); posf = sb("posf", [16, 128], F32, esb); post = sb("post", [128, 16], F32, esb)
                    fw.dma(posi[:], pos_d[b].rearrange("(t p) -> t p", p=128), writes=[posi])
                    CP("dve", posf[:], posi[:], [posi], [posf])
                    pb = PS()
                    TR(pb[:, 0:16], posf[:], ident[0:16, 0:16], [posf, ident], [pb])
                    CP("dve", post[:], pb[:, 0:16], [pb], [post])
                    angb = sb("angb", [128, 16, 32], F32, esb); angbi = sb("angbi", [128, 16, 32], I32, esb); angbf = sb("angbf", [128, 16, 32], F32, esb)
                    angb2 = sb("angb2", [128, 16, 32], F32, esb)
                    TT("dve", angb[:], post[:].unsqueeze(2).to_broadcast([128, 16, 32]), invf[:].unsqueeze(1).to_broadcast([128, 16, 32]), ALU.mult, [post, invf], [angb])
                    wrap_pi(angb, None, angbi, angbf)
                    ACT(sinb[:], angb[:], AF.Sin, [angb], [sinb])
                    cos_arg(angb2, angb, angbf)
                    ACT(cosb[:], angb2[:], AF.Sin, [angb2], [cosb])
                    MSET("dve", carr[:], 0.0, [carr])
                    memx = sb("memx", [128, 2, D], F32, esb); memb = sb("memb", [128, D], BF16, esb); memT = sb("memT", [128, 8, MEM], BF16, esb)
                    mss = sb("mss", [128, 2], F32, esb)
                    fw.dma(memx[:], mem_d[b].rearrange("(t p) d -> p t d", p=128), writes=[memx])
                    for mt in range(2):
                        ACT(memb[:], memx[:, mt, :], AF.Square, [memx], [memb, mss], accum=mss[:, mt:mt + 1])
                    rstd_of(mss[:], D, [mss])
                    for mt in range(2):
                        TS("dve", memb[:], memx[:, mt, :], mss[:, mt:mt + 1], None, ALU.mult, None, [memx, mss], [memb])
                        pb = PS()
                        pbv = pb[:].bitcast(BF16)
                        for kc in range(8):
                            TR(pbv[:, kc * 128:(kc + 1) * 128], memb[:, kc * 128:(kc + 1) * 128], identb[:], [memb, identb], [pb])
                        TT("dve", memT[:, :, mt * 128:(mt + 1) * 128], pbv[:, 0:1024].rearrange("p (k n) -> p k n", k=8),
                           gmemT[:].unsqueeze(2).to_broadcast([128, 8, 128]), ALU.mult, [pb, gmemT], [memT])
                    slK, wK, hK = ringA.get(C_MKV0, 8, 512)
                    slV, wV, hV = ringA.get(C_MKV1, 8, 512)
                    kmraw = sb("kmraw", [128, 512], F32, esb); kmsq = sb("kmsq", [128, 512], F32, esb); kss = sb("kss", [128, 4], F32, esb)
                    kmn = sb("kmn", [128, 512], BF16, esb)
                    for mt in range(2):
                        pb = PS()
                        for kc in range(8):
                            MM(pb[:], memT[:, kc, mt * 128:(mt + 1) * 128], wK[:, kc, :], kc == 0, kc == 7, [memT, slK], [pb])
                        CP("act", kmraw[:], pb[:], [pb], [kmraw])
                        TT("dve", kmsq[:], kmraw[:], kmraw[:], ALU.mult, [kmraw], [kmsq])
                        op("dve", lambda E: E.tensor_reduce(out=kss[:], in_=kmsq[:].rearrange("p (h d) -> p h d", h=4), axis=AX.X, op=ALU.add), [kmsq], [kss])
                        rstd_of(kss[:], 128, [kss])
                        TT("dve", kmraw[:].rearrange("p (h d) -> p h d", h=4), kmraw[:].rearrange("p (h d) -> p h d", h=4),
                           kss[:].unsqueeze(2).to_broadcast([128, 4, 128]), ALU.mult, [kmraw, kss], [kmraw])
                        TT("dve", kmn[:].rearrange("p (h d) -> p h d", h=4), kmraw[:].rearrange("p (h d) -> p h d", h=4),
                           gkm[:].unsqueeze(1).to_broadcast([128, 4, 128]), ALU.mult, [kmraw, gkm], [kmn])
                        pb2 = PS()
                        pbv = pb2[:].bitcast(BF16)
                        for h in range(4):
                            TR(pbv[:, h * 128:(h + 1) * 128], kmn[:, h * 128:(h + 1) * 128], identb[:], [kmn, identb], [pb2])
                        CP("act", kmT[:, :, mt * 128:(mt + 1) * 128], pbv[:, 0:512].rearrange("p (h n) -> p h n", h=4), [pb2], [kmT])
                        pb3 = PS()
                        for kc in range(8):
                            MM(pb3[:], memT[:, kc, mt * 128:(mt + 1) * 128], wV[:, kc, :], kc == 0, kc == 7, [memT, slV], [pb3])
                        CP("act", vmaug[:, mt, :, 0:128], pb3[:].rearrange("p (h d) -> p h d", h=4), [pb3], [vmaug])
                    ringA.rel(hK); ringA.rel(hV)
                    fw.barrier()

            def attn_AG(b, st, A, esa):
                tok0 = st * 512
                hT = A["hT"]
                ydsaT, ymemT, yssmT, rst = A["ydsaT"], A["ymemT"], A["yssmT"], A["rst"]
                exa = ExitStack()
                xt = [sb("xt%d" % i, [128, D], F32, exa) for i in range(2)]
                hb = sb("hb", [128, D], BF16, exa)
                PSa = lambda: PSs("A")
                for t in range(4):
                    x_ = xt[t % 2]
                    fw.dma(x_[:], x_d[b, tok0 + t * 128:tok0 + (t + 1) * 128, :], writes=[x_], q=QA)
                    ACT(hb[:], x_[:], AF.Square, [x_], [hb, rst], accum=rst[:, t:t + 1])
                    yield 0.4
                    rstd_of(rst[:, t:t + 1], D, [rst])
                    TS("dve", hb[:], x_[:], rst[:, t:t + 1], None, ALU.mult, None, [x_, rst], [hb])
                    pb = PSa(); pbv = pb[:].bitcast(BF16)
                    for kc in range(8):
                        TR(pbv[:, kc * 128:(kc + 1) * 128], hb[:, kc * 128:(kc + 1) * 128], identb[:], [hb, identb], [pb])
                    TT("dve", hT[:, :, t * 128:(t + 1) * 128], pbv[:, 0:1024].rearrange("p (k n) -> p k n", k=8),
                       gmixT[:].unsqueeze(2).to_broadcast([128, 8, 128]), ALU.mult, [pb, gmixT], [hT])
                    yield 0.4
                fence(xt + [hb])
                exa.close()
                e2a = ExitStack(); e2b = ExitStack()
                uT = sb("uT", [128, 4, 512], BF16, e2a)
                qT = sb("qT", [64, 8, 512], BF16, e2b); qiT = sb("qiT", [64, 4, 512], F32, e2b)
                qmT = sb("qmT", [128, 4, 512], BF16, e2b); wis = sb("wis", [128, 4, 4], F32, e2b)
                sl, w, hh = ringA.get(C_U, 8, 512)
                for fc in range(4):
                    pb = PSa()
                    for kc in range(8):
                        MM(pb[:], w[:, kc, fc * 128:(fc + 1) * 128], hT[:, kc, :], kc == 0, kc == 7, [sl, hT], [pb])
                    CP("act", uT[:, fc, :], pb[:], [pb], [uT])
                    yield 0.4
                ringA.rel(hh)
                with ExitStack() as est:
                    raws = [(sb("qraw", [128, 512], F32, est), sb("kviraw", [128, 452], F32, est), sb("qmraw", [128, 512], F32, est)) for i_ in range(2)]
                    sq = sb("sq", [128, 512], F32, est); s8 = sb("s8", [128, 12], F32, est)
                    qn = sb("qn", [128, 8, 64], F32, est); qr = sb("qr", [128, 8, 64], BF16, est); tq = sb("tq", [128, 8, 32], F32, est)
                    kn = sb("kn", [128, 1, 64], F32, est); kr = sb("kr", [128, 1, 64], BF16, est)
                    qir = sb("qir", [128, 5, 64], F32, est)
                    qmn = sb("qmn", [128, 4, 128], BF16, est)
                    dtiles = [x_ for r_ in raws for x_ in r_] + [sq, s8, qn, qr, tq, kn, kr, qir, qmn]

                    def proj(t_):
                        tsl_ = slice(t_ * 128, (t_ + 1) * 128)
                        for (cid, dst, n) in zip((C_Q, C_KVI, C_QM), raws[t_ % 2], (512, 452, 512)):
                            sl, w, hh = ringA.get(cid, 8, n)
                            pb = PSa()
                            for kc in range(8):
                                MM(pb[:, 0:n], hT[:, kc, tsl_], w[:, kc, :], kc == 0, kc == 7, [hT, sl], [pb])
                            CP("act", dst[:], pb[:, 0:n], [pb], [dst])
                            ringA.rel(hh)
                    cst32 = [sb("cst32_%d" % i_, [128, 4096], F32, est) for i_ in range(2)]
                    cst16 = [sb("cst16_%d" % i_, [128, 4096], BF16, est) for i_ in range(2)]
                    exp_ids = [C_EXP + 3 * e_ + j_ for e_ in range(NEXP) for j_ in range(3)]
                    n_st = NB * NST
                    per = (len(exp_ids) + n_st - 1) // n_st
                    my_ids = exp_ids[(b * NST + st) * per:(b * NST + st + 1) * per]

                    def c_in(k_):
                        src_, kc_, n_ = srcs[my_ids[k_]]
                        a_ = cst32[k_ % 2]
                        fw.dma(a_[:, 0:kc_ * n_].rearrange("p (k n) -> p k n", k=kc_), src_, writes=[a_])

                    def c_out(k_):
                        cid_ = my_ids[k_]
                        src_, kc_, n_ = srcs[cid_]
                        a_ = cst32[k_ % 2]; q_ = cst16[k_ % 2]
                        CP("act", q_[:, 0:kc_ * n_], a_[:, 0:kc_ * n_], [a_], [q_])
                        fw.dma(wsc[cid_][:, 0:kc_ * n_], q_[:, 0:kc_ * n_], reads=[q_], writes=[chunk_buf[cid_]])

                    for k_ in range(min(2, len(my_ids))):
                        c_in(k_)
                    c_next = 0
                    dtiles += cst32 + cst16
                    proj(0)
                    for t in range(4):
                        gt_ = st * 4 + t
                        tsl = slice(t * 128, (t + 1) * 128)
                        cos_t = cosb[:, gt_, :]; sin_t = sinb[:, gt_, :]
                        qraw, kviraw, qmraw = raws[t % 2]
                        if t + 1 < 4:
                            proj(t + 1)
                        yield 0.4
                        q3 = qraw[:].rearrange("p (h d) -> p h d", h=8)
                        TT("pool", sq[:], qraw[:], qraw[:], ALU.mult, [qraw], [sq])
                        op("dve", lambda E: E.tensor_reduce(out=s8[:, 0:8], in_=sq[:].rearrange("p (h d) -> p h d", h=8), axis=AX.X, op=ALU.add), [sq], [s8])
                        TT("pool", sq[:, 0:64], kviraw[:, 0:64], kviraw[:, 0:64], ALU.mult, [kviraw, sq], [sq])
                        op("dve", lambda E: E.tensor_reduce(out=s8[:, 8:9], in_=sq[:, 0:64], axis=AX.X, op=ALU.add), [sq], [s8])
                        rstd_of(s8[:, 0:9], 64, [s8])
                        TT("dve", qn[:], q3, s8[:, 0:8].unsqueeze(2).to_broadcast([128, 8, 64]), ALU.mult, [qraw, s8], [qn])
                        TT("pool", qn[:], qn[:], gq[:].unsqueeze(1).to_broadcast([128, 8, 64]), ALU.mult, [qn, gq], [qn])
                        cb = cos_t.unsqueeze(1).to_broadcast([128, 8, 32]); sbb = sin_t.unsqueeze(1).to_broadcast([128, 8, 32])
                        rope(qr, qn, cb, sbb, tq[:], [qn, cosb, sinb], [qr, tq], eng="pool")
                        yield 0.4
                        pb = PSa(); pbv = pb[:].bitcast(BF16)
                        for h in range(8):
                            TR(pbv[0:64, h * 128:(h + 1) * 128], qr[:, h, :], identb[:], [qr, identb], [pb])
                        CP("act", qT[:, :, tsl], pbv[0:64, 0:1024].rearrange("p (h n) -> p h n", h=8), [pb], [qT])
                        yield 0.4
                        TS("dve", kn[:, 0, :], kviraw[:, 0:64], s8[:, 8:9], None, ALU.mult, None, [kviraw, s8], [kn])
                        TT("pool", kn[:, 0, :], kn[:, 0, :], gk[:], ALU.mult, [kn, gk], [kn])
                        c1b = cos_t.unsqueeze(1); s1b = sin_t.unsqueeze(1)
                        rope(kr, kn, c1b, s1b, tq[:, 0:1, :], [kn, cosb, sinb], [kr, tq], eng="pool")
                        yield 0.4
                        pb = PSa(); pbv = pb[:].bitcast(BF16)
                        TR(pbv[0:64, 0:128], kr[:, 0, :], identb[:], [kr, identb], [pb])
                        CP("act", kT[:, gt_ * 128:(gt_ + 1) * 128], pbv[0:64, 0:128], [pb], [kT])
                        yield 0.4
                        CP("pool", vaug[:, gt_, 0:64], kviraw[:, 64:128], [kviraw], [vaug])
                        yield 0.4
                        qi3 = kviraw[:, 128:448].rearrange("p (h d) -> p h d", h=5)
                        cb5 = cos_t.unsqueeze(1).to_broadcast([128, 5, 32]); sb5 = sin_t.unsqueeze(1).to_broadcast([128, 5, 32])
                        rope(qir, qi3, cb5, sb5, tq[:, 0:5, :], [kviraw, cosb, sinb], [qir, tq], eng="pool")
                        yield 0.4
                        pb = PSa()
                        for h in range(4):
                            TR(pb[0:64, h * 128:(h + 1) * 128], qir[:, h, :], ident[:], [qir, ident], [pb])
                        CP("act", qiT[:, :, tsl], pb[0:64, :].rearrange("p (h n) -> p h n", h=4), [pb], [qiT])
                        yield 0.4
                        pb = PSa()
                        TR(pb[0:64, 0:128], qir[:, 4, :], ident[:], [qir, ident], [pb])
                        CP("act", kiT[:, gt_ * 128:(gt_ + 1) * 128], pb[0:64, 0:128], [pb], [kiT])
                        yield 0.4
                        TS("dve", wis[:, t, :], kviraw[:, 448:452], 0.5 * 0.125, None, ALU.mult, None, [kviraw], [wis])
                        yield 0.4
                        TT("pool", sq[:], qmraw[:], qmraw[:], ALU.mult, [qmraw], [sq])
                        op("dve", lambda E: E.tensor_reduce(out=s8[:, 0:4], in_=sq[:].rearrange("p (h d) -> p h d", h=4), axis=AX.X, op=ALU.add), [sq], [s8])
                        rstd_of(s8[:, 0:4], 128, [s8])
                        TT("dve", qmraw[:].rearrange("p (h d) -> p h d", h=4), qmraw[:].rearrange("p (h d) -> p h d", h=4),
                           s8[:, 0:4].unsqueeze(2).to_broadcast([128, 4, 128]), ALU.mult, [qmraw, s8], [qmraw])
                        TT("pool", qmn[:], qmraw[:].rearrange("p (h d) -> p h d", h=4), gqm[:].unsqueeze(1).to_broadcast([128, 4, 128]), ALU.mult, [qmraw, gqm], [qmn])
                        pb = PSa(); pbv = pb[:].bitcast(BF16)
                        for h in range(4):
                            TR(pbv[:, h * 128:(h + 1) * 128], qmn[:, h, :], identb[:], [qmn, identb], [pb])
                        CP("act", qmT[:, :, tsl], pbv[:, 0:512].rearrange("p (h n) -> p h n", h=4), [pb], [qmT])
                        yield 0.4
                        for _ in range(2 if t < 3 else len(my_ids)):
                            if c_next < len(my_ids):
                                c_out(c_next)
                                if c_next + 2 < len(my_ids):
                                    c_in(c_next + 2)
                                c_next += 1
                    fence(dtiles)

                estC = ExitStack()
                fences = []

                def genE(est):
                    pm = [sb("pm%d" % i, [128, 128], BF16, est) for i in range(2)]
                    ymem = sb("ymem", [128, 4, 128], BF16, est)
                    rden = sb("rden", [128, 4], F32, est)
                    pmi = 0
                    for t in range(4):
                        tsl = slice(t * 128, (t + 1) * 128)
                        for hp in range(2):
                            acc = pbank[ps_reserve("A")]
                            for h2 in range(2):
                                h = 2 * hp + h2
                                for mh in range(2):
                                    pb = PSa()
                                    MM(pb[:, 0:128], kmT[:, h, mh * 128:(mh + 1) * 128], qmT[:, h, tsl], True, True, [kmT, qmT], [pb])
                                    p_ = pm[pmi % 2]; pmi += 1
                                    ACT(p_[:], pb[:, 0:128], AF.Exp, [pb], [p_], scale=128 ** -0.5)
                                    yield 0.4
                                    MM(acc[:, h2 * 129:h2 * 129 + 129], p_[:], vmaug[:, mh, h, :], mh == 0, mh == 1, [p_, vmaug], [acc])
                            a3 = acc[:, 0:258].rearrange("p (h d) -> p h d", h=2)
                            op("dve", lambda E: E.reciprocal(out=rden[:, 2 * hp:2 * hp + 2], in_=a3[:, :, 128]), [acc], [rden])
                            TT("dve", ymem[:, 2 * hp:2 * hp + 2, :], a3[:, :, 0:128], rden[:, 2 * hp:2 * hp + 2].unsqueeze(2).to_broadcast([128, 2, 128]),
                               ALU.mult, [acc, rden], [ymem])
                            ps_release("A", pbank.index(acc))
                            yield 0.4
                        pb = PSa(); pbv = pb[:].bitcast(BF16)
                        for h in range(4):
                            TR(pbv[:, h * 128:(h + 1) * 128], ymem[:, h, :], identb[:], [ymem, identb], [pb])
                        CP("act", ymemT[:, :, tsl], pbv[:, 0:512].rearrange("p (h n) -> p h n", h=4), [pb], [ymemT])
                        yield 0.4
                    fences.append(pm + [ymem, rden])

                def genF(est, tiles):
                    isc = sb("isc", [128, SEQ], F32, est)
                    mneg = sb("mneg", [128, SEQ], BF16, est)
                    rl = [sb("rl%d" % i, [128, 512], F32, est) for i in range(2)]
                    PT = [sb("PT%d" % i, [128, 1024], BF16, est) for i in range(2)]
                    m8 = sb("m8", [128, 8], F32, est)
                    ydsa = sb("ydsa", [128, 8, 64], BF16, est)
                    rden8 = sb("rden8", [128, 8], F32, est)
                    oacc = sb("oacc", [128, 2, 260], F32, est)
                    rli = 0; pti = 0
                    for t in tiles:
                        gt_ = st * 4 + t
                        nk = (gt_ + 1) * 128
                        tsl = slice(t * 128, (t + 1) * 128)
                        if gt_ >= 2:
                            for c0 in range(0, nk, 512):
                                cw = min(512, nk - c0)
                                for h in range(4):
                                    pb = PSa()
                                    MM(pb[:, 0:cw], qiT[:, h, tsl], kiT[:, c0:c0 + cw], True, True, [qiT, kiT], [pb])
                                    r_ = rl[rli % 2]; rli += 1
                                    ACT(r_[:, 0:cw], pb[:, 0:cw], AF.Relu, [pb], [r_])
                                    yield 0.4
                                    if h == 0:
                                        TS("dve", isc[:, c0:c0 + cw], r_[:, 0:cw], wis[:, t, 0:1], None, ALU.mult, None, [r_, wis], [isc])
                                    else:
                                        STT(isc[:, c0:c0 + cw], r_[:, 0:cw], wis[:, t, h:h + 1], isc[:, c0:c0 + cw], ALU.mult, ALU.add, [r_, wis, isc], [isc])
                                yield 0.4
                            TT("dve", isc[:, nk - 128:nk], isc[:, nk - 128:nk], causal[:], ALU.add, [isc, causal], [isc])
                            for r in range(32):
                                op("dve", lambda E: E.max(out=m8[:], in_=isc[:, 0:nk]), [isc], [m8])
                                yield nk / 960.0
                                op("dve", lambda E: E.match_replace(out=isc[:, 0:nk], in_to_replace=m8[:], in_values=isc[:, 0:nk], imm_value=-3.0e38), [isc, m8], [isc])
                                yield nk / 960.0
                            TS("dve", mneg[:, 0:nk], isc[:, 0:nk], -1.0e35, -30000.0, ALU.is_gt, ALU.mult, [isc], [mneg])
                            yield 0.4
                        for j in range(gt_ + 1):
                            ksl = slice(j * 128, (j + 1) * 128)
                            need_mask = (gt_ >= 2) or (j == gt_)
                            p_ = PT[pti % 2]; pti += 1
                            for half in range(2):
                                pb = PSa()
                                MM(pb[:], kT[:, ksl], qT[:, 4 * half:4 * half + 4, tsl], True, not need_mask, [kT, qT], [pb])
                                if need_mask:
                                    ml = mneg[:, ksl] if gt_ >= 2 else causalb[:]
                                    MM(pb[:], ml, irep[:], False, True, [mneg, causalb, irep], [pb])
                                ACT(p_[:, half * 512:(half + 1) * 512], pb[:], AF.Exp, [pb], [p_], scale=0.125)
                                yield 0.4
                            for hp in range(2):
                                acc = PSa()
                                for h4 in range(4):
                                    h = 4 * hp + h4
                                    MM(acc[:, h4 * 65:h4 * 65 + 65], p_[:, h * 128:(h + 1) * 128], vaug[:, j, :], True, True, [p_, vaug], [acc])
                                if j == 0:
                                    CP("act", oacc[:, hp, :], acc[:, 0:260], [acc], [oacc])
                                    yield 0.4
                                else:
                                    TT("dve", oacc[:, hp, :], oacc[:, hp, :], acc[:, 0:260], ALU.add, [acc, oacc], [oacc])
                            yield 0.4
                        a3 = oacc[:].rearrange("p a (h d) -> p (a h) d", h=4)
                        op("dve", lambda E: E.reciprocal(out=rden8[:], in_=a3[:, :, 64]), [oacc], [rden8])
                        TT("dve", ydsa[:], a3[:, :, 0:64], rden8[:].unsqueeze(2).to_broadcast([128, 8, 64]), ALU.mult, [oacc, rden8], [ydsa])
                        pb = PSa(); pbv = pb[:].bitcast(BF16)
                        for c in range(4):
                            TR(pbv[:, c * 128:(c + 1) * 128], ydsa[:, 2 * c:2 * c + 2, :].rearrange("p h d -> p (h d)"), identb[:], [ydsa, identb], [pb])
                        CP("act", ydsaT[:, :, tsl], pbv[:, 0:512].rearrange("p (c n) -> p c n", c=4), [pb], [ydsaT])
                        yield 0.4
                    fences.append([isc, mneg, m8, ydsa, rden8, oacc] + rl + PT)

                yg = sb("yg", [128, 4, 512], BF16, estC)
                sz = sb("sz", [128, 512], BF16, estC)

                def gluG():
                    sl, w, hh = ringA.get(C_GLU, 4, 512)
                    for fo in range(4):
                        pb = PSa()
                        for fc in range(4):
                            MM(pb[:], w[:, fc, fo * 128:(fo + 1) * 128], yg[:, fc, :], fc == 0, fc == 3, [sl, yg], [pb])
                        ACT(sz[:], pb[:], AF.Sigmoid, [pb], [sz])
                        yield 0.4
                        TT("dve", yssmT[:, fo, :], yg[:, fo, :], sz[:], ALU.mult, [yg, sz], [yssmT])
                        yield 0.4
                    ringA.rel(hh)

                def genG(est, fcs, do_glu):
                    sets = []
                    t1_ = sb("t1", [128, 4, 128], F32, est); t2_ = sb("t2", [128, 4, 128], F32, est)
                    t3_ = sb("t3", [128, 4, 128], F32, est); t4_ = sb("t4", [128, 4, 128], F32, est)
                    for i_ in range(1):
                        sets.append(dict(
                            wre=sb("wre", [128, 4, 128], F32, est), wim=sb("wim", [128, 4, 128], F32, est),
                            t1=t1_, t2=t2_, t3=t3_, t4=t4_,
                            wsr=sb("wsr", [128, 4, 128], F32, est), wsi=sb("wsi", [128, 4, 128], F32, est),
                            sre=sb("sre", [128, 4, 128], BF16, est), sim=sb("sim", [128, 4, 128], BF16, est),
                            ini=sb("ini", [128, 2], F32, est), tiny=sb("tiny", [128, 2], F32, est)))
                    ypre = sb("ypre", [128, 512], F32, est)
                    for fc in fcs:
                        ypb = pbank[ps_reserve("A")]
                        for r in range(4):
                            pc = 4 * fc + r
                            S_ = sets[pc % len(sets)]
                            wre, wim, t1, t2, t3, t4 = S_["wre"], S_["wim"], S_["t1"], S_["t2"], S_["t3"], S_["t4"]
                            wsr, wsi, sre, sim, ini, tiny = S_["wsr"], S_["wsi"], S_["sre"], S_["sim"], S_["ini"], S_["tiny"]
                            psl = slice(32 * r, 32 * r + 32) if r < 3 else slice(64, 128)
                            osl = slice(64 * (r // 2), 64 * (r // 2) + 64)
                            pre = PSa(); pim = PSa()
                            MM(pre[:], BTre[psl, pc, :], uT[psl, fc, :], True, True, [BTre, uT], [pre])
                            MM(pim[:], BTim[psl, pc, :], uT[psl, fc, :], True, True, [BTim, uT], [pim])
                            Cb = tabc[:, pc:pc + 1, :].to_broadcast([128, 4, 128]); Sb = tabs[:, pc:pc + 1, :].to_broadcast([128, 4, 128])
                            pre3 = pre[:].rearrange("p (k n) -> p k n", k=4); pim3 = pim[:].rearrange("p (k n) -> p k n", k=4)
                            TT("dve", t1[:], pre3, Cb, ALU.mult, [pre, tabc], [t1])
                            TT("dve", t2[:], pim3, Sb, ALU.mult, [pim, tabs], [t2])
                            TT("dve", wre[:], t1[:], t2[:], ALU.add, [t1, t2], [wre])
                            TT("dve", t1[:], pim3, Cb, ALU.mult, [pim, tabc], [t1])
                            TT("dve", t2[:], pre3, Sb, ALU.mult, [pre, tabs], [t2])
                            TT("dve", wim[:], t1[:], t2[:], ALU.subtract, [t1, t2], [wim])
                            yield 3.0
                            magb = s5_mag[:, pc:pc + 1].to_broadcast([128, 128])
                            for k in range(4):
                                if k == 0:
                                    i_re = carr[:, pc, 0:1]; i_im = carr[:, pc, 1:2]; ib = carr
                                else:
                                    i_re = ini[:, 0:1]; i_im = ini[:, 1:2]; ib = ini
                                op("dve", lambda E: E.tensor_tensor_scan(out=wsr[:, k, :], data0=magb, data1=wre[:, k, :], initial=i_re, op0=ALU.mult, op1=ALU.add), [s5_mag, wre, ib], [wsr])
                                op("dve", lambda E: E.tensor_tensor_scan(out=wsi[:, k, :], data0=magb, data1=wim[:, k, :], initial=i_im, op0=ALU.mult, op1=ALU.add), [s5_mag, wim, ib], [wsi])
                                dst = ini if k < 3 else carr
                                d_re = ini[:, 0:1] if k < 3 else carr[:, pc, 0:1]
                                d_im = ini[:, 1:2] if k < 3 else carr[:, pc, 1:2]
                                TS("dve", tiny[:, 0:1], wsr[:, k, 127:128], s5_c128[:, pc:pc + 1], None, ALU.mult, None, [wsr, s5_c128], [tiny])
                                TS("dve", tiny[:, 1:2], wsi[:, k, 127:128], s5_c128[:, pc:pc + 1], None, ALU.mult, None, [wsi, s5_c128], [tiny])
                                STT(d_re, wsi[:, k, 127:128], s5_s128n[:, pc:pc + 1], tiny[:, 0:1], ALU.mult, ALU.add, [wsi, s5_s128n, tiny], [dst])
                                STT(d_im, wsr[:, k, 127:128], s5_s128[:, pc:pc + 1], tiny[:, 1:2], ALU.mult, ALU.add, [wsr, s5_s128, tiny], [dst])
                                yield 1.5
                            TT("pool", t3[:], wsr[:], Cb, ALU.mult, [wsr, tabc], [t3])
                            TT("pool", t4[:], wsi[:], Sb, ALU.mult, [wsi, tabs], [t4])
                            TT("pool", sre[:], t3[:], t4[:], ALU.subtract, [t3, t4], [sre])
                            TT("pool", t3[:], wsi[:], Cb, ALU.mult, [wsi, tabc], [t3])
                            TT("pool", t4[:], wsr[:], Sb, ALU.mult, [wsr, tabs], [t4])
                            TT("pool", sim[:], t3[:], t4[:], ALU.add, [t3, t4], [sim])
                            yield 0.4
                            MM(ypb[osl, :], Cwre[:, pc, :], sre[:].rearrange("p k n -> p (k n)"), r % 2 == 0, False, [Cwre, sre], [ypb])
                            MM(ypb[osl, :], Cwim[:, pc, :], sim[:].rearrange("p k n -> p (k n)"), False, r % 2 == 1, [Cwim, sim], [ypb])
                            yield 5.0
                        STT(ypre[:], uT[:, fc, :], dsk[:, fc:fc + 1], ypb[:], ALU.mult, ALU.add, [uT, dsk, ypb], [ypre])
                        ACT(yg[:, fc, :], ypre[:], AF.Gelu_apprx_tanh, [ypre], [yg])
                        yield 0.4
                        ps_release("A", pbank.index(ypb))
                    if do_glu:
                        yield from gluG()
                    fences.append([x_ for S2 in sets for x_ in S2.values()] + [ypre])

                fences.append([yg, sz])
                if st == 0:
                    gens = [genG(estC, (0, 2), False), genG(estC, (1, 3), False), genF(estC, (0, 1, 2, 3)), genE(estC)]
                else:
                    gens = [genG(estC, (0, 1, 2, 3), True), genF(estC, (0, 2)), genF(estC, (1, 3)), genE(estC)]
                acc_t = [0.0] * len(gens)
                live = [True] * len(gens)
                while any(live):
                    gi = min((k_ for k_ in range(len(gens)) if live[k_]), key=lambda k_: acc_t[k_])
                    try:
                        w_ = next(gens[gi])
                        acc_t[gi] += (w_ if w_ is not None else 0.4)
                    except StopIteration:
                        live[gi] = False
                if st == 0:
                    for _ in gluG():
                        pass
                for f_ in fences:
                    fence(f_)
                estC.close()
                fence([qT, qiT, qmT, wis])
                e2b.close()
                fence([uT])
                e2a.close()
                yield 0.4

            def stage_H(b, st, A):
                tok0 = st * 512
                hT, ydsaT, ymemT, yssmT = A["hT"], A["ydsaT"], A["ymemT"], A["yssmT"]
                PSa = lambda: PSs("A")
                esx = ExitStack()
                Xs = sb("Xs", [128, 4, D], F32, esx)
                with ExitStack() as est:
                    gsb = sb("gsb", [128, 4, 1024], BF16, est)
                    mg = sb("mg", [128, 4, D], F32, est)
                    mgb = sb("mgb", [128, 4, D], BF16, est)
                    mT = sb("mT", [128, 4, 8, 128], BF16, est)
                    fw.dma(Xs[:], x_d[b, tok0:tok0 + 512, :].rearrange("(t p) d -> p t d", p=128), writes=XsAll, q=QA)
                    for bi, (cid, yT_) in enumerate(((C_UPS, yssmT), (C_UPD, ydsaT), (C_UPM, ymemT))):
                        for i2 in range(2):
                            sl, w, hh = ringA.get(C_G0 + 2 * bi + i2, 8, 512)
                            for t in range(4):
                                tsl = slice(t * 128, (t + 1) * 128)
                                pb = PSa()
                                for kc in range(8):
                                    MM(pb[:], hT[:, kc, tsl], w[:, kc, :], kc == 0, kc == 7, [hT, sl], [pb])
                                ACT(gsb[:, t, i2 * 512:(i2 + 1) * 512], pb[:], AF.Sigmoid, [pb], [gsb])
                            ringA.rel(hh)
                        sl, w, hh = ringA.get(cid, 4, 1024)
                        for t in range(4):
                            tsl = slice(t * 128, (t + 1) * 128)
                            for half in range(2):
                                pb = PSa()
                                for fc in range(4):
                                    MM(pb[:], yT_[:, fc, tsl], w[:, fc, half * 512:(half + 1) * 512], fc == 0, fc == 3, [yT_, sl], [pb])
                                gsl = gsb[:, t, half * 512:(half + 1) * 512]
                                msl = mg[:, t, half * 512:(half + 1) * 512]
                                if bi == 0:
                                    TT("dve", msl, pb[:], gsl, ALU.mult, [pb, gsb], [mg])
                                else:
                                    TT("dve", pb[:], pb[:], gsl, ALU.mult, [pb, gsb], [pb])
                                    if bi == 1:
                                        TT("dve", msl, msl, pb[:], ALU.add, [pb, mg], [mg])
                                    else:
                                        TT("dve", mgb[:, t, half * 512:(half + 1) * 512], msl, pb[:], ALU.add, [pb, mg], [mgb])
                        ringA.rel(hh)
                    for t in range(4):
                        pb = PSa(); pbv = pb[:].bitcast(BF16)
                        for kc in range(8):
                            TR(pbv[:, kc * 128:(kc + 1) * 128], mgb[:, t, kc * 128:(kc + 1) * 128], identb[:], [mgb, identb], [pb])
                        CP("act", mT[:, t, :, :], pbv[:, 0:1024].rearrange("p (k n) -> p k n", k=8), [pb], [mT])
                    for half, cid in enumerate((C_OUT0, C_OUT1)):
                        sl, w, hh = ringA.get(cid, 8, 512)
                        for t in range(4):
                            pb = PSa()
                            for kc in range(8):
                                MM(pb[:], mT[:, t, kc, :], w[:, kc, :], kc == 0, kc == 7, [mT, sl], [pb])
                            xs_ = Xs[:, t, half * 512:(half + 1) * 512]
                            TT("dve", xs_, xs_, pb[:], ALU.add, [XsB[t][half], pb], [XsB[t][half]])
                        ringA.rel(hh)
                    fw.barrier()
                with ExitStack() as est:
                    hn = sb("hn", [128, D], F32, est); hnb = sb("hnb", [128, D], BF16, est)
                    hnb2 = [sb("hnb2_%d" % i_, [128, D], BF16, est) for i_ in range(2)]
                    ohs = sb("ohs", [128, 32], F32, est); rk = sb("rk", [128, 32], F32, est); rkm = sb("rkm", [128, 32], F32, est)
                    hnT32 = sb("hnT32", [128, 8, 128], F32, est)
                    rst2 = sb("rst2", [128, 4], F32, est)
                    lg = sb("lg", [128, 36], F32, est)
                    r4 = sb("r4", [128, 16], F32, est)
                    ohg = sb("ohg", [128, 4], F32, est)
                    le = sb("le", [128, 4, 8], F32, est); el = sb("el", [128, 8], F32, est); ee = sb("ee", [128, 8], F32, est)
                    oh1 = sb("oh1", [128, 8], F32, est); oh2 = sb("oh2", [128, 8], F32, est); e2 = sb("e2", [128, 8], F32, est)
                    for t in range(4):
                        ACT(hnb[:], Xs[:, t, :], AF.Square, XsB[t], [hnb, rst2], accum=rst2[:, t:t + 1])
                    rstd_of(rst2[:], D, [rst2])
                    for t in range(4):
                        tsl = slice(t * 128, (t + 1) * 128)
                        TS("dve", hn[:], Xs[:, t, :], rst2[:, t:t + 1], None, ALU.mult, None, XsB[t] + [rst2], [hn])
                        tt = (b * NST + st) * 4 + t
                        hb_ = hnb2[t % 2]
                        CP("pool", hb_[:], hn[:], [hn], [hb_])
                        fw.dma(HN[tt * 128:(tt + 1) * 128, :], hb_[:], reads=[hb_], writes=[HNb])
                        for q4 in range(2):
                            pb = PSa()
                            for c in range(4):
                                kc = 4 * q4 + c
                                TR(pb[:, c * 128:(c + 1) * 128], hn[:, kc * 128:(kc + 1) * 128], ident[:], [hn, ident], [pb])
                            TT("dve", hnT32[:, 4 * q4:4 * q4 + 4, :], pb[:].rearrange("p (k n) -> p k n", k=4),
                               gffnT[:, 4 * q4:4 * q4 + 4].unsqueeze(2).to_broadcast([128, 4, 128]), ALU.mult, [pb, gffnT], [hnT32])
                        pb = PSa()
                        for kc in range(8):
                            MM(pb[:, 0:36], hnT32[:, kc, :], wr32[:, kc, :], kc == 0, kc == 7, [hnT32, wr32], [pb])
                        TT("dve", lg[:], pb[:, 0:36], rbias[:], ALU.add, [pb, rbias], [lg])
                        op("dve", lambda E: E.tensor_reduce(out=r4[:, 0:1], in_=lg[:, 0:4], axis=AX.X, op=ALU.max), [lg], [r4])
                        TS("dve", ohg[:], lg[:, 0:4], r4[:, 0:1], None, ALU.is_equal, None, [lg, r4], [ohg])
                        TS("dve", r4[:, 1:2], r4[:, 0:1], -1.0, None, ALU.mult, None, [r4], [r4])
                        ACT(r4[:, 4:8], lg[:, 0:4], AF.Exp, [lg, r4], [r4], bias=r4[:, 1:2])
                        op("dve", lambda E: E.tensor_reduce(out=r4[:, 2:3], in_=r4[:, 4:8], axis=AX.X, op=ALU.add), [r4], [r4])
                        op("dve", lambda E: E.reciprocal(out=r4[:, 2:3], in_=r4[:, 2:3]), [r4], [r4])
                        TT("dve", le[:], lg[:, 4:36].rearrange("p (g e) -> p g e", g=4), ohg[:].unsqueeze(2).to_broadcast([128, 4, 8]), ALU.mult, [lg, ohg], [le])
                        op("dve", lambda E: E.tensor_reduce(out=el[:], in_=le[:].rearrange("p g e -> p e g"), axis=AX.X, op=ALU.add), [le], [el])
                        op("dve", lambda E: E.tensor_reduce(out=r4[:, 3:4], in_=el[:], axis=AX.X, op=ALU.max), [el], [r4])
                        TS("dve", r4[:, 8:9], r4[:, 3:4], -1.0, None, ALU.mult, None, [r4], [r4])
                        ACT(ee[:], el[:], AF.Exp, [el, r4], [ee], bias=r4[:, 8:9])
                        op("dve", lambda E: E.tensor_reduce(out=r4[:, 9:10], in_=ee[:], axis=AX.X, op=ALU.max), [ee], [r4])
                        TS("dve", oh1[:], ee[:], r4[:, 9:10], None, ALU.is_equal, None, [ee, r4], [oh1])
                        STT(e2[:], oh1[:], -4.0, ee[:], ALU.mult, ALU.add, [oh1, ee], [e2])
                        op("dve", lambda E: E.tensor_reduce(out=r4[:, 10:11], in_=e2[:], axis=AX.X, op=ALU.max), [e2], [r4])
                        TS("dve", oh2[:], e2[:], r4[:, 10:11], None, ALU.is_equal, None, [e2, r4], [oh2])
                        TT("dve", r4[:, 11:12], r4[:, 9:10], r4[:, 10:11], ALU.add, [r4], [r4])
                        op("dve", lambda E: E.reciprocal(out=r4[:, 11:12], in_=r4[:, 11:12]), [r4], [r4])
                        TT("dve", r4[:, 11:12], r4[:, 11:12], r4[:, 2:3], ALU.mult, [r4], [r4])
                        TT("dve", r4[:, 12:13], r4[:, 9:10], r4[:, 11:12], ALU.mult, [r4], [r4])
                        TT("dve", r4[:, 13:14], r4[:, 10:11], r4[:, 11:12], ALU.mult, [r4], [r4])
                        CP("dve", W12[:, tt, :], r4[:, 12:14], [r4], [W12])
                        TT("dve", OH1[:, tt, :].rearrange("p (g e) -> p g e", g=4), oh1[:].unsqueeze(1).to_broadcast([128, 4, 8]),
                           ohg[:].unsqueeze(2).to_broadcast([128, 4, 8]), ALU.mult, [oh1, ohg], [OH1])
                        TT("dve", OH2[:, tt, :].rearrange("p (g e) -> p g e", g=4), oh2[:].unsqueeze(1).to_broadcast([128, 4, 8]),
                           ohg[:].unsqueeze(2).to_broadcast([128, 4, 8]), ALU.mult, [oh2, ohg], [OH2])
                        TT("dve", ohs[:], OH1[:, tt, :], OH2[:, tt, :], ALU.add, [OH1, OH2], [ohs])
                        pb = PSa()
                        MM(pb[:, 0:32], ltri[:], ohs[:], True, True, [ltri, ohs], [pb])
                        MM(pb[:, 32:64], ones[:], ohs[:], True, True, [ones, ohs], [pb])
                        TT("dve", rk[:], pb[:, 0:32], run[:], ALU.add, [pb, run], [rk])
                        TT("dve", run[:], run[:], pb[:, 32:64], ALU.add, [pb, run], [run])
                        TT("dve", rkm[:], rk[:], OH1[:, tt, :], ALU.mult, [rk, OH1], [rkm])
                        op("dve", lambda E: E.tensor_reduce(out=RNK[:, tt, 0:1], in_=rkm[:], axis=AX.X, op=ALU.add), [rkm], [RNK])
                        TT("dve", rkm[:], rk[:], OH2[:, tt, :], ALU.mult, [rk, OH2], [rkm])
                        op("dve", lambda E: E.tensor_reduce(out=RNK[:, tt, 1:2], in_=rkm[:], axis=AX.X, op=ALU.add), [rkm], [RNK])
                    g0 = (b * NST + st) * 512
                    fw.dma(XM[g0:g0 + 512, :].rearrange("(t p) d -> p t d", p=128), Xs[:], reads=XsAll, writes=[XMb])
                    fw.barrier()
                esx.close()

            def drive(ga):
                for _ in ga:
                    pass

            for b in range(NB):
                batch_prep(b)
                for st in range(NST):
                    with ExitStack() as esa:
                        A = {
                            "hT": sb("hT", [128, 8, 512], BF16, esa),
                            "ydsaT": sb("ydsaT", [128, 4, 512], BF16, esa), "ymemT": sb("ymemT", [128, 4, 512], BF16, esa),
                            "yssmT": sb("yssmT", [128, 4, 512], BF16, esa), "rst": sb("rst", [128, 4], F32, esa),
                        }
                        drive(attn_AG(b, st, A, esa))
                        fw.barrier()
                        stage_H(b, st, A)
                    fw.barrier()
            fw.barrier()
            esP1.close()

            PS8 = lambda: PSs("A")
            with ExitStack() as e15:
                cmp = sb("cmp", [128, 32, 64], F32, e15)
                ltc = sb("ltc", [128, 1024], F32, e15)
                fw.dma(ltc[:], ltc_d.partition_broadcast(128), writes=[ltc])
                kt = sb("kt", [128, 32], F32, e15); tsv = sb("tsv", [128, 32], F32, e15); te = sb("te", [128, 32], F32, e15)
                ts128 = sb("ts128", [128, 32], F32, e15)
                cm2 = sb("cm2", [128, 32, 32], F32, e15)
                msk = sb("msk", [128, NTILE, 32], F32, e15)
                texp = sb("texp", [128, NTILE], F32, e15); wf = sb("wf", [128, NTILE, 3], F32, e15)
                big = sb("big", [128, NTT, 32], F32, e15); dpf = sb("dpf", [128, NTT], F32, e15)
                TT("dve", cmp[:], run[:].unsqueeze(2).to_broadcast([128, 32, 64]), thr[:].unsqueeze(1).to_broadcast([128, 32, 64]), ALU.is_gt, [run, thr], [cmp])
                op("dve", lambda E: E.tensor_reduce(out=kt[:], in_=cmp[:], axis=AX.X, op=ALU.add), [cmp], [kt])
                TT("dve", cm2[:], ltc[:].rearrange("p (a c) -> p a c", a=32), kt[:].unsqueeze(1).to_broadcast([128, 32, 32]), ALU.mult, [ltc, kt], [cm2])
                op("dve", lambda E: E.tensor_reduce(out=tsv[:], in_=cm2[:], axis=AX.X, op=ALU.add), [cm2], [tsv])
                TT("dve", te[:], tsv[:], kt[:], ALU.add, [tsv, kt], [te])
                TS("dve", ts128[:], tsv[:], 128.0, None, ALU.mult, None, [tsv], [ts128])
                TT("dve", msk[:], te[:].unsqueeze(1).to_broadcast([128, NTILE, 32]), iot[:].unsqueeze(2).to_broadcast([128, NTILE, 32]), ALU.is_le, [te, iot], [msk])
                op("dve", lambda E: E.tensor_reduce(out=texp[:], in_=msk[:], axis=AX.X, op=ALU.add), [msk], [texp])
                TS("dve", texp[:], texp[:], 31.0, None, ALU.min, None, [texp], [texp])
                for j in range(3):
                    TS("dve", wf[:, :, j], texp[:], 384.0, float((C_EXP + j) * 128), ALU.mult, ALU.add, [texp], [wf])
                TS("dve", wf[:], wf[:], pcol[:, 0:1], None, ALU.add, None, [wf, pcol], [wf])
                CP("dve", widx[:], wf[:], [wf], [widx])
                for c, OH in enumerate((OH1, OH2)):
                    TT("dve", big[:], OH[:], ts128[:].unsqueeze(1).to_broadcast([128, NTT, 32]), ALU.mult, [OH, ts128], [big])
                    op("dve", lambda E: E.tensor_reduce(out=dpf[:], in_=big[:], axis=AX.X, op=ALU.add), [big], [dpf])
                    TT("dve", dpf[:], dpf[:], RNK[:, :, c], ALU.add, [dpf, RNK], [dpf])
                    CP("dve", dpi[c][:], dpf[:], [dpf], [dpi[c]])
                hnl = [sb("hnl%d" % i_, [128, D], BF16, e15) for i_ in range(4)]
                for tt in range(NTT):
                    h_ = hnl[tt % 4]
                    fw.dma(h_[:], HN[tt * 128:(tt + 1) * 128, :], reads=[HNb], writes=[h_])
                    for c in range(2):
                        fw.idma(HS, bass.IndirectOffsetOnAxis(ap=dpi[c][:, tt:tt + 1], axis=0), h_[:], None, NTILE * 128 - 1,
                                reads=[h_, dpi[c]], writes=[HSb])
                fw.barrier()
            fw.barrier()

            wscf = wsc.rearrange("c p n -> (c p) n")
            with ExitStack() as e2:
                wgu = [[ring[0], ring[1]]] + [[sb("wgu%d_%d" % (i_, j_), [128, 4096], BF16, e2) for j_ in range(2)] for i_ in range(2)]
                wdn = [sb("wdn%d" % i_, [128, 4096], BF16, e2) for i_ in range(3)]
                hsl = [sb("hsl%d" % i_, [128, D], BF16, e2) for i_ in range(3)]
                hsT = [sb("hsT%d" % i_, [128, 8, 128], BF16, e2) for i_ in range(2)]
                sg2 = [sb("sg2_%d" % i_, [128, 512], F32, e2) for i_ in range(2)]
                hidb = [sb("hidb%d" % i_, [128, 512], BF16, e2) for i_ in range(2)]
                hidT = [sb("hidT%d" % i_, [128, 4, 128], BF16, e2) for i_ in range(2)]
                ysb = [sb("ysb%d" % i_, [128, D], F32, e2) for i_ in range(3)]

                def load_gu(i):
                    for j in range(2):
                        sl = wgu[i % 3][j]
                        fw.idma(sl[:], None, wscf, bass.IndirectOffsetOnAxis(ap=widx[:, i, j:j + 1], axis=0), NCH * 128 - 1, reads=[widx], writes=[sl])

                def load_dn(i):
                    sl = wdn[i % 3]
                    fw.idma(sl[:], None, wscf, bass.IndirectOffsetOnAxis(ap=widx[:, i, 2:3], axis=0), NCH * 128 - 1, reads=[widx], writes=[sl])

                def load_hs(i):
                    fw.dma(hsl[i % 3][:], HS[i * 128:(i + 1) * 128, :], reads=[HSb], writes=[hsl[i % 3]])

                def S1(i):
                    h_ = hsl[i % 3]; hT_ = hsT[i % 2]
                    pb = PS8(); pbv = pb[:].bitcast(BF16)
                    for kc in range(8):
                        TR(pbv[:, kc * 128:(kc + 1) * 128], h_[:, kc * 128:(kc + 1) * 128], identb[:], [h_, identb], [pb])
                    TT("dve", hT_[:], pbv[:, 0:1024].rearrange("p (k n) -> p k n", k=8),
                       gffnT[:].unsqueeze(2).to_broadcast([128, 8, 128]), ALU.mult, [pb, gffnT], [hT_])

                def S2(i):
                    hT_ = hsT[i % 2]
                    slg, slu = wgu[i % 3]
                    wg = slg[:].rearrange("p (k n) -> p k n", k=8); wu = slu[:].rearrange("p (k n) -> p k n", k=8)
                    pg = PS8(); pu = PS8()
                    for kc in range(8):
                        MM(pg[:], hT_[:, kc, :], wg[:, kc, :], kc == 0, kc == 7, [hT_, slg], [pg])
                    for kc in range(8):
                        MM(pu[:], hT_[:, kc, :], wu[:, kc, :], kc == 0, kc == 7, [hT_, slu], [pu])
                    ACT(sg2[i % 2][:], pg[:], AF.Silu, [pg], [sg2[i % 2]])
                    TT("dve", hidb[i % 2][:], pu[:], sg2[i % 2][:], ALU.mult, [pu, sg2[i % 2]], [hidb[i % 2]])

                def S3(i):
                    pb = PS8(); pbv = pb[:].bitcast(BF16)
                    for fc in range(4):
                        TR(pbv[:, fc * 128:(fc + 1) * 128], hidb[i % 2][:, fc * 128:(fc + 1) * 128], identb[:], [hidb[i % 2], identb], [pb])
                    CP("act", hidT[i % 2][:], pbv[:, 0:512].rearrange("p (k n) -> p k n", k=4), [pb], [hidT[i % 2]])

                def S4(i):
                    sld = wdn[i % 3]
                    wd = sld[:].rearrange("p (k n) -> p k n", k=4)
                    y_ = ysb[i % 3]
                    for half in range(2):
                        po = PS8()
                        for fc in range(4):
                            MM(po[:], hidT[i % 2][:, fc, :], wd[:, fc, half * 512:(half + 1) * 512], fc == 0, fc == 3, [hidT[i % 2], sld], [po])
                        CP("act" if half == 0 else "dve", y_[:, half * 512:(half + 1) * 512], po[:], [po], [y_])
                    fw.dma(YS[i * 128:(i + 1) * 128, :], y_[:], reads=[y_], writes=[YSb])

                for i in range(min(3, NTILE)):
                    load_hs(i); load_gu(i); load_dn(i)
                S1(0)
                if NTILE > 3:
                    load_hs(3)
                for i in range(NTILE + 1):
                    if i < NTILE:
                        S2(i)
                        if i + 3 < NTILE:
                            load_gu(i + 3)
                    if i + 1 < NTILE:
                        S1(i + 1)
                        if i + 4 < NTILE:
                            load_hs(i + 4)
                    if i >= 1:
                        S4(i - 1)
                        if i + 2 < NTILE:
                            load_dn(i + 2)
                    if i < NTILE:
                        S3(i)
                fw.barrier()
            fw.barrier()

            out_f = out_d.rearrange("b s d -> (b s) d")
            with ExitStack() as e3:
                xm = [sb("xm%d" % i_, [128, D], F32, e3) for i_ in range(3)]
                y1 = [sb("y1_%d" % i_, [128, D], F32, e3) for i_ in range(3)]
                y2 = [sb("y2_%d" % i_, [128, D], F32, e3) for i_ in range(3)]
                for tt in range(NTT):
                    k_ = tt % 3
                    fw.dma(xm[k_][:], XM[tt * 128:(tt + 1) * 128, :], reads=[XMb], writes=[xm[k_]])
                    fw.idma(y1[k_][:], None, YS, bass.IndirectOffsetOnAxis(ap=dpi[0][:, tt:tt + 1], axis=0), NTILE * 128 - 1, reads=[YSb, dpi[0]], writes=[y1[k_]])
                    fw.idma(y2[k_][:], None, YS, bass.IndirectOffsetOnAxis(ap=dpi[1][:, tt:tt + 1], axis=0), NTILE * 128 - 1, reads=[YSb, dpi[1]], writes=[y2[k_]])
                    STT(xm[k_][:], y1[k_][:], W12[:, tt, 0:1], xm[k_][:], ALU.mult, ALU.add, [y1[k_], W12, xm[k_]], [xm[k_]])
                    STT(xm[k_][:], y2[k_][:], W12[:, tt, 1:2], xm[k_][:], ALU.mult, ALU.add, [y2[k_], W12, xm[k_]], [xm[k_]])
                    fw.dma(out_f[tt * 128:(tt + 1) * 128, :], xm[k_][:], reads=[xm[k_]], q="act")
                fw.barrier()
        except _Stop:
            pass
        fw.barrier()
        print("[build] instrs=%d waits=%d" % (fw.ninstr, fw.nwaits))
    return nc, dbg_t


def _consts():
    ident = np.eye(128, dtype=np.float32)
    q = np.arange(128)[:, None]; k = np.arange(128)[None, :]
    causal = np.where(k <= q, 0.0, -1.0e30).astype(np.float32)
    jj = np.broadcast_to(np.arange(1, 129, dtype=np.float32)[None, :], (128, 128)).copy()
    invf = (1.0 / (np.float32(10000.0) ** (np.arange(0, 64, 2, dtype=np.float32) / np.float32(64.0)))).astype(np.float32)[None, :]
    par = np.zeros((128, 2), np.float32)
    g8 = np.arange(128) // 16
    par[:, 0] = (g8 % 2 == 0); par[:, 1] = (g8 % 2 == 1)
    ltri = (np.arange(128)[:, None] < np.arange(128)[None, :]).astype(np.float32)
    thr = (128.0 * np.arange(64, dtype=np.float32))[None, :]
    lt = (np.arange(32)[None, :] < np.arange(32)[:, None]).astype(np.float32).reshape(1, 1024)
    iot = np.arange(NTILE, dtype=np.float32)[None, :]
    pcol = np.arange(128, dtype=np.float32)[:, None]
    return {"c_ident": ident, "c_causal": causal, "c_jj": jj, "c_invf": invf, "c_par": par,
            "c_ltri": ltri, "c_thr": thr, "c_lt": lt, "c_iot": iot, "c_pcol": pcol}


def _in_maps(inputs, NB, ncores):
    f = lambda a: np.ascontiguousarray(a)
    shared = {
        "g_mix": f(inputs["g_mix"]), "g_mem": f(inputs["g_mem"]), "g_ffn": f(inputs["g_ffn"]),
        "w_in": f(inputs["w_in"][0]), "lam_re": f(inputs["lam_re"][0]), "lam_im": f(inputs["lam_im"][0]), "log_dt": f(inputs["log_dt"]),
        "b_re": f(inputs["b_re"][0]), "b_im": f(inputs["b_im"][0]),
        "c_re": f(inputs["c_re"][0].reshape(512, 64)), "c_im": f(inputs["c_im"][0].reshape(512, 64)),
        "d_skip": f(inputs["d_skip"]), "w_glu": f(inputs["w_glu"][0]),
        "g_q": f(inputs["g_q"]), "g_k": f(inputs["g_k"]), "g_qm": f(inputs["g_qm"]), "g_km": f(inputs["g_km"]),
        "w_mem_kv": f(inputs["w_mem_kv"][0]), "w_up_ssm": f(inputs["w_up_ssm"][0]), "w_up_dsa": f(inputs["w_up_dsa"][0]),
        "w_up_mem": f(inputs["w_up_mem"][0]), "w_out": f(inputs["w_out"][0]),
        "w_group": f(inputs["w_group"][0]), "b_group": f(inputs["b_group"]), "w_expert": f(inputs["w_expert"][0]),
        "b_expert": f(inputs["b_expert"][0].reshape(1, 32)),
        "w_gate": f(inputs["w_gate"][0]), "w_up": f(inputs["w_up"][0]), "w_down": f(inputs["w_down"][0]),
    }
    shared.update(_consts())
    maps = []
    for c in range(ncores):
        m = dict(shared)
        m["x"] = f(inputs["x"][c * NB:(c + 1) * NB])
        m["mem"] = f(inputs["mem"][c * NB:(c + 1) * NB])
        m["positions"] = f(inputs["positions"][c * NB:(c + 1) * NB].astype(np.int32))
        maps.append(m)
    return maps


def kernel(**inputs):
    NB = 4
    nc, _ = build_program(NB=NB, NST=4, NEXP=32)
    maps = _in_maps(inputs, NB, NCORES)
    res = run_bass_kernel_spmd(nc, maps, core_ids=list(range(NCORES)))
    out = np.concatenate([np.asarray(r["out"]) for r in res.results], axis=0)
    return out.astype(np.float32, copy=False)
```

```python
import math
from os import environ as _os_env
import numpy as np
from contextlib import ExitStack
import concourse.bass as bass
import concourse.mybir as mybir
from concourse.bass_utils import run_bass_kernel_spmd

F32 = mybir.dt.float32
BF16 = mybir.dt.bfloat16
I32 = mybir.dt.int32
AF = mybir.ActivationFunctionType
ALU = mybir.AluOpType
AX = mybir.AxisListType

NCORES = 8
MOE_SCALE = float(_os_env.get("MOE_SCALE", "0.8"))
QA = "pool"
import os as _os
MOE_SUB = int(_os.environ.get("MOE_SUB", "9"))
D = 1024
SEQ = 2048
MEM = 256
INC = 5060
TWO_PI = 2.0 * math.pi


class Buf:
    __slots__ = ("name", "w", "r")

    def __init__(self, name=""):
        self.name = name
        self.w = None
        self.r = {}


class T:
    def __init__(self, t, name=""):
        self.t = t
        self.b = Buf(name)

    def __getitem__(self, idx):
        return self.t[idx]


def _b(x):
    return x.b if isinstance(x, T) else x


class FW:
    NDMA = 32

    def __init__(self, nc, es):
        self.nc = nc
        self.es = es
        self.eng = {"pe": nc.tensor, "dve": nc.vector, "act": nc.scalar, "pool": nc.gpsimd, "sp": nc.sync}
        self.sem = {k: es.enter_context(nc.semaphore("s_" + k)) for k in self.eng}
        self.cnt = {k: 0 for k in self.eng}
        self.seen = {k: {} for k in self.eng}
        self.dsem = [es.enter_context(nc.semaphore("d%d" % i)) for i in range(self.NDMA)]
        self.dcnt = [0] * self.NDMA
        self.dnext = 0
        self.ninstr = 0
        self.nwaits = 0

    def sb(self, name, shape, dt, es=None):
        self.nalloc = getattr(self, "nalloc", 0) + 1
        return T((es or self.es).enter_context(self.nc.sbuf_tensor("%s_%d" % (name, self.nalloc), list(shape), dt)), name)

    def _wait(self, e, ev):
        sem, val, _ = ev
        key = id(sem)
        if self.seen[e].get(key, 0) >= val:
            return
        self.eng[e].wait_ge(sem, val)
        self.seen[e][key] = val
        self.nwaits += 1

    def _deps(self, e, reads, writes, pe_acc=False):
        for b in reads:
            if b.w is not None:
                self._wait(e, b.w)
        for b in writes:
            if b.w is not None and not (pe_acc and b.w[2] == "pe" and e == "pe"):
                self._wait(e, b.w)
            for ev in b.r.values():
                self._wait(e, ev)

    def _mark(self, ev, reads, writes):
        key = id(ev[0])
        for b in reads:
            old = b.r.get(key)
            if old is None or old[1] < ev[1]:
                b.r[key] = ev
        for b in writes:
            b.w = ev
            b.r = {}

    def op(self, e, fn, reads=(), writes=(), pe_acc=False):
        reads = [_b(x) for x in reads]
        writes = [_b(x) for x in writes]
        self._deps(e, reads, writes, pe_acc)
        ins = fn(self.eng[e])
        self.cnt[e] += 1
        ins.then_inc(self.sem[e], 1)
        self._mark((self.sem[e], self.cnt[e], e), reads, writes)
        self.ninstr += 1

    def dma(self, out, in_, reads=(), writes=(), q="sp", **kw):
        reads = [_b(x) for x in reads]
        writes = [_b(x) for x in writes]
        i = self.dnext
        self.dnext = (self.dnext + 1) % self.NDMA
        sem = self.dsem[i]
        if self.dcnt[i] > 0:
            self._wait(q, (sem, self.dcnt[i], "dma"))
        self._deps(q, reads, writes)
        ins = self.eng[q].dma_start(out=out, in_=in_, **kw)
        self.dcnt[i] += 16
        ins.then_inc(sem, 16)
        self._mark((sem, self.dcnt[i], "dma"), reads, writes)
        self.ninstr += 1

    def idma(self, out, out_off, in_, in_off, bound, reads=(), writes=()):
        q = "pool"
        reads = [_b(x) for x in reads]
        writes = [_b(x) for x in writes]
        i = self.dnext
        self.dnext = (self.dnext + 1) % self.NDMA
        sem = self.dsem[i]
        if self.dcnt[i] > 0:
            self._wait(q, (sem, self.dcnt[i], "dma"))
        self._deps(q, reads, writes)
        ins = self.nc.gpsimd.indirect_dma_start(out=out, out_offset=out_off, in_=in_, in_offset=in_off)
        self.dcnt[i] += 16
        ins.then_inc(sem, 16)
        self._mark((sem, self.dcnt[i], "dma"), reads, writes)
        self.ninstr += 1

    def barrier(self, engines=None):
        for e in (engines or list(self.eng)):
            for i in range(self.NDMA):
                if self.dcnt[i] > 0:
                    self._wait(e, (self.dsem[i], self.dcnt[i], "dma"))
            for k in self.eng:
                if k != e and self.cnt[k] > 0:
                    self._wait(e, (self.sem[k], self.cnt[k], k))


C_U, C_Q, C_KVI, C_QM, C_G0 = 0, 1, 2, 3, 4
C_GLU, C_UPS, C_UPD, C_UPM, C_OUT0, C_OUT1, C_MKV0, C_MKV1, C_EXP = 10, 11, 12, 13, 14, 15, 16, 17, 18
NCH = C_EXP + 96
NSLOT = 2
NTILE = 160


class _Stop(Exception):
    pass


def build_program(NB=4, NST=4, NEXP=32, dbg=(), upto=99):
    nc = bass.Bass("TRN2", target_bir_lowering=False)
    dbg = set(dbg)

    def stage(k):
        if upto >= k:
            with ExitStack() as e_:
                yield e_

    def din(name, shape, dt=F32):
        return nc.dram_tensor(name, list(shape), dt, kind="ExternalInput").ap()

    x_d = din("x", [NB, SEQ, D])
    mem_d = din("mem", [NB, MEM, D])
    pos_d = din("positions", [NB, SEQ], I32)
    g_mix_d = din("g_mix", [1, D]); g_mem_d = din("g_mem", [1, D]); g_ffn_d = din("g_ffn", [1, D])
    w_in_d = din("w_in", [D, INC])
    lam_re_d = din("lam_re", [32, 64]); lam_im_d = din("lam_im", [32, 64]); log_dt_d = din("log_dt", [1, 32])
    b_re_d = din("b_re", [32, 64, 16]); b_im_d = din("b_im", [32, 64, 16])
    c_re_d = din("c_re", [512, 64]); c_im_d = din("c_im", [512, 64])
    d_skip_d = din("d_skip", [1, 512])
    w_glu_d = din("w_glu", [512, 512])
    g_q_d = din("g_q", [1, 64]); g_k_d = din("g_k", [1, 64]); g_qm_d = din("g_qm", [1, 128]); g_km_d = din("g_km", [1, 128])
    w_mem_kv_d = din("w_mem_kv", [D, D])
    w_up_ssm_d = din("w_up_ssm", [512, D]); w_up_dsa_d = din("w_up_dsa", [512, D]); w_up_mem_d = din("w_up_mem", [512, D])
    w_out_d = din("w_out", [D, D])
    w_group_d = din("w_group", [D, 4]); b_group_d = din("b_group", [1, 4])
    w_expert_d = din("w_expert", [D, 32]); b_expert_d = din("b_expert", [1, 32])
    w_gate_d = din("w_gate", [32, D, 512]); w_upx_d = din("w_up", [32, D, 512]); w_down_d = din("w_down", [32, 512, D])
    ident_d = din("c_ident", [128, 128]); causal_d = din("c_causal", [128, 128]); jj_d = din("c_jj", [128, 128])
    invf_d = din("c_invf", [1, 32]); par_d = din("c_par", [128, 2])
    ltri_d = din("c_ltri", [128, 128]); thr_d = din("c_thr", [1, 64]); ltc_d = din("c_lt", [1, 1024])
    iot_d = din("c_iot", [1, NTILE]); pcol_d = din("c_pcol", [128, 1])

    out_d = nc.dram_tensor("out", [NB, SEQ, D], F32, kind="ExternalOutput").ap()
    wsc = nc.dram_tensor("wsc", [NCH, 128, 4096], BF16, kind="Internal").ap()
    HN = nc.dram_tensor("hn_scr", [NB * SEQ, D], BF16, kind="Internal").ap()
    XM = nc.dram_tensor("xm_scr", [NB * SEQ, D], F32, kind="Internal").ap()
    HS = nc.dram_tensor("hs_scr", [NTILE * 128, D], BF16, kind="Internal").ap()
    YS = nc.dram_tensor("ys_scr", [NTILE * 128, D], F32, kind="Internal").ap()
    HNb, XMb, HSb, YSb = Buf("HN"), Buf("XM"), Buf("HS"), Buf("YS")
    dbg_t = {}

    def dbg_out(name, shape, dt=F32):
        dbg_t[name] = nc.dram_tensor("dbg_" + name, list(shape), dt, kind="ExternalOutput").ap()
        return dbg_t[name]

    with ExitStack() as es:
        fw = FW(nc, es)
        op = fw.op

        def TT(e, out, in0, in1, o, R, W):
            op(e, lambda E: E.tensor_tensor(out=out, in0=in0, in1=in1, op=o), R, W)

        def TS(e, out, in0, s1, s2, o0, o1, R, W):
            if o1 is None:
                op(e, lambda E: E.tensor_scalar(out=out, in0=in0, scalar1=s1, scalar2=None, op0=o0), R, W)
            else:
                op(e, lambda E: E.tensor_scalar(out=out, in0=in0, scalar1=s1, scalar2=s2, op0=o0, op1=o1), R, W)

        def STT(out, in0, sc, in1, o0, o1, R, W):
            op("dve", lambda E: E.scalar_tensor_tensor(out=out, in0=in0, scalar=sc, in1=in1, op0=o0, op1=o1), R, W)

        def ACT(out, in_, func, R, W, scale=None, bias=None, accum=None):
            kw = {}
            if scale is not None:
                kw["scale"] = scale
            if bias is not None:
                kw["bias"] = bias
            if accum is not None:
                kw["accum_out"] = accum
            op("act", lambda E: E.activation(out=out, in_=in_, func=func, **kw), R, W)

        def CP(e, out, in_, R, W):
            if e == "act":
                op("act", lambda E: E.copy(out=out, in_=in_), R, W)
            else:
                op(e, lambda E: E.tensor_copy(out=out, in_=in_), R, W)

        def MM(out, lhsT, rhs, start, stop, R, W):
            op("pe", lambda E: E.matmul(out, lhsT=lhsT, rhs=rhs, start=start, stop=stop), R, W, pe_acc=True)

        def TR(out, in_, idn, R, W):
            op("pe", lambda E: E.transpose(out=out, in_=in_, identity=idn), R, W, pe_acc=True)

        def MSET(e, out, val, W):
            op(e, lambda E: E.memset(out, val), (), W)

        def rstd_of(ssq, dim, R):
            TS("dve", ssq, ssq, 1.0 / dim, 1e-6, ALU.mult, ALU.add, R, R)
            ACT(ssq, ssq, AF.Sqrt, R, R)
            op("dve", lambda E: E.reciprocal(out=ssq, in_=ssq), R, R)

        pbank = [T(es.enter_context(nc.psum_tensor("pb%d" % i, [128, 512], F32)), "pb%d" % i) for i in range(8)]
        ps_state = {"i": 0, "ring": list(range(8))}

        def PS():
            r = ps_state["ring"]
            ps_state["i"] = (ps_state["i"] + 1) % len(r)
            return pbank[r[ps_state["i"]]]

        sb = fw.sb
        esP1 = ExitStack()

        def sbp(name, shape, dt):
            return fw.sb(name, shape, dt, esP1)

        ident = sb("ident", [128, 128], F32)
        identb = sb("identb", [128, 128], BF16)
        gffnT = sb("gffnT", [128, 8], F32)
        ring = [sb("ring%d" % i, [128, 4096], BF16) for i in range(NSLOT)]
        ltri = sb("ltri", [128, 128], F32); ones = sb("ones", [128, 128], F32)
        thr = sb("thr", [128, 64], F32); iot = sb("iot", [128, NTILE], F32); pcol = sb("pcol", [128, 1], F32)
        NTT = NB * NST * 4
        OH1 = sb("OH1", [128, NTT, 32], BF16); OH2 = sb("OH2", [128, NTT, 32], BF16)
        RNK = sb("RNK", [128, NTT, 2], F32)
        W12 = sb("W12", [128, NTT, 2], F32)
        run = sb("run", [128, 32], F32)
        dpi = [sb("dpi%d" % c, [128, NTT], I32) for c in range(2)]
        widx = sb("widx", [128, NTILE, 3], I32)
        irep = sbp("irep", [128, 512], BF16)
        causal = sbp("causal", [128, 128], F32)
        causalb = sbp("causalb", [128, 128], BF16)
        zerob = sbp("zerob", [128, 128], BF16)
        jj = sbp("jj", [128, 128], F32)
        invf = sbp("invf", [128, 32], F32)
        par = sbp("par", [128, 2], F32)
        gmixT = sbp("gmixT", [128, 8], F32)
        gffnT64 = sbp("gffnT64", [64, 16], F32)
        gmemT = sbp("gmemT", [128, 8], F32)
        gq = sbp("gq", [128, 64], F32); gk = sbp("gk", [128, 64], F32)
        gqm = sbp("gqm", [128, 128], F32); gkm = sbp("gkm", [128, 128], F32)
        wr32 = sbp("wr32", [128, 8, 36], F32)
        rbias = sbp("rbias", [128, 36], F32)
        XsB = [[Buf("Xs_%d_%d" % (t_, h_)) for h_ in range(2)] for t_ in range(4)]
        XsAll = [XsB[t_][h_] for t_ in range(4) for h_ in range(2)]
        cosb = sbp("cosb", [128, 16, 32], F32); sinb = sbp("sinb", [128, 16, 32], F32)
        kT = sbp("kT", [64, SEQ], BF16)
        kiT = sbp("kiT", [64, SEQ], F32)
        vaug = sbp("vaug", [128, 16, 65], BF16)
        kmT = sbp("kmT", [128, 4, MEM], BF16)
        vmaug = sbp("vmaug", [128, 2, 4, 129], BF16)
        s5_mag = sbp("s5_mag", [128, 16], F32)
        s5_c128 = sbp("s5_c128", [128, 16], F32); s5_s128 = sbp("s5_s128", [128, 16], F32); s5_s128n = sbp("s5_s128n", [128, 16], F32)
        tabc = sbp("tabc", [128, 16, 128], F32); tabs = sbp("tabs", [128, 16, 128], F32)
        BTre = sbp("BTre", [128, 16, 128], BF16); BTim = sbp("BTim", [128, 16, 128], BF16)
        Cwre = sbp("Cwre", [128, 16, 64], BF16); Cwim = sbp("Cwim", [128, 16, 64], BF16)
        dsk = sbp("dsk", [128, 4], F32)
        carr = sbp("carr", [128, 16, 2], F32)

        chunk_buf = [Buf("ch%d" % i) for i in range(NCH)]

        class WRing:
            def __init__(self, slots, q="sp"):
                self.q = q
                self.slots = slots
                self.plan = []
                self.next_issue = 0
                self.next_req = 0
                self.free = list(range(len(slots)))
                self.slot_of = {}

            def pump(self):
                while self.free and self.next_issue < len(self.plan):
                    cid = self.plan[self.next_issue]
                    s_ = self.free.pop(0)
                    fw.dma(self.slots[s_][:], wsc[cid], reads=[chunk_buf[cid]], writes=[self.slots[s_]], q=self.q)
                    self.slot_of[self.next_issue] = s_
                    self.next_issue += 1

            def get(self, cid, kc, n):
                k = self.next_req
                assert self.plan[k] == cid, (k, self.plan[k], cid)
                self.pump()
                assert k in self.slot_of, "ring overflow"
                self.next_req += 1
                s_ = self.slot_of[k]
                return self.slots[s_], self.slots[s_][:, 0:kc * n].rearrange("p (k n) -> p k n", k=kc), (k, s_)

            def rel(self, h):
                self.free.append(h[1])
                self.pump()

        ringA = WRing(ring[0:2], q=QA)

        psr = {"A": {"ring": [0, 1, 2, 3, 4, 5, 6, 7], "i": 0}}

        def PSs(s_):
            r_ = psr[s_]
            r_["i"] = (r_["i"] + 1) % len(r_["ring"])
            return pbank[r_["ring"][r_["i"]]]

        def ps_reserve(s_):
            r_ = psr[s_]
            bk = r_["ring"].pop(0)
            r_["i"] = 0
            return bk

        def ps_release(s_, bk):
            psr[s_]["ring"].append(bk)

        try:
            fw.dma(ident[:], ident_d, writes=[ident])
            fw.dma(causal[:], causal_d, writes=[causal])
            fw.dma(jj[:], jj_d, writes=[jj])
            fw.dma(invf[:], invf_d.partition_broadcast(128), writes=[invf])
            fw.dma(par[:], par_d, writes=[par])
            fw.dma(ltri[:], ltri_d, writes=[ltri])
            fw.dma(thr[:], thr_d.partition_broadcast(128), writes=[thr])
            fw.dma(iot[:], iot_d.partition_broadcast(128), writes=[iot])
            fw.dma(pcol[:], pcol_d, writes=[pcol])
            MSET("dve", ones[:], 1.0, [ones])
            MSET("dve", run[:], 0.0, [run])
            with nc.allow_non_contiguous_dma(reason="tiny gain loads"):
                fw.dma(gmixT[:], g_mix_d.rearrange("o (k p) -> p (o k)", p=128), writes=[gmixT])
                fw.dma(gffnT[:], g_ffn_d.rearrange("o (k p) -> p (o k)", p=128), writes=[gffnT])
                fw.dma(gffnT64[:], g_ffn_d.rearrange("o (k p) -> p (o k)", p=64), writes=[gffnT64])
                fw.dma(gmemT[:], g_mem_d.rearrange("o (k p) -> p (o k)", p=128), writes=[gmemT])
            fw.dma(gq[:], g_q_d.partition_broadcast(128), writes=[gq])
            fw.dma(gk[:], g_k_d.partition_broadcast(128), writes=[gk])
            fw.dma(gqm[:], g_qm_d.partition_broadcast(128), writes=[gqm])
            fw.dma(gkm[:], g_km_d.partition_broadcast(128), writes=[gkm])
            fw.dma(wr32[:, :, 0:4], w_group_d.rearrange("(k p) n -> p k n", p=128), writes=[wr32])
            fw.dma(wr32[:, :, 4:36], w_expert_d.rearrange("(k p) n -> p k n", p=128), writes=[wr32])
            fw.dma(rbias[:, 0:4], b_group_d.partition_broadcast(128), writes=[rbias])
            fw.dma(rbias[:, 4:36], b_expert_d.partition_broadcast(128), writes=[rbias])
            CP("dve", identb[:], ident[:], [ident], [identb])
            for r in range(4):
                CP("dve", irep[:, r * 128:(r + 1) * 128], ident[:], [ident], [irep])
            TS("dve", causalb[:], causal[:], -1.0, -30000.0, ALU.is_lt, ALU.mult, [causal], [causalb])
            MSET("dve", vaug[:, :, 64:65], 1.0, [vaug])
            MSET("dve", zerob[:], 0.0, [zerob])
            MSET("dve", vmaug[:, :, :, 128:129], 1.0, [vmaug])

            def w_in_cols(c0, c1):
                return w_in_d.rearrange("(k p) n -> p k n", p=128)[:, :, c0:c1], 8, c1 - c0

            srcs = {C_U: w_in_cols(0, 512), C_Q: w_in_cols(512, 1024), C_KVI: w_in_cols(1024, 1476), C_QM: w_in_cols(1476, 1988)}
            for i in range(6):
                srcs[C_G0 + i] = w_in_cols(1988 + 512 * i, 1988 + 512 * (i + 1))
            srcs[C_GLU] = (w_glu_d.rearrange("(k p) n -> p k n", p=128), 4, 512)
            srcs[C_UPS] = (w_up_ssm_d.rearrange("(k p) n -> p k n", p=128), 4, 1024)
            srcs[C_UPD] = (w_up_dsa_d.rearrange("(k p) n -> p k n", p=128), 4, 1024)
            srcs[C_UPM] = (w_up_mem_d.rearrange("(k p) n -> p k n", p=128), 4, 1024)
            srcs[C_OUT0] = (w_out_d.rearrange("(k p) n -> p k n", p=128)[:, :, 0:512], 8, 512)
            srcs[C_OUT1] = (w_out_d.rearrange("(k p) n -> p k n", p=128)[:, :, 512:1024], 8, 512)
            srcs[C_MKV0] = (w_mem_kv_d.rearrange("(k p) n -> p k n", p=128)[:, :, 0:512], 8, 512)
            srcs[C_MKV1] = (w_mem_kv_d.rearrange("(k p) n -> p k n", p=128)[:, :, 512:1024], 8, 512)
            for e in range(32):
                srcs[C_EXP + 3 * e] = (w_gate_d[e].rearrange("(k p) n -> p k n", p=128), 8, 512)
                srcs[C_EXP + 3 * e + 1] = (w_upx_d[e].rearrange("(k p) n -> p k n", p=128), 8, 512)
                srcs[C_EXP + 3 * e + 2] = (w_down_d[e].rearrange("(k p) n -> p k n", p=128), 4, 1024)

            with ExitStack() as es0:
                st32 = [sb("st32_%d" % i, [128, 4096], F32, es0) for i in range(3)]
                st16 = [sb("st16_%d" % i, [128, 4096], BF16, es0) for i in range(3)]
                cast_eng = ["pool", "dve", "act"]
                z16 = sb("z16", [128, 4096], BF16, es0)
                MSET("pool", z16[:], 0.0, [z16])
                hs_z = HS.rearrange("(a p k) n -> a p (k n)", p=128, k=4)
                for a_ in range(NTILE // 4):
                    fw.dma(hs_z[a_], z16[:], reads=[z16], writes=[HSb])
                order = list(range(C_EXP))
                for n_, cid in enumerate(order):
                    src, kc, n = srcs[cid]
                    a = st32[n_ % 3]; bq = st16[n_ % 3]
                    fw.dma(a[:, 0:kc * n].rearrange("p (k n) -> p k n", k=kc), src, writes=[a])
                    CP(cast_eng[n_ % 3], bq[:, 0:kc * n], a[:, 0:kc * n], [a], [bq])
                    fw.dma(wsc[cid][:, 0:kc * n], bq[:, 0:kc * n], reads=[bq], writes=[chunk_buf[cid]], q="act")

                fw.barrier()
            fw.barrier()
            with ExitStack() as es0:
                lre = sb("lre", [128, 16], F32, es0); lim = sb("lim", [128, 16], F32, es0); dtv = sb("dtv", [128, 16], F32, es0)
                with nc.allow_non_contiguous_dma(reason="tiny param loads"):
                    fw.dma(lre[:], lam_re_d.rearrange("(c g) p -> (g p) c", g=2), writes=[lre])
                    fw.dma(lim[:], lam_im_d.rearrange("(c g) p -> (g p) c", g=2), writes=[lim])
                    ldt2 = log_dt_d.rearrange("o (c g) -> o g c", g=2)
                    for gl in range(2):
                        fw.dma(dtv[gl * 64:(gl + 1) * 64, :], ldt2[:, gl, :].partition_broadcast(64), writes=[dtv])
                    fw.dma(dsk[:], d_skip_d.rearrange("o (c p) -> p (o c)", p=128), writes=[dsk])
                ACT(dtv[:], dtv[:], AF.Exp, [dtv], [dtv])
                are = sb("are", [128, 16], F32, es0); aim = sb("aim", [128, 16], F32, es0)
                TT("dve", are[:], lre[:], dtv[:], ALU.mult, [lre, dtv], [are])
                TT("dve", aim[:], lim[:], dtv[:], ALU.mult, [lim, dtv], [aim])
                ACT(s5_mag[:], are[:], AF.Exp, [are], [s5_mag])

                def wrap_pi(xt, shape, tmp_i, tmp_f):
                    TS("dve", tmp_f[:], xt[:], 1.0 / TWO_PI, None, ALU.mult, None, [xt], [tmp_f])
                    CP("dve", tmp_i[:], tmp_f[:], [tmp_f], [tmp_i])
                    CP("dve", tmp_f[:], tmp_i[:], [tmp_i], [tmp_f])
                    c1 = 6.28125
                    c2 = TWO_PI - c1
                    STT(xt[:], tmp_f[:], -c1, xt[:], ALU.mult, ALU.add, [tmp_f, xt], [xt])
                    STT(xt[:], tmp_f[:], -c2, xt[:], ALU.mult, ALU.add, [tmp_f, xt], [xt])
                    TS("dve", tmp_f[:], xt[:], math.pi, -TWO_PI, ALU.is_gt, ALU.mult, [xt], [tmp_f])
                    TT("dve", xt[:], xt[:], tmp_f[:], ALU.add, [xt, tmp_f], [xt])
                    TS("dve", tmp_f[:], xt[:], -math.pi, TWO_PI, ALU.is_lt, ALU.mult, [xt], [tmp_f])
                    TT("dve", xt[:], xt[:], tmp_f[:], ALU.add, [xt, tmp_f], [xt])

                def cos_arg(dst, src, tmp_f):
                    TS("dve", tmp_f[:], src[:], math.pi / 2, -TWO_PI, ALU.is_gt, ALU.mult, [src], [tmp_f])
                    STT(dst[:], src[:], math.pi / 2, tmp_f[:], ALU.add, ALU.add, [src, tmp_f], [dst])

                ti16 = sb("ti16", [128, 16], I32, es0); tf16 = sb("tf16", [128, 16], F32, es0)
                wrap_pi(aim, None, ti16, tf16)
                lbr = sb("lbr", [128, 16], F32, es0); lbi = sb("lbi", [128, 16], F32, es0); ca16 = sb("ca16", [128, 16], F32, es0)
                cos_arg(ca16, aim, tf16)
                ACT(lbi[:], aim[:], AF.Sin, [aim], [lbi])
                ACT(lbr[:], ca16[:], AF.Sin, [ca16], [lbr])
                TT("dve", lbr[:], lbr[:], s5_mag[:], ALU.mult, [lbr, s5_mag], [lbr])
                TT("dve", lbi[:], lbi[:], s5_mag[:], ALU.mult, [lbi, s5_mag], [lbi])
                n2 = sb("n2", [128, 16], F32, es0); t16a = sb("t16a", [128, 16], F32, es0)
                cfr = sb("cfr", [128, 16], F32, es0); cfi = sb("cfi", [128, 16], F32, es0); lb1 = sb("lb1", [128, 16], F32, es0)
                TT("dve", n2[:], lre[:], lre[:], ALU.mult, [lre], [n2])
                TT("dve", t16a[:], lim[:], lim[:], ALU.mult, [lim], [t16a])
                TT("dve", n2[:], n2[:], t16a[:], ALU.add, [n2, t16a], [n2])
                op("dve", lambda E: E.reciprocal(out=n2[:], in_=n2[:]), [n2], [n2])
                TS("dve", lb1[:], lbr[:], -1.0, None, ALU.add, None, [lbr], [lb1])
                TT("dve", cfr[:], lb1[:], lre[:], ALU.mult, [lb1, lre], [cfr])
                TT("dve", t16a[:], lbi[:], lim[:], ALU.mult, [lbi, lim], [t16a])
                TT("dve", cfr[:], cfr[:], t16a[:], ALU.add, [cfr, t16a], [cfr])
                TT("dve", cfr[:], cfr[:], n2[:], ALU.mult, [cfr, n2], [cfr])
                TT("dve", cfi[:], lbi[:], lre[:], ALU.mult, [lbi, lre], [cfi])
                TT("dve", t16a[:], lb1[:], lim[:], ALU.mult, [lb1, lim], [t16a])
                TT("dve", cfi[:], cfi[:], t16a[:], ALU.subtract, [cfi, t16a], [cfi])
                TT("dve", cfi[:], cfi[:], n2[:], ALU.mult, [cfi, n2], [cfi])
                bre = sb("bre", [128, 16, 16], F32, es0); bim = sb("bim", [128, 16, 16], F32, es0)
                fw.dma(bre[:], b_re_d.rearrange("(c g) p h -> (g p) c h", g=2), writes=[bre])
                fw.dma(bim[:], b_im_d.rearrange("(c g) p h -> (g p) c h", g=2), writes=[bim])
                bbr = sb("bbr", [128, 16, 16], F32, es0); bbi = sb("bbi", [128, 16, 16], F32, es0); tb = sb("tb", [128, 16, 16], F32, es0)
                cfr_b = cfr[:].unsqueeze(2).to_broadcast([128, 16, 16]); cfi_b = cfi[:].unsqueeze(2).to_broadcast([128, 16, 16])
                TT("dve", bbr[:], bre[:], cfr_b, ALU.mult, [bre, cfr], [bbr])
                TT("dve", tb[:], bim[:], cfi_b, ALU.mult, [bim, cfi], [tb])
                TT("dve", bbr[:], bbr[:], tb[:], ALU.subtract, [bbr, tb], [bbr])
                TT("dve", bbi[:], bim[:], cfr_b, ALU.mult, [bim, cfr], [bbi])
                TT("dve", tb[:], bre[:], cfi_b, ALU.mult, [bre, cfi], [tb])
                TT("dve", bbi[:], bbi[:], tb[:], ALU.add, [bbi, tb], [bbi])
                Xw = sb("Xw", [128, 16, 128], F32, es0)
                for (src_bb, dstBT) in ((bbr, BTre), (bbi, BTim)):
                    MSET("pool", Xw[:], 0.0, [Xw])
                    for gl in range(2):
                        for r in range(4):
                            CP("dve", Xw[gl * 64:(gl + 1) * 64, r::4, 32 * r + 16 * gl:32 * r + 16 * gl + 16],
                               src_bb[gl * 64:(gl + 1) * 64, r::4, :], [src_bb], [Xw])
                    for fc in range(4):
                        pb = PS()
                        for r in range(4):
                            MM(pb[:, r * 128:(r + 1) * 128], Xw[:, 4 * fc + r, :], ident[:], True, True, [Xw, ident], [pb])
                        CP("act", dstBT[:, 4 * fc:4 * fc + 4, :], pb[:].rearrange("p (r n) -> p r n", r=4), [pb], [dstBT])
                for (c_d, dstC, sign) in ((c_re_d, Cwre, 1.0), (c_im_d, Cwim, -1.0)):
                    MSET("dve", dstC[:], 0.0, [dstC])
                    for k in range(4):
                        cl = sb("cl_%d_%d" % (k, int(sign > 0)), [128, 64], F32, es0)
                        aw = sb("aw_%d_%d" % (k, int(sign > 0)), [128, 128], F32, es0)
                        fw.dma(cl[:], c_d[k * 128:(k + 1) * 128, :], writes=[cl])
                        TS("dve", aw[:, 0:64], cl[:], par[:, 0:1], None, ALU.mult, None, [cl, par], [aw])
                        TS("dve", aw[:, 64:128], cl[:], par[:, 1:2], None, ALU.mult, None, [cl, par], [aw])
                        pb = PS()
                        for r in range(4):
                            MM(pb[:, 32 * r:32 * r + 32], aw[:], ident[:, 32 * r:32 * r + 32], True, True, [aw, ident], [pb])
                        for r in range(4):
                            ACT(dstC[:, 4 * k + r, 32 * (r % 2):32 * (r % 2) + 32], pb[:, 32 * r:32 * r + 32], AF.Copy, [pb], [dstC], scale=sign)
                ang = sb("ang", [128, 16, 128], F32, es0); angi = sb("angi", [128, 16, 128], I32, es0); angf = sb("angf", [128, 16, 128], F32, es0)
                TT("dve", ang[:], jj[:].unsqueeze(1).to_broadcast([128, 16, 128]), aim[:].unsqueeze(2).to_broadcast([128, 16, 128]), ALU.mult, [jj, aim], [ang])
                wrap_pi(ang, None, angi, angf)
                ACT(tabs[:], ang[:], AF.Sin, [ang], [tabs])
                ang2 = sb("ang2", [128, 16, 128], F32, es0)
                cos_arg(ang2, ang, angf)
                ACT(tabc[:], ang2[:], AF.Sin, [ang2], [tabc])
                CP("dve", s5_c128[:], tabc[:, :, 127], [tabc], [s5_c128])
                CP("dve", s5_s128[:], tabs[:, :, 127], [tabs], [s5_s128])
                TS("dve", s5_s128n[:], s5_s128[:], -1.0, None, ALU.mult, None, [s5_s128], [s5_s128n])
                fw.barrier()
            fw.barrier()

            planA_st = [C_U] + [C_Q, C_KVI, C_QM] + [C_GLU] + [C_G0, C_G0 + 1, C_UPS, C_G0 + 2, C_G0 + 3, C_UPD, C_G0 + 4, C_G0 + 5, C_UPM, C_OUT0, C_OUT1]
            planB_st = [C_EXP + 3 * e + j for e in range(NEXP) for j in range(3)]
            for b in range(NB):
                ringA.plan += [C_MKV0, C_MKV1]
                for s in range(NST):
                    ringA.plan += planA_st


            def fence(tiles):
                evs = []
                for t_ in tiles:
                    bb = _b(t_)
                    if bb.w is not None:
                        evs.append(bb.w)
                    evs += list(bb.r.values())
                for e_ in fw.eng:
                    for ev in evs:
                        fw._wait(e_, ev)

            def rope(dst, src, cbx, sbx, tmp, R, W, eng="dve"):
                x1 = src[:, :, 0:32]; x2 = src[:, :, 32:64]
                TT(eng, tmp, x2, sbx, ALU.mult, R, W)
                TT(eng, dst[:, :, 0:32], x1, cbx, ALU.mult, R, W)
                TT(eng, dst[:, :, 0:32], dst[:, :, 0:32], tmp, ALU.subtract, W, W)
                TT(eng, tmp, x1, sbx, ALU.mult, R, W)
                TT(eng, dst[:, :, 32:64], x2, cbx, ALU.mult, R, W)
                TT(eng, dst[:, :, 32:64], dst[:, :, 32:64], tmp, ALU.add, W, W)

            def batch_prep(b):
                with ExitStack() as esb:
                    posi = sb("posi", [16, 128], I32, esb); posf = sb("posf", [16, 128], F32, esb); post = sb("post", [128, 16], F32, esb)
                    fw.dma(posi[:], pos_d[b].rearrange("(t p) -> t p", p=128), writes=[posi])
                    CP("dve", posf[:], posi[:], [posi], [posf])
                    pb = PS()
                    TR(pb[:, 0:16], posf[:], ident[0:16, 0:16], [posf, ident], [pb])
                    CP("dve", post[:], pb[:, 0:16], [pb], [post])
                    angb = sb("angb", [128, 16, 32], F32, esb); angbi = sb("angbi", [128, 16, 32], I32, esb); angbf = sb("angbf", [128, 16, 32], F32, esb)
                    angb2 = sb("angb2", [128, 16, 32], F32, esb)
                    TT("dve", angb[:], post[:].unsqueeze(2).to_broadcast([128, 16, 32]), invf[:].unsqueeze(1).to_broadcast([128, 16, 32]), ALU.mult, [post, invf], [angb])
                    wrap_pi(angb, None, angbi, angbf)
                    ACT(sinb[:], angb[:], AF.Sin, [angb], [sinb])
                    cos_arg(angb2, angb, angbf)
                    ACT(cosb[:], angb2[:], AF.Sin, [angb2], [cosb])
                    MSET("dve", carr[:], 0.0, [carr])
                    memx = sb("memx", [128, 2, D], F32, esb); memb = sb("memb", [128, D], BF16, esb); memT = sb("memT", [128, 8, MEM], BF16, esb)
                    mss = sb("mss", [128, 2], F32, esb)
                    fw.dma(memx[:], mem_d[b].rearrange("(t p) d -> p t d", p=128), writes=[memx])
                    for mt in range(2):
                        ACT(memb[:], memx[:, mt, :], AF.Square, [memx], [memb, mss], accum=mss[:, mt:mt + 1])
                    rstd_of(mss[:], D, [mss])
                    for mt in range(2):
                        TS("dve", memb[:], memx[:, mt, :], mss[:, mt:mt + 1], None, ALU.mult, None, [memx, mss], [memb])
                        pb = PS()
                        pbv = pb[:].bitcast(BF16)
                        for kc in range(8):
                            TR(pbv[:, kc * 128:(kc + 1) * 128], memb[:, kc * 128:(kc + 1) * 128], identb[:], [memb, identb], [pb])
                        TT("dve", memT[:, :, mt * 128:(mt + 1) * 128], pbv[:, 0:1024].rearrange("p (k n) -> p k n", k=8),
                           gmemT[:].unsqueeze(2).to_broadcast([128, 8, 128]), ALU.mult, [pb, gmemT], [memT])
                    slK, wK, hK = ringA.get(C_MKV0, 8, 512)
                    slV, wV, hV = ringA.get(C_MKV1, 8, 512)
                    kmraw = sb("kmraw", [128, 512], F32, esb); kmsq = sb("kmsq", [128, 512], F32, esb); kss = sb("kss", [128, 4], F32, esb)
                    kmn = sb("kmn", [128, 512], BF16, esb)
                    for mt in range(2):
                        pb = PS()
                        for kc in range(8):
                            MM(pb[:], memT[:, kc, mt * 128:(mt + 1) * 128], wK[:, kc, :], kc == 0, kc == 7, [memT, slK], [pb])
                        CP("act", kmraw[:], pb[:], [pb], [kmraw])
                        TT("dve", kmsq[:], kmraw[:], kmraw[:], ALU.mult, [kmraw], [kmsq])
                        op("dve", lambda E: E.tensor_reduce(out=kss[:], in_=kmsq[:].rearrange("p (h d) -> p h d", h=4), axis=AX.X, op=ALU.add), [kmsq], [kss])
                        rstd_of(kss[:], 128, [kss])
                        TT("dve", kmraw[:].rearrange("p (h d) -> p h d", h=4), kmraw[:].rearrange("p (h d) -> p h d", h=4),
                           kss[:].unsqueeze(2).to_broadcast([128, 4, 128]), ALU.mult, [kmraw, kss], [kmraw])
                        TT("dve", kmn[:].rearrange("p (h d) -> p h d", h=4), kmraw[:].rearrange("p (h d) -> p h d", h=4),
                           gkm[:].unsqueeze(1).to_broadcast([128, 4, 128]), ALU.mult, [kmraw, gkm], [kmn])
                        pb2 = PS()
                        pbv = pb2[:].bitcast(BF16)
                        for h in range(4):
                            TR(pbv[:, h * 128:(h + 1) * 128], kmn[:, h * 128:(h + 1) * 128], identb[:], [kmn, identb], [pb2])
                        CP("act", kmT[:, :, mt * 128:(mt + 1) * 128], pbv[:, 0:512].rearrange("p (h n) -> p h n", h=4), [pb2], [kmT])
                        pb3 = PS()
                        for kc in range(8):
                            MM(pb3[:], memT[:, kc, mt * 128:(mt + 1) * 128], wV[:, kc, :], kc == 0, kc == 7, [memT, slV], [pb3])
                        CP("act", vmaug[:, mt, :, 0:128], pb3[:].rearrange("p (h d) -> p h d", h=4), [pb3], [vmaug])
                    ringA.rel(hK); ringA.rel(hV)
                    fw.barrier()

            def attn_AG(b, st, A, esa):
                tok0 = st * 512
                hT = A["hT"]
                ydsaT, ymemT, yssmT, rst = A["ydsaT"], A["ymemT"], A["yssmT"], A["rst"]
                exa = ExitStack()
                xt = [sb("xt%d" % i, [128, D], F32, exa) for i in range(2)]
                hb = sb("hb", [128, D], BF16, exa)
                PSa = lambda: PSs("A")
                for t in range(4):
                    x_ = xt[t % 2]
                    fw.dma(x_[:], x_d[b, tok0 + t * 128:tok0 + (t + 1) * 128, :], writes=[x_], q=QA)
                    ACT(hb[:], x_[:], AF.Square, [x_], [hb, rst], accum=rst[:, t:t + 1])
                    yield 0.4
                    rstd_of(rst[:, t:t + 1], D, [rst])
                    TS("dve", hb[:], x_[:], rst[:, t:t + 1], None, ALU.mult, None, [x_, rst], [hb])
                    pb = PSa(); pbv = pb[:].bitcast(BF16)
                    for kc in range(8):
                        TR(pbv[:, kc * 128:(kc + 1) * 128], hb[:, kc * 128:(kc + 1) * 128], identb[:], [hb, identb], [pb])
                    TT("dve", hT[:, :, t * 128:(t + 1) * 128], pbv[:, 0:1024].rearrange("p (k n) -> p k n", k=8),
                       gmixT[:].unsqueeze(2).to_broadcast([128, 8, 128]), ALU.mult, [pb, gmixT], [hT])
                    yield 0.4
                fence(xt + [hb])
                exa.close()
                e2a = ExitStack(); e2b = ExitStack()
                uT = sb("uT", [128, 4, 512], BF16, e2a)
                qT = sb("qT", [64, 8, 512], BF16, e2b); qiT = sb("qiT", [64, 4, 512], F32, e2b)
                qmT = sb("qmT", [128, 4, 512], BF16, e2b); wis = sb("wis", [128, 4, 4], F32, e2b)
                sl, w, hh = ringA.get(C_U, 8, 512)
                for fc in range(4):
                    pb = PSa()
                    for kc in range(8):
                        MM(pb[:], w[:, kc, fc * 128:(fc + 1) * 128], hT[:, kc, :], kc == 0, kc == 7, [sl, hT], [pb])
                    CP("act", uT[:, fc, :], pb[:], [pb], [uT])
                    yield 0.4
                ringA.rel(hh)
                with ExitStack() as est:
                    raws = [(sb("qraw", [128, 512], F32, est), sb("kviraw", [128, 452], F32, est), sb("qmraw", [128, 512], F32, est)) for i_ in range(4)]
                    sq = sb("sq", [128, 512], F32, est); s8 = sb("s8", [128, 12], F32, est)
                    qn = sb("qn", [128, 8, 64], F32, est); qr = sb("qr", [128, 8, 64], BF16, est); tq = sb("tq", [128, 8, 32], F32, est)
                    kn = sb("kn", [128, 1, 64], F32, est); kr = sb("kr", [128, 1, 64], BF16, est)
                    qir = sb("qir", [128, 5, 64], F32, est)
                    qmn = sb("qmn", [128, 4, 128], BF16, est)
                    dtiles = [x_ for r_ in raws for x_ in r_] + [sq, s8, qn, qr, tq, kn, kr, qir, qmn]
                    cst32 = [sb("cst32_%d" % i_, [128, 4096], F32, est) for i_ in range(2)]
                    cst16 = [sb("cst16_%d" % i_, [128, 4096], BF16, est) for i_ in range(1)]
                    exp_ids = [C_EXP + 3 * e_ + j_ for e_ in range(NEXP) for j_ in range(3)]
                    n_st = NB * NST
                    per = (len(exp_ids) + n_st - 1) // n_st
                    my_ids = exp_ids[(b * NST + st) * per:(b * NST + st + 1) * per]

                    def c_in(k_):
                        src_, kc_, n_ = srcs[my_ids[k_]]
                        a_ = cst32[k_ % 2]
                        fw.dma(a_[:, 0:kc_ * n_].rearrange("p (k n) -> p k n", k=kc_), src_, writes=[a_])

                    def c_out(k_):
                        cid_ = my_ids[k_]
                        src_, kc_, n_ = srcs[cid_]
                        a_ = cst32[k_ % 2]; q_ = cst16[k_ % len(cst16)]
                        CP("act", q_[:, 0:kc_ * n_], a_[:, 0:kc_ * n_], [a_], [q_])
                        fw.dma(wsc[cid_][:, 0:kc_ * n_], q_[:, 0:kc_ * n_], reads=[q_], writes=[chunk_buf[cid_]])

                    for k_ in range(min(2, len(my_ids))):
                        c_in(k_)
                    c_next = 0
                    dtiles += cst32 + cst16
                    for ci, (cid, n) in enumerate(((C_Q, 512), (C_KVI, 452), (C_QM, 512))):
                        sl, w, hh = ringA.get(cid, 8, n)
                        for t in range(4):
                            tsl = slice(t * 128, (t + 1) * 128)
                            dst = raws[t][ci]
                            pb = PSa()
                            for kc in range(8):
                                MM(pb[:, 0:n], hT[:, kc, tsl], w[:, kc, :], kc == 0, kc == 7, [hT, sl], [pb])
                            CP("act", dst[:], pb[:, 0:n], [pb], [dst])
                            yield 0.4
                        ringA.rel(hh)
                    for t in range(4):
                        gt_ = st * 4 + t
                        tsl = slice(t * 128, (t + 1) * 128)
                        cos_t = cosb[:, gt_, :]; sin_t = sinb[:, gt_, :]
                        qraw, kviraw, qmraw = raws[t]
                        q3 = qraw[:].rearrange("p (h d) -> p h d", h=8)
                        TT("pool", sq[:], qraw[:], qraw[:], ALU.mult, [qraw], [sq])
                        op("dve", lambda E: E.tensor_reduce(out=s8[:, 0:8], in_=sq[:].rearrange("p (h d) -> p h d", h=8), axis=AX.X, op=ALU.add), [sq], [s8])
                        TT("pool", sq[:, 0:64], kviraw[:, 0:64], kviraw[:, 0:64], ALU.mult, [kviraw, sq], [sq])
                        op("dve", lambda E: E.tensor_reduce(out=s8[:, 8:9], in_=sq[:, 0:64], axis=AX.X, op=ALU.add), [sq], [s8])
                        rstd_of(s8[:, 0:9], 64, [s8])
                        TT("dve", qn[:], q3, s8[:, 0:8].unsqueeze(2).to_broadcast([128, 8, 64]), ALU.mult, [qraw, s8], [qn])
                        TT("pool", qn[:], qn[:], gq[:].unsqueeze(1).to_broadcast([128, 8, 64]), ALU.mult, [qn, gq], [qn])
                        cb = cos_t.unsqueeze(1).to_broadcast([128, 8, 32]); sbb = sin_t.unsqueeze(1).to_broadcast([128, 8, 32])
                        rope(qr, qn, cb, sbb, tq[:], [qn, cosb, sinb], [qr, tq], eng="pool")
                        yield 0.4
                        pb = PSa(); pbv = pb[:].bitcast(BF16)
                        for h in range(8):
                            TR(pbv[0:64, h * 128:(h + 1) * 128], qr[:, h, :], identb[:], [qr, identb], [pb])
                        CP("act", qT[:, :, tsl], pbv[0:64, 0:1024].rearrange("p (h n) -> p h n", h=8), [pb], [qT])
                        yield 0.4
                        TS("dve", kn[:, 0, :], kviraw[:, 0:64], s8[:, 8:9], None, ALU.mult, None, [kviraw, s8], [kn])
                        TT("pool", kn[:, 0, :], kn[:, 0, :], gk[:], ALU.mult, [kn, gk], [kn])
                        c1b = cos_t.unsqueeze(1); s1b = sin_t.unsqueeze(1)
                        rope(kr, kn, c1b, s1b, tq[:, 0:1, :], [kn, cosb, sinb], [kr, tq], eng="pool")
                        yield 0.4
                        pb = PSa(); pbv = pb[:].bitcast(BF16)
                        TR(pbv[0:64, 0:128], kr[:, 0, :], identb[:], [kr, identb], [pb])
                        CP("act", kT[:, gt_ * 128:(gt_ + 1) * 128], pbv[0:64, 0:128], [pb], [kT])
                        yield 0.4
                        CP("pool", vaug[:, gt_, 0:64], kviraw[:, 64:128], [kviraw], [vaug])
                        yield 0.4
                        qi3 = kviraw[:, 128:448].rearrange("p (h d) -> p h d", h=5)
                        cb5 = cos_t.unsqueeze(1).to_broadcast([128, 5, 32]); sb5 = sin_t.unsqueeze(1).to_broadcast([128, 5, 32])
                        rope(qir, qi3, cb5, sb5, tq[:, 0:5, :], [kviraw, cosb, sinb], [qir, tq], eng="pool")
                        yield 0.4
                        pb = PSa()
                        for h in range(4):
                            TR(pb[0:64, h * 128:(h + 1) * 128], qir[:, h, :], ident[:], [qir, ident], [pb])
                        CP("act", qiT[:, :, tsl], pb[0:64, :].rearrange("p (h n) -> p h n", h=4), [pb], [qiT])
                        yield 0.4
                        pb = PSa()
                        TR(pb[0:64, 0:128], qir[:, 4, :], ident[:], [qir, ident], [pb])
                        CP("act", kiT[:, gt_ * 128:(gt_ + 1) * 128], pb[0:64, 0:128], [pb], [kiT])
                        yield 0.4
                        TS("dve", wis[:, t, :], kviraw[:, 448:452], 0.5 * 0.125, None, ALU.mult, None, [kviraw], [wis])
                        yield 0.4
                        TT("pool", sq[:], qmraw[:], qmraw[:], ALU.mult, [qmraw], [sq])
                        op("dve", lambda E: E.tensor_reduce(out=s8[:, 0:4], in_=sq[:].rearrange("p (h d) -> p h d", h=4), axis=AX.X, op=ALU.add), [sq], [s8])
                        rstd_of(s8[:, 0:4], 128, [s8])
                        TT("dve", qmraw[:].rearrange("p (h d) -> p h d", h=4), qmraw[:].rearrange("p (h d) -> p h d", h=4),
                           s8[:, 0:4].unsqueeze(2).to_broadcast([128, 4, 128]), ALU.mult, [qmraw, s8], [qmraw])
                        TT("pool", qmn[:], qmraw[:].rearrange("p (h d) -> p h d", h=4), gqm[:].unsqueeze(1).to_broadcast([128, 4, 128]), ALU.mult, [qmraw, gqm], [qmn])
                        pb = PSa(); pbv = pb[:].bitcast(BF16)
                        for h in range(4):
                            TR(pbv[:, h * 128:(h + 1) * 128], qmn[:, h, :], identb[:], [qmn, identb], [pb])
                        CP("act", qmT[:, :, tsl], pbv[:, 0:512].rearrange("p (h n) -> p h n", h=4), [pb], [qmT])
                        yield 0.4
                        for _ in range(2 if t < 3 else len(my_ids)):
                            if c_next < len(my_ids):
                                c_out(c_next)
                                if c_next + 2 < len(my_ids):
                                    c_in(c_next + 2)
                                c_next += 1
                    fence(dtiles)

                estC = ExitStack()
                fences = []

                def genE(est):
                    pm = [sb("pm%d" % i, [128, 128], BF16, est) for i in range(2)]
                    ymem = sb("ymem", [128, 4, 128], BF16, est)
                    rden = sb("rden", [128, 4], F32, est)
                    pmi = 0
                    for t in range(4):
                        tsl = slice(t * 128, (t + 1) * 128)
                        for hp in range(2):
                            acc = pbank[ps_reserve("A")]
                            for h2 in range(2):
                                h = 2 * hp + h2
                                for mh in range(2):
                                    pb = PSa()
                                    MM(pb[:, 0:128], kmT[:, h, mh * 128:(mh + 1) * 128], qmT[:, h, tsl], True, True, [kmT, qmT], [pb])
                                    p_ = pm[pmi % 2]; pmi += 1
                                    ACT(p_[:], pb[:, 0:128], AF.Exp, [pb], [p_], scale=128 ** -0.5)
                                    yield 0.4
                                    MM(acc[:, h2 * 129:h2 * 129 + 129], p_[:], vmaug[:, mh, h, :], mh == 0, mh == 1, [p_, vmaug], [acc])
                            a3 = acc[:, 0:258].rearrange("p (h d) -> p h d", h=2)
                            op("dve", lambda E: E.reciprocal(out=rden[:, 2 * hp:2 * hp + 2], in_=a3[:, :, 128]), [acc], [rden])
                            TT("dve", ymem[:, 2 * hp:2 * hp + 2, :], a3[:, :, 0:128], rden[:, 2 * hp:2 * hp + 2].unsqueeze(2).to_broadcast([128, 2, 128]),
                               ALU.mult, [acc, rden], [ymem])
                            ps_release("A", pbank.index(acc))
                            yield 0.4
                        pb = PSa(); pbv = pb[:].bitcast(BF16)
                        for h in range(4):
                            TR(pbv[:, h * 128:(h + 1) * 128], ymem[:, h, :], identb[:], [ymem, identb], [pb])
                        CP("act", ymemT[:, :, tsl], pbv[:, 0:512].rearrange("p (h n) -> p h n", h=4), [pb], [ymemT])
                        yield 0.4
                    fences.append(pm + [ymem, rden])

                def genF(est, tiles):
                    isc = sb("isc", [128, SEQ], F32, est)
                    mneg = sb("mneg", [128, SEQ], BF16, est)
                    rl = [sb("rl%d" % i, [128, 512], F32, est) for i in range(2)]
                    PT = [sb("PT%d" % i, [128, 1024], BF16, est) for i in range(2)]
                    m8 = sb("m8", [128, 8], F32, est)
                    ydsa = sb("ydsa", [128, 8, 64], BF16, est)
                    rden8 = sb("rden8", [128, 8], F32, est)
                    oacc = sb("oacc", [128, 2, 260], F32, est)
                    rli = 0; pti = 0
                    for t in tiles:
                        gt_ = st * 4 + t
                        nk = (gt_ + 1) * 128
                        tsl = slice(t * 128, (t + 1) * 128)
                        if gt_ >= 2:
                            for c0 in range(0, nk, 512):
                                cw = min(512, nk - c0)
                                for h in range(4):
                                    pb = PSa()
                                    MM(pb[:, 0:cw], qiT[:, h, tsl], kiT[:, c0:c0 + cw], True, True, [qiT, kiT], [pb])
                                    r_ = rl[rli % 2]; rli += 1
                                    ACT(r_[:, 0:cw], pb[:, 0:cw], AF.Relu, [pb], [r_])
                                    yield 0.4
                                    if h == 0:
                                        TS("dve", isc[:, c0:c0 + cw], r_[:, 0:cw], wis[:, t, 0:1], None, ALU.mult, None, [r_, wis], [isc])
                                    else:
                                        STT(isc[:, c0:c0 + cw], r_[:, 0:cw], wis[:, t, h:h + 1], isc[:, c0:c0 + cw], ALU.mult, ALU.add, [r_, wis, isc], [isc])
                                yield 0.4
                            TT("dve", isc[:, nk - 128:nk], isc[:, nk - 128:nk], causal[:], ALU.add, [isc, causal], [isc])
                            for r in range(32):
                                op("dve", lambda E: E.max(out=m8[:], in_=isc[:, 0:nk]), [isc], [m8])
                                yield nk / 960.0
                                op("dve", lambda E: E.match_replace(out=isc[:, 0:nk], in_to_replace=m8[:], in_values=isc[:, 0:nk], imm_value=-3.0e38), [isc, m8], [isc])
                                yield nk / 960.0
                            TS("dve", mneg[:, 0:nk], isc[:, 0:nk], -1.0e35, -30000.0, ALU.is_gt, ALU.mult, [isc], [mneg])
                            yield 0.4
                        for j in range(gt_ + 1):
                            ksl = slice(j * 128, (j + 1) * 128)
                            need_mask = (gt_ >= 2) or (j == gt_)
                            p_ = PT[pti % 2]; pti += 1
                            for half in range(2):
                                pb = PSa()
                                MM(pb[:], kT[:, ksl], qT[:, 4 * half:4 * half + 4, tsl], True, not need_mask, [kT, qT], [pb])
                                if need_mask:
                                    ml = mneg[:, ksl] if gt_ >= 2 else causalb[:]
                                    MM(pb[:], ml, irep[:], False, True, [mneg, causalb, irep], [pb])
                                ACT(p_[:, half * 512:(half + 1) * 512], pb[:], AF.Exp, [pb], [p_], scale=0.125)
                                yield 0.4
                            for hp in range(2):
                                acc = PSa()
                                for h4 in range(4):
                                    h = 4 * hp + h4
                                    MM(acc[:, h4 * 65:h4 * 65 + 65], p_[:, h * 128:(h + 1) * 128], vaug[:, j, :], True, True, [p_, vaug], [acc])
                                if j == 0:
                                    CP("act", oacc[:, hp, :], acc[:, 0:260], [acc], [oacc])
                                    yield 0.4
                                else:
                                    TT("dve", oacc[:, hp, :], oacc[:, hp, :], acc[:, 0:260], ALU.add, [acc, oacc], [oacc])
                            yield 0.4
                        a3 = oacc[:].rearrange("p a (h d) -> p (a h) d", h=4)
                        op("dve", lambda E: E.reciprocal(out=rden8[:], in_=a3[:, :, 64]), [oacc], [rden8])
                        TT("dve", ydsa[:], a3[:, :, 0:64], rden8[:].unsqueeze(2).to_broadcast([128, 8, 64]), ALU.mult, [oacc, rden8], [ydsa])
                        pb = PSa(); pbv = pb[:].bitcast(BF16)
                        for c in range(4):
                            TR(pbv[:, c * 128:(c + 1) * 128], ydsa[:, 2 * c:2 * c + 2, :].rearrange("p h d -> p (h d)"), identb[:], [ydsa, identb], [pb])
                        CP("act", ydsaT[:, :, tsl], pbv[:, 0:512].rearrange("p (c n) -> p c n", c=4), [pb], [ydsaT])
                        yield 0.4
                    fences.append([isc, mneg, m8, ydsa, rden8, oacc] + rl + PT)

                yg = sb("yg", [128, 4, 512], BF16, estC)
                sz = sb("sz", [128, 512], BF16, estC)

                def gluG():
                    sl, w, hh = ringA.get(C_GLU, 4, 512)
                    for fo in range(4):
                        pb = PSa()
                        for fc in range(4):
                            MM(pb[:], w[:, fc, fo * 128:(fo + 1) * 128], yg[:, fc, :], fc == 0, fc == 3, [sl, yg], [pb])
                        ACT(sz[:], pb[:], AF.Sigmoid, [pb], [sz])
                        yield 0.4
                        TT("dve", yssmT[:, fo, :], yg[:, fo, :], sz[:], ALU.mult, [yg, sz], [yssmT])
                        yield 0.4
                    ringA.rel(hh)

                def genG(est, fcs, do_glu):
                    sets = []
                    t1_ = sb("t1", [128, 4, 128], F32, est); t2_ = sb("t2", [128, 4, 128], F32, est)
                    t3_ = sb("t3", [128, 4, 128], F32, est); t4_ = sb("t4", [128, 4, 128], F32, est)
                    for i_ in range(1):
                        sets.append(dict(
                            wre=sb("wre", [128, 4, 128], F32, est), wim=sb("wim", [128, 4, 128], F32, est),
                            t1=t1_, t2=t2_, t3=t3_, t4=t4_,
                            wsr=sb("wsr", [128, 4, 128], F32, est), wsi=sb("wsi", [128, 4, 128], F32, est),
                            sre=sb("sre", [128, 4, 128], BF16, est), sim=sb("sim", [128, 4, 128], BF16, est),
                            ini=sb("ini", [128, 2], F32, est), tiny=sb("tiny", [128, 2], F32, est)))
                    ypre = sb("ypre", [128, 512], F32, est)
                    for fc in fcs:
                        ypb = pbank[ps_reserve("A")]
                        for r in range(4):
                            pc = 4 * fc + r
                            S_ = sets[pc % len(sets)]
                            wre, wim, t1, t2, t3, t4 = S_["wre"], S_["wim"], S_["t1"], S_["t2"], S_["t3"], S_["t4"]
                            wsr, wsi, sre, sim, ini, tiny = S_["wsr"], S_["wsi"], S_["sre"], S_["sim"], S_["ini"], S_["tiny"]
                            psl = slice(32 * r, 32 * r + 32) if r < 3 else slice(64, 128)
                            osl = slice(64 * (r // 2), 64 * (r // 2) + 64)
                            pre = PSa(); pim = PSa()
                            MM(pre[:], BTre[psl, pc, :], uT[psl, fc, :], True, True, [BTre, uT], [pre])
                            MM(pim[:], BTim[psl, pc, :], uT[psl, fc, :], True, True, [BTim, uT], [pim])
                            Cb = tabc[:, pc:pc + 1, :].to_broadcast([128, 4, 128]); Sb = tabs[:, pc:pc + 1, :].to_broadcast([128, 4, 128])
                            pre3 = pre[:].rearrange("p (k n) -> p k n", k=4); pim3 = pim[:].rearrange("p (k n) -> p k n", k=4)
                            TT("dve", t1[:], pre3, Cb, ALU.mult, [pre, tabc], [t1])
                            TT("dve", t2[:], pim3, Sb, ALU.mult, [pim, tabs], [t2])
                            TT("dve", wre[:], t1[:], t2[:], ALU.add, [t1, t2], [wre])
                            TT("dve", t1[:], pim3, Cb, ALU.mult, [pim, tabc], [t1])
                            TT("dve", t2[:], pre3, Sb, ALU.mult, [pre, tabs], [t2])
                            TT("dve", wim[:], t1[:], t2[:], ALU.subtract, [t1, t2], [wim])
                            yield 3.0
                            magb = s5_mag[:, pc:pc + 1].to_broadcast([128, 128])
                            for k in range(4):
                                if k == 0:
                                    i_re = carr[:, pc, 0:1]; i_im = carr[:, pc, 1:2]; ib = carr
                                else:
                                    i_re = ini[:, 0:1]; i_im = ini[:, 1:2]; ib = ini
                                op("dve", lambda E: E.tensor_tensor_scan(out=wsr[:, k, :], data0=magb, data1=wre[:, k, :], initial=i_re, op0=ALU.mult, op1=ALU.add), [s5_mag, wre, ib], [wsr])
                                op("dve", lambda E: E.tensor_tensor_scan(out=wsi[:, k, :], data0=magb, data1=wim[:, k, :], initial=i_im, op0=ALU.mult, op1=ALU.add), [s5_mag, wim, ib], [wsi])
                                dst = ini if k < 3 else carr
                                d_re = ini[:, 0:1] if k < 3 else carr[:, pc, 0:1]
                                d_im = ini[:, 1:2] if k < 3 else carr[:, pc, 1:2]
                                TS("dve", tiny[:, 0:1], wsr[:, k, 127:128], s5_c128[:, pc:pc + 1], None, ALU.mult, None, [wsr, s5_c128], [tiny])
                                TS("dve", tiny[:, 1:2], wsi[:, k, 127:128], s5_c128[:, pc:pc + 1], None, ALU.mult, None, [wsi, s5_c128], [tiny])
                                STT(d_re, wsi[:, k, 127:128], s5_s128n[:, pc:pc + 1], tiny[:, 0:1], ALU.mult, ALU.add, [wsi, s5_s128n, tiny], [dst])
                                STT(d_im, wsr[:, k, 127:128], s5_s128[:, pc:pc + 1], tiny[:, 1:2], ALU.mult, ALU.add, [wsr, s5_s128, tiny], [dst])
                                yield 1.5
                            TT("pool", t3[:], wsr[:], Cb, ALU.mult, [wsr, tabc], [t3])
                            TT("pool", t4[:], wsi[:], Sb, ALU.mult, [wsi, tabs], [t4])
                            TT("pool", sre[:], t3[:], t4[:], ALU.subtract, [t3, t4], [sre])
                            TT("pool", t3[:], wsi[:], Cb, ALU.mult, [wsi, tabc], [t3])
                            TT("pool", t4[:], wsr[:], Sb, ALU.mult, [wsr, tabs], [t4])
                            TT("pool", sim[:], t3[:], t4[:], ALU.add, [t3, t4], [sim])
                            yield 0.4
                            MM(ypb[osl, :], Cwre[:, pc, :], sre[:].rearrange("p k n -> p (k n)"), r % 2 == 0, False, [Cwre, sre], [ypb])
                            MM(ypb[osl, :], Cwim[:, pc, :], sim[:].rearrange("p k n -> p (k n)"), False, r % 2 == 1, [Cwim, sim], [ypb])
                            yield 5.0
                        STT(ypre[:], uT[:, fc, :], dsk[:, fc:fc + 1], ypb[:], ALU.mult, ALU.add, [uT, dsk, ypb], [ypre])
                        ACT(yg[:, fc, :], ypre[:], AF.Gelu_apprx_tanh, [ypre], [yg])
                        yield 0.4
                        ps_release("A", pbank.index(ypb))
                    if do_glu:
                        yield from gluG()
                    fences.append([x_ for S2 in sets for x_ in S2.values()] + [ypre])

                fences.append([yg, sz])
                if st == 0:
                    gens = [genG(estC, (0, 2), False), genG(estC, (1, 3), False), genF(estC, (0, 1, 2, 3)), genE(estC)]
                else:
                    gens = [genG(estC, (0, 1, 2, 3), True), genF(estC, (0, 2)), genF(estC, (1, 3)), genE(estC)]
                acc_t = [0.0] * len(gens)
                live = [True] * len(gens)
                while any(live):
                    gi = min((k_ for k_ in range(len(gens)) if live[k_]), key=lambda k_: acc_t[k_])
                    try:
                        w_ = next(gens[gi])
                        acc_t[gi] += (w_ if w_ is not None else 0.4)
                    except StopIteration:
                        live[gi] = False
                if st == 0:
                    for _ in gluG():
                        pass
                for f_ in fences:
                    fence(f_)
                estC.close()
                fence([qT, qiT, qmT, wis])
                e2b.close()
                fence([uT])
                e2a.close()
                yield 0.4

            def stage_H(b, st, A):
                tok0 = st * 512
                hT, ydsaT, ymemT, yssmT = A["hT"], A["ydsaT"], A["ymemT"], A["yssmT"]
                PSa = lambda: PSs("A")
                esx = ExitStack()
                Xs = sb("Xs", [128, 4, D], F32, esx)
                with ExitStack() as est:
                    gsb = sb("gsb", [128, 4, 1024], BF16, est)
                    mg = sb("mg", [128, 4, D], F32, est)
                    mgb = sb("mgb", [128, 4, D], BF16, est)
                    mT = sb("mT", [128, 4, 8, 128], BF16, est)
                    fw.dma(Xs[:], x_d[b, tok0:tok0 + 512, :].rearrange("(t p) d -> p t d", p=128), writes=XsAll, q=QA)
                    for bi, (cid, yT_) in enumerate(((C_UPS, yssmT), (C_UPD, ydsaT), (C_UPM, ymemT))):
                        for i2 in range(2):
                            sl, w, hh = ringA.get(C_G0 + 2 * bi + i2, 8, 512)
                            for t in range(4):
                                tsl = slice(t * 128, (t + 1) * 128)
                                pb = PSa()
                                for kc in range(8):
                                    MM(pb[:], hT[:, kc, tsl], w[:, kc, :], kc == 0, kc == 7, [hT, sl], [pb])
                                ACT(gsb[:, t, i2 * 512:(i2 + 1) * 512], pb[:], AF.Sigmoid, [pb], [gsb])
                            ringA.rel(hh)
                        sl, w, hh = ringA.get(cid, 4, 1024)
                        for t in range(4):
                            tsl = slice(t * 128, (t + 1) * 128)
                            for half in range(2):
                                pb = PSa()
                                for fc in range(4):
                                    MM(pb[:], yT_[:, fc, tsl], w[:, fc, half * 512:(half + 1) * 512], fc == 0, fc == 3, [yT_, sl], [pb])
                                gsl = gsb[:, t, half * 512:(half + 1) * 512]
                                msl = mg[:, t, half * 512:(half + 1) * 512]
                                if bi == 0:
                                    TT("dve", msl, pb[:], gsl, ALU.mult, [pb, gsb], [mg])
                                else:
                                    TT("dve", pb[:], pb[:], gsl, ALU.mult, [pb, gsb], [pb])
                                    if bi == 1:
                                        TT("dve", msl, msl, pb[:], ALU.add, [pb, mg], [mg])
                                    else:
                                        TT("dve", mgb[:, t, half * 512:(half + 1) * 512], msl, pb[:], ALU.add, [pb, mg], [mgb])
                        ringA.rel(hh)
                    for t in range(4):
                        pb = PSa(); pbv = pb[:].bitcast(BF16)
                        for kc in range(8):
                            TR(pbv[:, kc * 128:(kc + 1) * 128], mgb[:, t, kc * 128:(kc + 1) * 128], identb[:], [mgb, identb], [pb])
                        CP("act", mT[:, t, :, :], pbv[:, 0:1024].rearrange("p (k n) -> p k n", k=8), [pb], [mT])
                    for half, cid in enumerate((C_OUT0, C_OUT1)):
                        sl, w, hh = ringA.get(cid, 8, 512)
                        for t in range(4):
                            pb = PSa()
                            for kc in range(8):
                                MM(pb[:], mT[:, t, kc, :], w[:, kc, :], kc == 0, kc == 7, [mT, sl], [pb])
                            xs_ = Xs[:, t, half * 512:(half + 1) * 512]
                            TT("dve", xs_, xs_, pb[:], ALU.add, [XsB[t][half], pb], [XsB[t][half]])
                        ringA.rel(hh)
                    fw.barrier()
                with ExitStack() as est:
                    hn = sb("hn", [128, D], F32, est); hnb = sb("hnb", [128, D], BF16, est)
                    hnb2 = [sb("hnb2_%d" % i_, [128, D], BF16, est) for i_ in range(2)]
                    ohs = sb("ohs", [128, 32], F32, est); rk = sb("rk", [128, 32], F32, est); rkm = sb("rkm", [128, 32], F32, est)
                    hnT32 = sb("hnT32", [128, 8, 128], F32, est)
                    rst2 = sb("rst2", [128, 4], F32, est)
                    lg = sb("lg", [128, 36], F32, est)
                    r4 = sb("r4", [128, 16], F32, est)
                    ohg = sb("ohg", [128, 4], F32, est)
                    le = sb("le", [128, 4, 8], F32, est); el = sb("el", [128, 8], F32, est); ee = sb("ee", [128, 8], F32, est)
                    oh1 = sb("oh1", [128, 8], F32, est); oh2 = sb("oh2", [128, 8], F32, est); e2 = sb("e2", [128, 8], F32, est)
                    for t in range(4):
                        ACT(hnb[:], Xs[:, t, :], AF.Square, XsB[t], [hnb, rst2], accum=rst2[:, t:t + 1])
                    rstd_of(rst2[:], D, [rst2])
                    for t in range(4):
                        tsl = slice(t * 128, (t + 1) * 128)
                        TS("dve", hn[:], Xs[:, t, :], rst2[:, t:t + 1], None, ALU.mult, None, XsB[t] + [rst2], [hn])
                        tt = (b * NST + st) * 4 + t
                        hb_ = hnb2[t % 2]
                        CP("pool", hb_[:], hn[:], [hn], [hb_])
                        fw.dma(HN[tt * 128:(tt + 1) * 128, :], hb_[:], reads=[hb_], writes=[HNb])
                        for q4 in range(2):
                            pb = PSa()
                            for c in range(4):
                                kc = 4 * q4 + c
                                TR(pb[:, c * 128:(c + 1) * 128], hn[:, kc * 128:(kc + 1) * 128], ident[:], [hn, ident], [pb])
                            TT("dve", hnT32[:, 4 * q4:4 * q4 + 4, :], pb[:].rearrange("p (k n) -> p k n", k=4),
                               gffnT[:, 4 * q4:4 * q4 + 4].unsqueeze(2).to_broadcast([128, 4, 128]), ALU.mult, [pb, gffnT], [hnT32])
                        pb = PSa()
                        for kc in range(8):
                            MM(pb[:, 0:36], hnT32[:, kc, :], wr32[:, kc, :], kc == 0, kc == 7, [hnT32, wr32], [pb])
                        TT("dve", lg[:], pb[:, 0:36], rbias[:], ALU.add, [pb, rbias], [lg])
                        op("dve", lambda E: E.tensor_reduce(out=r4[:, 0:1], in_=lg[:, 0:4], axis=AX.X, op=ALU.max), [lg], [r4])
                        TS("dve", ohg[:], lg[:, 0:4], r4[:, 0:1], None, ALU.is_equal, None, [lg, r4], [ohg])
                        TS("dve", r4[:, 1:2], r4[:, 0:1], -1.0, None, ALU.mult, None, [r4], [r4])
                        ACT(r4[:, 4:8], lg[:, 0:4], AF.Exp, [lg, r4], [r4], bias=r4[:, 1:2])
                        op("dve", lambda E: E.tensor_reduce(out=r4[:, 2:3], in_=r4[:, 4:8], axis=AX.X, op=ALU.add), [r4], [r4])
                        op("dve", lambda E: E.reciprocal(out=r4[:, 2:3], in_=r4[:, 2:3]), [r4], [r4])
                        TT("dve", le[:], lg[:, 4:36].rearrange("p (g e) -> p g e", g=4), ohg[:].unsqueeze(2).to_broadcast([128, 4, 8]), ALU.mult, [lg, ohg], [le])
                        op("dve", lambda E: E.tensor_reduce(out=el[:], in_=le[:].rearrange("p g e -> p e g"), axis=AX.X, op=ALU.add), [le], [el])
                        op("dve", lambda E: E.tensor_reduce(out=r4[:, 3:4], in_=el[:], axis=AX.X, op=ALU.max), [el], [r4])
                        TS("dve", r4[:, 8:9], r4[:, 3:4], -1.0, None, ALU.mult, None, [r4], [r4])
                        ACT(ee[:], el[:], AF.Exp, [el, r4], [ee], bias=r4[:, 8:9])
                        op("dve", lambda E: E.tensor_reduce(out=r4[:, 9:10], in_=ee[:], axis=AX.X, op=ALU.max), [ee], [r4])
                        TS("dve", oh1[:], ee[:], r4[:, 9:10], None, ALU.is_equal, None, [ee, r4], [oh1])
                        STT(e2[:], oh1[:], -4.0, ee[:], ALU.mult, ALU.add, [oh1, ee], [e2])
                        op("dve", lambda E: E.tensor_reduce(out=r4[:, 10:11], in_=e2[:], axis=AX.X, op=ALU.max), [e2], [r4])
                        TS("dve", oh2[:], e2[:], r4[:, 10:11], None, ALU.is_equal, None, [e2, r4], [oh2])
                        TT("dve", r4[:, 11:12], r4[:, 9:10], r4[:, 10:11], ALU.add, [r4], [r4])
                        op("dve", lambda E: E.reciprocal(out=r4[:, 11:12], in_=r4[:, 11:12]), [r4], [r4])
                        TT("dve", r4[:, 11:12], r4[:, 11:12], r4[:, 2:3], ALU.mult, [r4], [r4])
                        TT("dve", r4[:, 12:13], r4[:, 9:10], r4[:, 11:12], ALU.mult, [r4], [r4])
                        TT("dve", r4[:, 13:14], r4[:, 10:11], r4[:, 11:12], ALU.mult, [r4], [r4])
                        CP("dve", W12[:, tt, :], r4[:, 12:14], [r4], [W12])
                        TT("dve", OH1[:, tt, :].rearrange("p (g e) -> p g e", g=4), oh1[:].unsqueeze(1).to_broadcast([128, 4, 8]),
                           ohg[:].unsqueeze(2).to_broadcast([128, 4, 8]), ALU.mult, [oh1, ohg], [OH1])
                        TT("dve", OH2[:, tt, :].rearrange("p (g e) -> p g e", g=4), oh2[:].unsqueeze(1).to_broadcast([128, 4, 8]),
                           ohg[:].unsqueeze(2).to_broadcast([128, 4, 8]), ALU.mult, [oh2, ohg], [OH2])
                        TT("dve", ohs[:], OH1[:, tt, :], OH2[:, tt, :], ALU.add, [OH1, OH2], [ohs])
                        pb = PSa()
                        MM(pb[:, 0:32], ltri[:], ohs[:], True, True, [ltri, ohs], [pb])
                        MM(pb[:, 32:64], ones[:], ohs[:], True, True, [ones, ohs], [pb])
                        TT("dve", rk[:], pb[:, 0:32], run[:], ALU.add, [pb, run], [rk])
                        TT("dve", run[:], run[:], pb[:, 32:64], ALU.add, [pb, run], [run])
                        TT("dve", rkm[:], rk[:], OH1[:, tt, :], ALU.mult, [rk, OH1], [rkm])
                        op("dve", lambda E: E.tensor_reduce(out=RNK[:, tt, 0:1], in_=rkm[:], axis=AX.X, op=ALU.add), [rkm], [RNK])
                        TT("dve", rkm[:], rk[:], OH2[:, tt, :], ALU.mult, [rk, OH2], [rkm])
                        op("dve", lambda E: E.tensor_reduce(out=RNK[:, tt, 1:2], in_=rkm[:], axis=AX.X, op=ALU.add), [rkm], [RNK])
                    g0 = (b * NST + st) * 512
                    fw.dma(XM[g0:g0 + 512, :].rearrange("(t p) d -> p t d", p=128), Xs[:], reads=XsAll, writes=[XMb])
                    fw.barrier()
                esx.close()

            def drive(ga):
                for _ in ga:
                    pass

            for b in range(NB):
                batch_prep(b)
                for st in range(NST):
                    with ExitStack() as esa:
                        A = {
                            "hT": sb("hT", [128, 8, 512], BF16, esa),
                            "ydsaT": sb("ydsaT", [128, 4, 512], BF16, esa), "ymemT": sb("ymemT", [128, 4, 512], BF16, esa),
                            "yssmT": sb("yssmT", [128, 4, 512], BF16, esa), "rst": sb("rst", [128, 4], F32, esa),
                        }
                        drive(attn_AG(b, st, A, esa))
                        fw.barrier()
                        stage_H(b, st, A)
                    fw.barrier()
            fw.barrier()
            esP1.close()

            PS8 = lambda: PSs("A")
            with ExitStack() as e15:
                cmp = sb("cmp", [128, 32, 64], F32, e15)
                ltc = sb("ltc", [128, 1024], F32, e15)
                fw.dma(ltc[:], ltc_d.partition_broadcast(128), writes=[ltc])
                kt = sb("kt", [128, 32], F32, e15); tsv = sb("tsv", [128, 32], F32, e15); te = sb("te", [128, 32], F32, e15)
                ts128 = sb("ts128", [128, 32], F32, e15)
                cm2 = sb("cm2", [128, 32, 32], F32, e15)
                msk = sb("msk", [128, NTILE, 32], F32, e15)
                texp = sb("texp", [128, NTILE], F32, e15); wf = sb("wf", [128, NTILE, 3], F32, e15)
                big = sb("big", [128, NTT, 32], F32, e15); dpf = sb("dpf", [128, NTT], F32, e15)
                TT("dve", cmp[:], run[:].unsqueeze(2).to_broadcast([128, 32, 64]), thr[:].unsqueeze(1).to_broadcast([128, 32, 64]), ALU.is_gt, [run, thr], [cmp])
                op("dve", lambda E: E.tensor_reduce(out=kt[:], in_=cmp[:], axis=AX.X, op=ALU.add), [cmp], [kt])
                TT("dve", cm2[:], ltc[:].rearrange("p (a c) -> p a c", a=32), kt[:].unsqueeze(1).to_broadcast([128, 32, 32]), ALU.mult, [ltc, kt], [cm2])
                op("dve", lambda E: E.tensor_reduce(out=tsv[:], in_=cm2[:], axis=AX.X, op=ALU.add), [cm2], [tsv])
                TT("dve", te[:], tsv[:], kt[:], ALU.add, [tsv, kt], [te])
                TS("dve", ts128[:], tsv[:], 128.0, None, ALU.mult, None, [tsv], [ts128])
                TT("dve", msk[:], te[:].unsqueeze(1).to_broadcast([128, NTILE, 32]), iot[:].unsqueeze(2).to_broadcast([128, NTILE, 32]), ALU.is_le, [te, iot], [msk])
                op("dve", lambda E: E.tensor_reduce(out=texp[:], in_=msk[:], axis=AX.X, op=ALU.add), [msk], [texp])
                TS("dve", texp[:], texp[:], 31.0, None, ALU.min, None, [texp], [texp])
                for j in range(3):
                    TS("dve", wf[:, :, j], texp[:], 384.0, float((C_EXP + j) * 128), ALU.mult, ALU.add, [texp], [wf])
                TS("dve", wf[:], wf[:], pcol[:, 0:1], None, ALU.add, None, [wf, pcol], [wf])
                CP("dve", widx[:], wf[:], [wf], [widx])
                for c, OH in enumerate((OH1, OH2)):
                    TT("dve", big[:], OH[:], ts128[:].unsqueeze(1).to_broadcast([128, NTT, 32]), ALU.mult, [OH, ts128], [big])
                    op("dve", lambda E: E.tensor_reduce(out=dpf[:], in_=big[:], axis=AX.X, op=ALU.add), [big], [dpf])
                    TT("dve", dpf[:], dpf[:], RNK[:, :, c], ALU.add, [dpf, RNK], [dpf])
                    CP("dve", dpi[c][:], dpf[:], [dpf], [dpi[c]])
                hnl = [sb("hnl%d" % i_, [128, D], BF16, e15) for i_ in range(4)]
                for tt in range(NTT):
                    h_ = hnl[tt % 4]
                    fw.dma(h_[:], HN[tt * 128:(tt + 1) * 128, :], reads=[HNb], writes=[h_])
                    for c in range(2):
                        fw.idma(HS, bass.IndirectOffsetOnAxis(ap=dpi[c][:, tt:tt + 1], axis=0), h_[:], None, NTILE * 128 - 1,
                                reads=[h_, dpi[c]], writes=[HSb])
                fw.barrier()
            fw.barrier()

            wscf = wsc.rearrange("c p n -> (c p) n")
            with ExitStack() as e2:
                wgu = [[ring[0], ring[1]]] + [[sb("wgu%d_%d" % (i_, j_), [128, 4096], BF16, e2) for j_ in range(2)] for i_ in range(2)]
                wdn = [sb("wdn%d" % i_, [128, 4096], BF16, e2) for i_ in range(3)]
                hsl = [sb("hsl%d" % i_, [128, D], BF16, e2) for i_ in range(3)]
                hsT = [sb("hsT%d" % i_, [128, 8, 128], BF16, e2) for i_ in range(2)]
                sg2 = [sb("sg2_%d" % i_, [128, 512], F32, e2) for i_ in range(2)]
                hidb = [sb("hidb%d" % i_, [128, 512], BF16, e2) for i_ in range(2)]
                hidT = [sb("hidT%d" % i_, [128, 4, 128], BF16, e2) for i_ in range(2)]
                ysb = [sb("ysb%d" % i_, [128, D], F32, e2) for i_ in range(3)]

                def load_gu(i):
                    for j in range(2):
                        sl = wgu[i % 3][j]
                        fw.idma(sl[:], None, wscf, bass.IndirectOffsetOnAxis(ap=widx[:, i, j:j + 1], axis=0), NCH * 128 - 1, reads=[widx], writes=[sl])

                def load_dn(i):
                    sl = wdn[i % 3]
                    fw.idma(sl[:], None, wscf, bass.IndirectOffsetOnAxis(ap=widx[:, i, 2:3], axis=0), NCH * 128 - 1, reads=[widx], writes=[sl])

                def load_hs(i):
                    fw.dma(hsl[i % 3][:], HS[i * 128:(i + 1) * 128, :], reads=[HSb], writes=[hsl[i % 3]])

                def S1(i):
                    h_ = hsl[i % 3]; hT_ = hsT[i % 2]
                    pb = PS8(); pbv = pb[:].bitcast(BF16)
                    for kc in range(8):
                        TR(pbv[:, kc * 128:(kc + 1) * 128], h_[:, kc * 128:(kc + 1) * 128], identb[:], [h_, identb], [pb])
                    TT("dve", hT_[:], pbv[:, 0:1024].rearrange("p (k n) -> p k n", k=8),
                       gffnT[:].unsqueeze(2).to_broadcast([128, 8, 128]), ALU.mult, [pb, gffnT], [hT_])

                def S2(i):
                    hT_ = hsT[i % 2]
                    slg, slu = wgu[i % 3]
                    wg = slg[:].rearrange("p (k n) -> p k n", k=8); wu = slu[:].rearrange("p (k n) -> p k n", k=8)
                    pg = PS8(); pu = PS8()
                    for kc in range(8):
                        MM(pg[:], hT_[:, kc, :], wg[:, kc, :], kc == 0, kc == 7, [hT_, slg], [pg])
                    for kc in range(8):
                        MM(pu[:], hT_[:, kc, :], wu[:, kc, :], kc == 0, kc == 7, [hT_, slu], [pu])
                    ACT(sg2[i % 2][:], pg[:], AF.Silu, [pg], [sg2[i % 2]])
                    TT("dve", hidb[i % 2][:], pu[:], sg2[i % 2][:], ALU.mult, [pu, sg2[i % 2]], [hidb[i % 2]])

                def S3(i):
                    pb = PS8(); pbv = pb[:].bitcast(BF16)
                    for fc in range(4):
                        TR(pbv[:, fc * 128:(fc + 1) * 128], hidb[i % 2][:, fc * 128:(fc + 1) * 128], identb[:], [hidb[i % 2], identb], [pb])
                    CP("act", hidT[i % 2][:], pbv[:, 0:512].rearrange("p (k n) -> p k n", k=4), [pb], [hidT[i % 2]])

                def S4(i):
                    sld = wdn[i % 3]
                    wd = sld[:].rearrange("p (k n) -> p k n", k=4)
                    y_ = ysb[i % 3]
                    for half in range(2):
                        po = PS8()
                        for fc in range(4):
                            MM(po[:], hidT[i % 2][:, fc, :], wd[:, fc, half * 512:(half + 1) * 512], fc == 0, fc == 3, [hidT[i % 2], sld], [po])
                        CP("act" if half == 0 else "dve", y_[:, half * 512:(half + 1) * 512], po[:], [po], [y_])
                    fw.dma(YS[i * 128:(i + 1) * 128, :], y_[:], reads=[y_], writes=[YSb])

                for i in range(min(3, NTILE)):
                    load_hs(i); load_gu(i); load_dn(i)
                S1(0)
                if NTILE > 3:
                    load_hs(3)
                for i in range(NTILE + 1):
                    if i < NTILE:
                        S2(i)
                        if i + 3 < NTILE:
                            load_gu(i + 3)
                    if i + 1 < NTILE:
                        S1(i + 1)
                        if i + 4 < NTILE:
                            load_hs(i + 4)
                    if i >= 1:
                        S4(i - 1)
                        if i + 2 < NTILE:
                            load_dn(i + 2)
                    if i < NTILE:
                        S3(i)
                fw.barrier()
            fw.barrier()

            out_f = out_d.rearrange("b s d -> (b s) d")
            with ExitStack() as e3:
                xm = [sb("xm%d" % i_, [128, D], F32, e3) for i_ in range(3)]
                y1 = [sb("y1_%d" % i_, [128, D], F32, e3) for i_ in range(3)]
                y2 = [sb("y2_%d" % i_, [128, D], F32, e3) for i_ in range(3)]
                for tt in range(NTT):
                    k_ = tt % 3
                    fw.dma(xm[k_][:], XM[tt * 128:(tt + 1) * 128, :], reads=[XMb], writes=[xm[k_]])
                    fw.idma(y1[k_][:], None, YS, bass.IndirectOffsetOnAxis(ap=dpi[0][:, tt:tt + 1], axis=0), NTILE * 128 - 1, reads=[YSb, dpi[0]], writes=[y1[k_]])
                    fw.idma(y2[k_][:], None, YS, bass.IndirectOffsetOnAxis(ap=dpi[1][:, tt:tt + 1], axis=0), NTILE * 128 - 1, reads=[YSb, dpi[1]], writes=[y2[k_]])
                    STT(xm[k_][:], y1[k_][:], W12[:, tt, 0:1], xm[k_][:], ALU.mult, ALU.add, [y1[k_], W12, xm[k_]], [xm[k_]])
                    STT(xm[k_][:], y2[k_][:], W12[:, tt, 1:2], xm[k_][:], ALU.mult, ALU.add, [y2[k_], W12, xm[k_]], [xm[k_]])
                    fw.dma(out_f[tt * 128:(tt + 1) * 128, :], xm[k_][:], reads=[xm[k_]], q="act")
                fw.barrier()
        except _Stop:
            pass
        fw.barrier()
        print("[build] instrs=%d waits=%d" % (fw.ninstr, fw.nwaits))
    return nc, dbg_t


def _consts():
    ident = np.eye(128, dtype=np.float32)
    q = np.arange(128)[:, None]; k = np.arange(128)[None, :]
    causal = np.where(k <= q, 0.0, -1.0e30).astype(np.float32)
    jj = np.broadcast_to(np.arange(1, 129, dtype=np.float32)[None, :], (128, 128)).copy()
    invf = (1.0 / (np.float32(10000.0) ** (np.arange(0, 64, 2, dtype=np.float32) / np.float32(64.0)))).astype(np.float32)[None, :]
    par = np.zeros((128, 2), np.float32)
    g8 = np.arange(128) // 16
    par[:, 0] = (g8 % 2 == 0); par[:, 1] = (g8 % 2 == 1)
    ltri = (np.arange(128)[:, None] < np.arange(128)[None, :]).astype(np.float32)
    thr = (128.0 * np.arange(64, dtype=np.float32))[None, :]
    lt = (np.arange(32)[None, :] < np.arange(32)[:, None]).astype(np.float32).reshape(1, 1024)
    iot = np.arange(NTILE, dtype=np.float32)[None, :]
    pcol = np.arange(128, dtype=np.float32)[:, None]
    return {"c_ident": ident, "c_causal": causal, "c_jj": jj, "c_invf": invf, "c_par": par,
            "c_ltri": ltri, "c_thr": thr, "c_lt": lt, "c_iot": iot, "c_pcol": pcol}


def _in_maps(inputs, NB, ncores):
    f = lambda a: np.ascontiguousarray(a)
    shared = {
        "g_mix": f(inputs["g_mix"]), "g_mem": f(inputs["g_mem"]), "g_ffn": f(inputs["g_ffn"]),
        "w_in": f(inputs["w_in"][0]), "lam_re": f(inputs["lam_re"][0]), "lam_im": f(inputs["lam_im"][0]), "log_dt": f(inputs["log_dt"]),
        "b_re": f(inputs["b_re"][0]), "b_im": f(inputs["b_im"][0]),
        "c_re": f(inputs["c_re"][0].reshape(512, 64)), "c_im": f(inputs["c_im"][0].reshape(512, 64)),
        "d_skip": f(inputs["d_skip"]), "w_glu": f(inputs["w_glu"][0]),
        "g_q": f(inputs["g_q"]), "g_k": f(inputs["g_k"]), "g_qm": f(inputs["g_qm"]), "g_km": f(inputs["g_km"]),
        "w_mem_kv": f(inputs["w_mem_kv"][0]), "w_up_ssm": f(inputs["w_up_ssm"][0]), "w_up_dsa": f(inputs["w_up_dsa"][0]),
        "w_up_mem": f(inputs["w_up_mem"][0]), "w_out": f(inputs["w_out"][0]),
        "w_group": f(inputs["w_group"][0]), "b_group": f(inputs["b_group"]), "w_expert": f(inputs["w_expert"][0]),
        "b_expert": f(inputs["b_expert"][0].reshape(1, 32)),
        "w_gate": f(inputs["w_gate"][0]), "w_up": f(inputs["w_up"][0]), "w_down": f(inputs["w_down"][0]),
    }
    shared.update(_consts())
    maps = []
    for c in range(ncores):
        m = dict(shared)
        m["x"] = f(inputs["x"][c * NB:(c + 1) * NB])
        m["mem"] = f(inputs["mem"][c * NB:(c + 1) * NB])
        m["positions"] = f(inputs["positions"][c * NB:(c + 1) * NB].astype(np.int32))
        maps.append(m)
    return maps


def kernel(**inputs):
    NB = 4
    nc, _ = build_program(NB=NB, NST=4, NEXP=32)
    maps = _in_maps(inputs, NB, NCORES)
    res = run_bass_kernel_spmd(nc, maps, core_ids=list(range(NCORES)))
    out = np.concatenate([np.asarray(r["out"]) for r in res.results], axis=0)
    return out.astype(np.float32, copy=False)
```

```python
import math
from os import environ as _os_env
import numpy as np
from contextlib import ExitStack
import concourse.bass as bass
import concourse.mybir as mybir
from concourse.bass_utils import run_bass_kernel_spmd

F32 = mybir.dt.float32
BF16 = mybir.dt.bfloat16
I32 = mybir.dt.int32
AF = mybir.ActivationFunctionType
ALU = mybir.AluOpType
AX = mybir.AxisListType

NCORES = 8
MOE_SCALE = float(_os_env.get("MOE_SCALE", "0.8"))
QA = "pool"
import os as _os
MOE_SUB = int(_os.environ.get("MOE_SUB", "9"))
D = 1024
SEQ = 2048
MEM = 256
INC = 5060
TWO_PI = 2.0 * math.pi


class Buf:
    __slots__ = ("name", "w", "r")

    def __init__(self, name=""):
        self.name = name
        self.w = None
        self.r = {}


class T:
    def __init__(self, t, name=""):
        self.t = t
        self.b = Buf(name)

    def __getitem__(self, idx):
        return self.t[idx]


def _b(x):
    return x.b if isinstance(x, T) else x


class FW:
    NDMA = 32

    def __init__(self, nc, es):
        self.nc = nc
        self.es = es
        self.eng = {"pe": nc.tensor, "dve": nc.vector, "act": nc.scalar, "pool": nc.gpsimd, "sp": nc.sync}
        self.sem = {k: es.enter_context(nc.semaphore("s_" + k)) for k in self.eng}
        self.cnt = {k: 0 for k in self.eng}
        self.seen = {k: {} for k in self.eng}
        self.dsem = [es.enter_context(nc.semaphore("d%d" % i)) for i in range(self.NDMA)]
        self.dcnt = [0] * self.NDMA
        self.dnext = 0
        self.ninstr = 0
        self.nwaits = 0

    def sb(self, name, shape, dt, es=None):
        self.nalloc = getattr(self, "nalloc", 0) + 1
        return T((es or self.es).enter_context(self.nc.sbuf_tensor("%s_%d" % (name, self.nalloc), list(shape), dt)), name)

    def _wait(self, e, ev):
        sem, val, _ = ev
        key = id(sem)
        if self.seen[e].get(key, 0) >= val:
            return
        self.eng[e].wait_ge(sem, val)
        self.seen[e][key] = val
        self.nwaits += 1

    def _deps(self, e, reads, writes, pe_acc=False):
        for b in reads:
            if b.w is not None:
                self._wait(e, b.w)
        for b in writes:
            if b.w is not None and not (pe_acc and b.w[2] == "pe" and e == "pe"):
                self._wait(e, b.w)
            for ev in b.r.values():
                self._wait(e, ev)

    def _mark(self, ev, reads, writes):
        key = id(ev[0])
        for b in reads:
            old = b.r.get(key)
            if old is None or old[1] < ev[1]:
                b.r[key] = ev
        for b in writes:
            b.w = ev
            b.r = {}

    def op(self, e, fn, reads=(), writes=(), pe_acc=False):
        reads = [_b(x) for x in reads]
        writes = [_b(x) for x in writes]
        self._deps(e, reads, writes, pe_acc)
        ins = fn(self.eng[e])
        self.cnt[e] += 1
        ins.then_inc(self.sem[e], 1)
        self._mark((self.sem[e], self.cnt[e], e), reads, writes)
        self.ninstr += 1

    def dma(self, out, in_, reads=(), writes=(), q="sp", **kw):
        reads = [_b(x) for x in reads]
        writes = [_b(x) for x in writes]
        i = self.dnext
        self.dnext = (self.dnext + 1) % self.NDMA
        sem = self.dsem[i]
        if self.dcnt[i] > 0:
            self._wait(q, (sem, self.dcnt[i], "dma"))
        self._deps(q, reads, writes)
        ins = self.eng[q].dma_start(out=out, in_=in_, **kw)
        self.dcnt[i] += 16
        ins.then_inc(sem, 16)
        self._mark((sem, self.dcnt[i], "dma"), reads, writes)
        self.ninstr += 1

    def idma(self, out, out_off, in_, in_off, bound, reads=(), writes=()):
        q = "pool"
        reads = [_b(x) for x in reads]
        writes = [_b(x) for x in writes]
        i = self.dnext
        self.dnext = (self.dnext + 1) % self.NDMA
        sem = self.dsem[i]
        if self.dcnt[i] > 0:
            self._wait(q, (sem, self.dcnt[i], "dma"))
        self._deps(q, reads, writes)
        ins = self.nc.gpsimd.indirect_dma_start(out=out, out_offset=out_off, in_=in_, in_offset=in_off)
        self.dcnt[i] += 16
        ins.then_inc(sem, 16)
        self._mark((sem, self.dcnt[i], "dma"), reads, writes)
        self.ninstr += 1

    def barrier(self, engines=None):
        for e in (engines or list(self.eng)):
            for i in range(self.NDMA):
                if self.dcnt[i] > 0:
                    self._wait(e, (self.dsem[i], self.dcnt[i], "dma"))
            for k in self.eng:
                if k != e and self.cnt[k] > 0:
                    self._wait(e, (self.sem[k], self.cnt[k], k))


C_U, C_Q, C_KVI, C_QM, C_G0 = 0, 1, 2, 3, 4
C_GLU, C_UPS, C_UPD, C_UPM, C_OUT0, C_OUT1, C_MKV0, C_MKV1, C_EXP = 10, 11, 12, 13, 14, 15, 16, 17, 18
NCH = C_EXP + 96
NSLOT = 2
NTILE = 160


class _Stop(Exception):
    pass


def build_program(NB=4, NST=4, NEXP=32, dbg=(), upto=99):
    nc = bass.Bass("TRN2", target_bir_lowering=False)
    dbg = set(dbg)

    def stage(k):
        if upto >= k:
            with ExitStack() as e_:
                yield e_

    def din(name, shape, dt=F32):
        return nc.dram_tensor(name, list(shape), dt, kind="ExternalInput").ap()

    x_d = din("x", [NB, SEQ, D])
    mem_d = din("mem", [NB, MEM, D])
    pos_d = din("positions", [NB, SEQ], I32)
    g_mix_d = din("g_mix", [1, D]); g_mem_d = din("g_mem", [1, D]); g_ffn_d = din("g_ffn", [1, D])
    w_in_d = din("w_in", [D, INC])
    lam_re_d = din("lam_re", [32, 64]); lam_im_d = din("lam_im", [32, 64]); log_dt_d = din("log_dt", [1, 32])
    b_re_d = din("b_re", [32, 64, 16]); b_im_d = din("b_im", [32, 64, 16])
    c_re_d = din("c_re", [512, 64]); c_im_d = din("c_im", [512, 64])
    d_skip_d = din("d_skip", [1, 512])
    w_glu_d = din("w_glu", [512, 512])
    g_q_d = din("g_q", [1, 64]); g_k_d = din("g_k", [1, 64]); g_qm_d = din("g_qm", [1, 128]); g_km_d = din("g_km", [1, 128])
    w_mem_kv_d = din("w_mem_kv", [D, D])
    w_up_ssm_d = din("w_up_ssm", [512, D]); w_up_dsa_d = din("w_up_dsa", [512, D]); w_up_mem_d = din("w_up_mem", [512, D])
    w_out_d = din("w_out", [D, D])
    w_group_d = din("w_group", [D, 4]); b_group_d = din("b_group", [1, 4])
    w_expert_d = din("w_expert", [D, 32]); b_expert_d = din("b_expert", [1, 32])
    w_gate_d = din("w_gate", [32, D, 512]); w_upx_d = din("w_up", [32, D, 512]); w_down_d = din("w_down", [32, 512, D])
    ident_d = din("c_ident", [128, 128]); causal_d = din("c_causal", [128, 128]); jj_d = din("c_jj", [128, 128])
    invf_d = din("c_invf", [1, 32]); par_d = din("c_par", [128, 2])
    ltri_d = din("c_ltri", [128, 128]); thr_d = din("c_thr", [1, 64]); ltc_d = din("c_lt", [1, 1024])
    iot_d = din("c_iot", [1, NTILE]); pcol_d = din("c_pcol", [128, 1])

    out_d = nc.dram_tensor("out", [NB, SEQ, D], F32, kind="ExternalOutput").ap()
    wsc = nc.dram_tensor("wsc", [NCH, 128, 4096], BF16, kind="Internal").ap()
    HN = nc.dram_tensor("hn_scr", [NB * SEQ, D], BF16, kind="Internal").ap()
    XM = nc.dram_tensor("xm_scr", [NB * SEQ, D], F32, kind="Internal").ap()
    HS = nc.dram_tensor("hs_scr", [NTILE * 128, D], BF16, kind="Internal").ap()
    YS = nc.dram_tensor("ys_scr", [NTILE * 128, D], F32, kind="Internal").ap()
    HNb, XMb, HSb, YSb = Buf("HN"), Buf("XM"), Buf("HS"), Buf("YS")
    dbg_t = {}

    def dbg_out(name, shape, dt=F32):
        dbg_t[name] = nc.dram_tensor("dbg_" + name, list(shape), dt, kind="ExternalOutput").ap()
        return dbg_t[name]

    with ExitStack() as es:
        fw = FW(nc, es)
        op = fw.op

        def TT(e, out, in0, in1, o, R, W):
            op(e, lambda E: E.tensor_tensor(out=out, in0=in0, in1=in1, op=o), R, W)

        def TS(e, out, in0, s1, s2, o0, o1, R, W):
            if o1 is None:
                op(e, lambda E: E.tensor_scalar(out=out, in0=in0, scalar1=s1, scalar2=None, op0=o0), R, W)
            else:
                op(e, lambda E: E.tensor_scalar(out=out, in0=in0, scalar1=s1, scalar2=s2, op0=o0, op1=o1), R, W)

        def STT(out, in0, sc, in1, o0, o1, R, W):
            op("dve", lambda E: E.scalar_tensor_tensor(out=out, in0=in0, scalar=sc, in1=in1, op0=o0, op1=o1), R, W)

        def ACT(out, in_, func, R, W, scale=None, bias=None, accum=None):
            kw = {}
            if scale is not None:
                kw["scale"] = scale
            if bias is not None:
                kw["bias"] = bias
            if accum is not None:
                kw["accum_out"] = accum
            op("act", lambda E: E.activation(out=out, in_=in_, func=func, **kw), R, W)

        def CP(e, out, in_, R, W):
            if e == "act":
                op("act", lambda E: E.copy(out=out, in_=in_), R, W)
            else:
                op(e, lambda E: E.tensor_copy(out=out, in_=in_), R, W)

        def MM(out, lhsT, rhs, start, stop, R, W):
            op("pe", lambda E: E.matmul(out, lhsT=lhsT, rhs=rhs, start=start, stop=stop), R, W, pe_acc=True)

        def TR(out, in_, idn, R, W):
            op("pe", lambda E: E.transpose(out=out, in_=in_, identity=idn), R, W, pe_acc=True)

        def MSET(e, out, val, W):
            op(e, lambda E: E.memset(out, val), (), W)

        def rstd_of(ssq, dim, R):
            TS("dve", ssq, ssq, 1.0 / dim, 1e-6, ALU.mult, ALU.add, R, R)
            ACT(ssq, ssq, AF.Sqrt, R, R)
            op("dve", lambda E: E.reciprocal(out=ssq, in_=ssq), R, R)

        pbank = [T(es.enter_context(nc.psum_tensor("pb%d" % i, [128, 512], F32)), "pb%d" % i) for i in range(8)]
        ps_state = {"i": 0, "ring": list(range(8))}

        def PS():
            r = ps_state["ring"]
            ps_state["i"] = (ps_state["i"] + 1) % len(r)
            return pbank[r[ps_state["i"]]]

        sb = fw.sb
        esP1 = ExitStack()

        def sbp(name, shape, dt):
            return fw.sb(name, shape, dt, esP1)

        ident = sb("ident", [128, 128], F32)
        identb = sb("identb", [128, 128], BF16)
        gffnT = sb("gffnT", [128, 8], F32)
        ring = [sb("ring%d" % i, [128, 4096], BF16) for i in range(NSLOT)]
        ltri = sb("ltri", [128, 128], F32); ones = sb("ones", [128, 128], F32)
        thr = sb("thr", [128, 64], F32); iot = sb("iot", [128, NTILE], F32); pcol = sb("pcol", [128, 1], F32)
        NTT = NB * NST * 4
        OH1 = sb("OH1", [128, NTT, 32], BF16); OH2 = sb("OH2", [128, NTT, 32], BF16)
        RNK = sb("RNK", [128, NTT, 2], F32)
        W12 = sb("W12", [128, NTT, 2], F32)
        run = sb("run", [128, 32], F32)
        dpi = [sb("dpi%d" % c, [128, NTT], I32) for c in range(2)]
        widx = sb("widx", [128, NTILE, 3], I32)
        irep = sbp("irep", [128, 512], BF16)
        causal = sbp("causal", [128, 128], F32)
        causalb = sbp("causalb", [128, 128], BF16)
        zerob = sbp("zerob", [128, 128], BF16)
        jj = sbp("jj", [128, 128], F32)
        invf = sbp("invf", [128, 32], F32)
        par = sbp("par", [128, 2], F32)
        gmixT = sbp("gmixT", [128, 8], F32)
        gffnT64 = sbp("gffnT64", [64, 16], F32)
        gmemT = sbp("gmemT", [128, 8], F32)
        gq = sbp("gq", [128, 64], F32); gk = sbp("gk", [128, 64], F32)
        gqm = sbp("gqm", [128, 128], F32); gkm = sbp("gkm", [128, 128], F32)
        wr32 = sbp("wr32", [128, 8, 36], F32)
        rbias = sbp("rbias", [128, 36], F32)
        XsB = [[Buf("Xs_%d_%d" % (t_, h_)) for h_ in range(2)] for t_ in range(4)]
        XsAll = [XsB[t_][h_] for t_ in range(4) for h_ in range(2)]
        cosb = sbp("cosb", [128, 16, 32], F32); sinb = sbp("sinb", [128, 16, 32], F32)
        kT = sbp("kT", [64, SEQ], BF16)
        kiT = sbp("kiT", [64, SEQ], F32)
        vaug = sbp("vaug", [128, 16, 65], BF16)
        kmT = sbp("kmT", [128, 4, MEM], BF16)
        vmaug = sbp("vmaug", [128, 2, 4, 129], BF16)
        s5_mag = sbp("s5_mag", [128, 16], F32)
        s5_c128 = sbp("s5_c128", [128, 16], F32); s5_s128 = sbp("s5_s128", [128, 16], F32); s5_s128n = sbp("s5_s128n", [128, 16], F32)
        tabc = sbp("tabc", [128, 16, 128], F32); tabs = sbp("tabs", [128, 16, 128], F32)
        BTre = sbp("BTre", [128, 16, 128], BF16); BTim = sbp("BTim", [128, 16, 128], BF16)
        Cwre = sbp("Cwre", [128, 16, 64], BF16); Cwim = sbp("Cwim", [128, 16, 64], BF16)
        dsk = sbp("dsk", [128, 4], F32)
        carr = sbp("carr", [128, 16, 2], F32)

        chunk_buf = [Buf("ch%d" % i) for i in range(NCH)]

        class WRing:
            def __init__(self, slots, q="sp"):
                self.q = q
                self.slots = slots
                self.plan = []
                self.next_issue = 0
                self.next_req = 0
                self.free = list(range(len(slots)))
                self.slot_of = {}

            def pump(self):
                while self.free and self.next_issue < len(self.plan):
                    cid = self.plan[self.next_issue]
                    s_ = self.free.pop(0)
                    fw.dma(self.slots[s_][:], wsc[cid], reads=[chunk_buf[cid]], writes=[self.slots[s_]], q=self.q)
                    self.slot_of[self.next_issue] = s_
                    self.next_issue += 1

            def get(self, cid, kc, n):
                k = self.next_req
                assert self.plan[k] == cid, (k, self.plan[k], cid)
                self.pump()
                assert k in self.slot_of, "ring overflow"
                self.next_req += 1
                s_ = self.slot_of[k]
                return self.slots[s_], self.slots[s_][:, 0:kc * n].rearrange("p (k n) -> p k n", k=kc), (k, s_)

            def rel(self, h):
                self.free.append(h[1])
                self.pump()

        ringA = WRing(ring[0:2], q=QA)

        psr = {"A": {"ring": [0, 1, 2, 3, 4, 5, 6, 7], "i": 0}}

        def PSs(s_):
            r_ = psr[s_]
            r_["i"] = (r_["i"] + 1) % len(r_["ring"])
            return pbank[r_["ring"][r_["i"]]]

        def ps_reserve(s_):
            r_ = psr[s_]
            bk = r_["ring"].pop(0)
            r_["i"] = 0
            return bk

        def ps_release(s_, bk):
            psr[s_]["ring"].append(bk)

        try:
            fw.dma(ident[:], ident_d, writes=[ident])
            fw.dma(causal[:], causal_d, writes=[causal])
            fw.dma(jj[:], jj_d, writes=[jj])
            fw.dma(invf[:], invf_d.partition_broadcast(128), writes=[invf])
            fw.dma(par[:], par_d, writes=[par])
            fw.dma(ltri[:], ltri_d, writes=[ltri])
            fw.dma(thr[:], thr_d.partition_broadcast(128), writes=[thr])
            fw.dma(iot[:], iot_d.partition_broadcast(128), writes=[iot])
            fw.dma(pcol[:], pcol_d, writes=[pcol])
            MSET("dve", ones[:], 1.0, [ones])
            MSET("dve", run[:], 0.0, [run])
            with nc.allow_non_contiguous_dma(reason="tiny gain loads"):
                fw.dma(gmixT[:], g_mix_d.rearrange("o (k p) -> p (o k)", p=128), writes=[gmixT])
                fw.dma(gffnT[:], g_ffn_d.rearrange("o (k p) -> p (o k)", p=128), writes=[gffnT])
                fw.dma(gffnT64[:], g_ffn_d.rearrange("o (k p) -> p (o k)", p=64), writes=[gffnT64])
                fw.dma(gmemT[:], g_mem_d.rearrange("o (k p) -> p (o k)", p=128), writes=[gmemT])
            fw.dma(gq[:], g_q_d.partition_broadcast(128), writes=[gq])
            fw.dma(gk[:], g_k_d.partition_broadcast(128), writes=[gk])
            fw.dma(gqm[:], g_qm_d.partition_broadcast(128), writes=[gqm])
            fw.dma(gkm[:], g_km_d.partition_broadcast(128), writes=[gkm])
            fw.dma(wr32[:, :, 0:4], w_group_d.rearrange("(k p) n -> p k n", p=128), writes=[wr32])
            fw.dma(wr32[:, :, 4:36], w_expert_d.rearrange("(k p) n -> p k n", p=128), writes=[wr32])
            fw.dma(rbias[:, 0:4], b_group_d.partition_broadcast(128), writes=[rbias])
            fw.dma(rbias[:, 4:36], b_expert_d.partition_broadcast(128), writes=[rbias])
            CP("dve", identb[:], ident[:], [ident], [identb])
            for r in range(4):
                CP("dve", irep[:, r * 128:(r + 1) * 128], ident[:], [ident], [irep])
            TS("dve", causalb[:], causal[:], -1.0, -30000.0, ALU.is_lt, ALU.mult, [causal], [causalb])
            MSET("dve", vaug[:, :, 64:65], 1.0, [vaug])
            MSET("dve", zerob[:], 0.0, [zerob])
            MSET("dve", vmaug[:, :, :, 128:129], 1.0, [vmaug])

            def w_in_cols(c0, c1):
                return w_in_d.rearrange("(k p) n -> p k n", p=128)[:, :, c0:c1], 8, c1 - c0

            srcs = {C_U: w_in_cols(0, 512), C_Q: w_in_cols(512, 1024), C_KVI: w_in_cols(1024, 1476), C_QM: w_in_cols(1476, 1988)}
            for i in range(6):
                srcs[C_G0 + i] = w_in_cols(1988 + 512 * i, 1988 + 512 * (i + 1))
            srcs[C_GLU] = (w_glu_d.rearrange("(k p) n -> p k n", p=128), 4, 512)
            srcs[C_UPS] = (w_up_ssm_d.rearrange("(k p) n -> p k n", p=128), 4, 1024)
            srcs[C_UPD] = (w_up_dsa_d.rearrange("(k p) n -> p k n", p=128), 4, 1024)
            srcs[C_UPM] = (w_up_mem_d.rearrange("(k p) n -> p k n", p=128), 4, 1024)
            srcs[C_OUT0] = (w_out_d.rearrange("(k p) n -> p k n", p=128)[:, :, 0:512], 8, 512)
            srcs[C_OUT1] = (w_out_d.rearrange("(k p) n -> p k n", p=128)[:, :, 512:1024], 8, 512)
            srcs[C_MKV0] = (w_mem_kv_d.rearrange("(k p) n -> p k n", p=128)[:, :, 0:512], 8, 512)
            srcs[C_MKV1] = (w_mem_kv_d.rearrange("(k p) n -> p k n", p=128)[:, :, 512:1024], 8, 512)
            for e in range(32):
                srcs[C_EXP + 3 * e] = (w_gate_d[e].rearrange("(k p) n -> p k n", p=128), 8, 512)
                srcs[C_EXP + 3 * e + 1] = (w_upx_d[e].rearrange("(k p) n -> p k n", p=128), 8, 512)
                srcs[C_EXP + 3 * e + 2] = (w_down_d[e].rearrange("(k p) n -> p k n", p=128), 4, 1024)

            with ExitStack() as es0:
                st32 = [sb("st32_%d" % i, [128, 4096], F32, es0) for i in range(3)]
                st16 = [sb("st16_%d" % i, [128, 4096], BF16, es0) for i in range(3)]
                cast_eng = ["pool", "dve", "act"]
                z16 = sb("z16", [128, 4096], BF16, es0)
                MSET("pool", z16[:], 0.0, [z16])
                hs_z = HS.rearrange("(a p k) n -> a p (k n)", p=128, k=4)
                for a_ in range(NTILE // 4):
                    fw.dma(hs_z[a_], z16[:], reads=[z16], writes=[HSb])
                order = list(range(C_EXP))
                for n_, cid in enumerate(order):
                    src, kc, n = srcs[cid]
                    a = st32[n_ % 3]; bq = st16[n_ % 3]
                    fw.dma(a[:, 0:kc * n].rearrange("p (k n) -> p k n", k=kc), src, writes=[a])
                    CP(cast_eng[n_ % 3], bq[:, 0:kc * n], a[:, 0:kc * n], [a], [bq])
                    fw.dma(wsc[cid][:, 0:kc * n], bq[:, 0:kc * n], reads=[bq], writes=[chunk_buf[cid]], q="act")

                fw.barrier()
            fw.barrier()
            with ExitStack() as es0:
                lre = sb("lre", [128, 16], F32, es0); lim = sb("lim", [128, 16], F32, es0); dtv = sb("dtv", [128, 16], F32, es0)
                with nc.allow_non_contiguous_dma(reason="tiny param loads"):
                    fw.dma(lre[:], lam_re_d.rearrange("(c g) p -> (g p) c", g=2), writes=[lre])
                    fw.dma(lim[:], lam_im_d.rearrange("(c g) p -> (g p) c", g=2), writes=[lim])
                    ldt2 = log_dt_d.rearrange("o (c g) -> o g c", g=2)
                    for gl in range(2):
                        fw.dma(dtv[gl * 64:(gl + 1) * 64, :], ldt2[:, gl, :].partition_broadcast(64), writes=[dtv])
                    fw.dma(dsk[:], d_skip_d.rearrange("o (c p) -> p (o c)", p=128), writes=[dsk])
                ACT(dtv[:], dtv[:], AF.Exp, [dtv], [dtv])
                are = sb("are", [128, 16], F32, es0); aim = sb("aim", [128, 16], F32, es0)
                TT("dve", are[:], lre[:], dtv[:], ALU.mult, [lre, dtv], [are])
                TT("dve", aim[:], lim[:], dtv[:], ALU.mult, [lim, dtv], [aim])
                ACT(s5_mag[:], are[:], AF.Exp, [are], [s5_mag])

                def wrap_pi(xt, shape, tmp_i, tmp_f):
                    TS("dve", tmp_f[:], xt[:], 1.0 / TWO_PI, None, ALU.mult, None, [xt], [tmp_f])
                    CP("dve", tmp_i[:], tmp_f[:], [tmp_f], [tmp_i])
                    CP("dve", tmp_f[:], tmp_i[:], [tmp_i], [tmp_f])
                    c1 = 6.28125
                    c2 = TWO_PI - c1
                    STT(xt[:], tmp_f[:], -c1, xt[:], ALU.mult, ALU.add, [tmp_f, xt], [xt])
                    STT(xt[:], tmp_f[:], -c2, xt[:], ALU.mult, ALU.add, [tmp_f, xt], [xt])
                    TS("dve", tmp_f[:], xt[:], math.pi, -TWO_PI, ALU.is_gt, ALU.mult, [xt], [tmp_f])
                    TT("dve", xt[:], xt[:], tmp_f[:], ALU.add, [xt, tmp_f], [xt])
                    TS("dve", tmp_f[:], xt[:], -math.pi, TWO_PI, ALU.is_lt, ALU.mult, [xt], [tmp_f])
                    TT("dve", xt[:], xt[:], tmp_f[:], ALU.add, [xt, tmp_f], [xt])

                def cos_arg(dst, src, tmp_f):
                    TS("dve", tmp_f[:], src[:], math.pi / 2, -TWO_PI, ALU.is_gt, ALU.mult, [src], [tmp_f])
                    STT(dst[:], src[:], math.pi / 2, tmp_f[:], ALU.add, ALU.add, [src, tmp_f], [dst])

                ti16 = sb("ti16", [128, 16], I32, es0); tf16 = sb("tf16", [128, 16], F32, es0)
                wrap_pi(aim, None, ti16, tf16)
                lbr = sb("lbr", [128, 16], F32, es0); lbi = sb("lbi", [128, 16], F32, es0); ca16 = sb("ca16", [128, 16], F32, es0)
                cos_arg(ca16, aim, tf16)
                ACT(lbi[:], aim[:], AF.Sin, [aim], [lbi])
                ACT(lbr[:], ca16[:], AF.Sin, [ca16], [lbr])
                TT("dve", lbr[:], lbr[:], s5_mag[:], ALU.mult, [lbr, s5_mag], [lbr])
                TT("dve", lbi[:], lbi[:], s5_mag[:], ALU.mult, [lbi, s5_mag], [lbi])
                n2 = sb("n2", [128, 16], F32, es0); t16a = sb("t16a", [128, 16], F32, es0)
                cfr = sb("cfr", [128, 16], F32, es0); cfi = sb("cfi", [128, 16], F32, es0); lb1 = sb("lb1", [128, 16], F32, es0)
                TT("dve", n2[:], lre[:], lre[:], ALU.mult, [lre], [n2])
                TT("dve", t16a[:], lim[:], lim[:], ALU.mult, [lim], [t16a])
                TT("dve", n2[:], n2[:], t16a[:], ALU.add, [n2, t16a], [n2])
                op("dve", lambda E: E.reciprocal(out=n2[:], in_=n2[:]), [n2], [n2])
                TS("dve", lb1[:], lbr[:], -1.0, None, ALU.add, None, [lbr], [lb1])
                TT("dve", cfr[:], lb1[:], lre[:], ALU.mult, [lb1, lre], [cfr])
                TT("dve", t16a[:], lbi[:], lim[:], ALU.mult, [lbi, lim], [t16a])
                TT("dve", cfr[:], cfr[:], t16a[:], ALU.add, [cfr, t16a], [cfr])
                TT("dve", cfr[:], cfr[:], n2[:], ALU.mult, [cfr, n2], [cfr])
                TT("dve", cfi[:], lbi[:], lre[:], ALU.mult, [lbi, lre], [cfi])
                TT("dve", t16a[:], lb1[:], lim[:], ALU.mult, [lb1, lim], [t16a])
                TT("dve", cfi[:], cfi[:], t16a[:], ALU.subtract, [cfi, t16a], [cfi])
                TT("dve", cfi[:], cfi[:], n2[:], ALU.mult, [cfi, n2], [cfi])
                bre = sb("bre", [128, 16, 16], F32, es0); bim = sb("bim", [128, 16, 16], F32, es0)
                fw.dma(bre[:], b_re_d.rearrange("(c g) p h -> (g p) c h", g=2), writes=[bre])
                fw.dma(bim[:], b_im_d.rearrange("(c g) p h -> (g p) c h", g=2), writes=[bim])
                bbr = sb("bbr", [128, 16, 16], F32, es0); bbi = sb("bbi", [128, 16, 16], F32, es0); tb = sb("tb", [128, 16, 16], F32, es0)
                cfr_b = cfr[:].unsqueeze(2).to_broadcast([128, 16, 16]); cfi_b = cfi[:].unsqueeze(2).to_broadcast([128, 16, 16])
                TT("dve", bbr[:], bre[:], cfr_b, ALU.mult, [bre, cfr], [bbr])
                TT("dve", tb[:], bim[:], cfi_b, ALU.mult, [bim, cfi], [tb])
                TT("dve", bbr[:], bbr[:], tb[:], ALU.subtract, [bbr, tb], [bbr])
                TT("dve", bbi[:], bim[:], cfr_b, ALU.mult, [bim, cfr], [bbi])
                TT("dve", tb[:], bre[:], cfi_b, ALU.mult, [bre, cfi], [tb])
                TT("dve", bbi[:], bbi[:], tb[:], ALU.add, [bbi, tb], [bbi])
                Xw = sb("Xw", [128, 16, 128], F32, es0)
                for (src_bb, dstBT) in ((bbr, BTre), (bbi, BTim)):
                    MSET("pool", Xw[:], 0.0, [Xw])
                    for gl in range(2):
                        for r in range(4):
                            CP("dve", Xw[gl * 64:(gl + 1) * 64, r::4, 32 * r + 16 * gl:32 * r + 16 * gl + 16],
                               src_bb[gl * 64:(gl + 1) * 64, r::4, :], [src_bb], [Xw])
                    for fc in range(4):
                        pb = PS()
                        for r in range(4):
                            MM(pb[:, r * 128:(r + 1) * 128], Xw[:, 4 * fc + r, :], ident[:], True, True, [Xw, ident], [pb])
                        CP("act", dstBT[:, 4 * fc:4 * fc + 4, :], pb[:].rearrange("p (r n) -> p r n", r=4), [pb], [dstBT])
                for (c_d, dstC, sign) in ((c_re_d, Cwre, 1.0), (c_im_d, Cwim, -1.0)):
                    MSET("dve", dstC[:], 0.0, [dstC])
                    for k in range(4):
                        cl = sb("cl_%d_%d" % (k, int(sign > 0)), [128, 64], F32, es0)
                        aw = sb("aw_%d_%d" % (k, int(sign > 0)), [128, 128], F32, es0)
                        fw.dma(cl[:], c_d[k * 128:(k + 1) * 128, :], writes=[cl])
                        TS("dve", aw[:, 0:64], cl[:], par[:, 0:1], None, ALU.mult, None, [cl, par], [aw])
                        TS("dve", aw[:, 64:128], cl[:], par[:, 1:2], None, ALU.mult, None, [cl, par], [aw])
                        pb = PS()
                        for r in range(4):
                            MM(pb[:, 32 * r:32 * r + 32], aw[:], ident[:, 32 * r:32 * r + 32], True, True, [aw, ident], [pb])
                        for r in range(4):
                            ACT(dstC[:, 4 * k + r, 32 * (r % 2):32 * (r % 2) + 32], pb[:, 32 * r:32 * r + 32], AF.Copy, [pb], [dstC], scale=sign)
                ang = sb("ang", [128, 16, 128], F32, es0); angi = sb("angi", [128, 16, 128], I32, es0); angf = sb("angf", [128, 16, 128], F32, es0)
                TT("dve", ang[:], jj[:].unsqueeze(1).to_broadcast([128, 16, 128]), aim[:].unsqueeze(2).to_broadcast([128, 16, 128]), ALU.mult, [jj, aim], [ang])
                wrap_pi(ang, None, angi, angf)
                ACT(tabs[:], ang[:], AF.Sin, [ang], [tabs])
                ang2 = sb("ang2", [128, 16, 128], F32, es0)
                cos_arg(ang2, ang, angf)
                ACT(tabc[:], ang2[:], AF.Sin, [ang2], [tabc])
                CP("dve", s5_c128[:], tabc[:, :, 127], [tabc], [s5_c128])
                CP("dve", s5_s128[:], tabs[:, :, 127], [tabs], [s5_s128])
                TS("dve", s5_s128n[:], s5_s128[:], -1.0, None, ALU.mult, None, [s5_s128], [s5_s128n])
                fw.barrier()
            fw.barrier()

            planA_st = [C_U] + [C_Q, C_KVI, C_QM] + [C_GLU] + [C_G0, C_G0 + 1, C_UPS, C_G0 + 2, C_G0 + 3, C_UPD, C_G0 + 4, C_G0 + 5, C_UPM, C_OUT0, C_OUT1]
            planB_st = [C_EXP + 3 * e + j for e in range(NEXP) for j in range(3)]
            for b in range(NB):
                ringA.plan += [C_MKV0, C_MKV1]
                for s in range(NST):
                    ringA.plan += planA_st


            def fence(tiles):
                evs = []
                for t_ in tiles:
                    bb = _b(t_)
                    if bb.w is not None:
                        evs.append(bb.w)
                    evs += list(bb.r.values())
                for e_ in fw.eng:
                    for ev in evs:
                        fw._wait(e_, ev)

            def rope(dst, src, cbx, sbx, tmp, R, W, eng="dve"):
                x1 = src[:, :, 0:32]; x2 = src[:, :, 32:64]
                TT(eng, tmp, x2, sbx, ALU.mult, R, W)
                TT(eng, dst[:, :, 0:32], x1, cbx, ALU.mult, R, W)
                TT(eng, dst[:, :, 0:32], dst[:, :, 0:32], tmp, ALU.subtract, W, W)
                TT(eng, tmp, x1, sbx, ALU.mult, R, W)
                TT(eng, dst[:, :, 32:64], x2, cbx, ALU.mult, R, W)
                TT(eng, dst[:, :, 32:64], dst[:, :, 32:64], tmp, ALU.add, W, W)

            def batch_prep(b):
                with ExitStack() as esb:
                    posi = sb("posi", [16, 128], I32, esb); posf = sb("posf", [16, 128], F32, esb); post = sb("post", [128, 16], F32, esb)
                    fw.dma(posi[:], pos_d[b].rearrange("(t p) -> t p", p=128), writes=[posi])
                    CP("dve", posf[:], posi[:], [posi], [posf])
                    pb = PS()
                    TR(pb[:, 0:16], posf[:], ident[0:16, 0:16], [posf, ident], [pb])
                    CP("dve", post[:], pb[:, 0:16], [pb], [post])
                    angb = sb("angb", [128, 16, 32], F32, esb); angbi = sb("angbi", [128, 16, 32], I32, esb); angbf = sb("angbf", [128, 16, 32], F32, esb)
                    angb2 = sb("angb2", [128, 16, 32], F32, esb)
                    TT("dve", angb[:], post[:].unsqueeze(2).to_broadcast([128, 16, 32]), invf[:].unsqueeze(1).to_broadcast([128, 16, 32]), ALU.mult, [post, invf], [angb])
                    wrap_pi(angb, None, angbi, angbf)
                    ACT(sinb[:], angb[:], AF.Sin, [angb], [sinb])
                    cos_arg(angb2, angb, angbf)
                    ACT(cosb[:], angb2[:], AF.Sin, [angb2], [cosb])
                    MSET("dve", carr[:], 0.0, [carr])
                    memx = sb("memx", [128, 2, D], F32, esb); memb = sb("memb", [128, D], BF16, esb); memT = sb("memT", [128, 8, MEM], BF16, esb)
                    mss = sb("mss", [128, 2], F32, esb)
                    fw.dma(memx[:], mem_d[b].rearrange("(t p) d -> p t d", p=128), writes=[memx])
                    for mt in range(2):
                        ACT(memb[:], memx[:, mt, :], AF.Square, [memx], [memb, mss], accum=mss[:, mt:mt + 1])
                    rstd_of(mss[:], D, [mss])
                    for mt in range(2):
                        TS("dve", memb[:], memx[:, mt, :], mss[:, mt:mt + 1], None, ALU.mult, None, [memx, mss], [memb])
                        pb = PS()
                        pbv = pb[:].bitcast(BF16)
                        for kc in range(8):
                            TR(pbv[:, kc * 128:(kc + 1) * 128], memb[:, kc * 128:(kc + 1) * 128], identb[:], [memb, identb], [pb])
                        TT("dve", memT[:, :, mt * 128:(mt + 1) * 128], pbv[:, 0:1024].rearrange("p (k n) -> p k n", k=8),
                           gmemT[:].unsqueeze(2).to_broadcast([128, 8, 128]), ALU.mult, [pb, gmemT], [memT])
                    slK, wK, hK = ringA.get(C_MKV0, 8, 512)
                    slV, wV, hV = ringA.get(C_MKV1, 8, 512)
                    kmraw = sb("kmraw", [128, 512], F32, esb); kmsq = sb("kmsq", [128, 512], F32, esb); kss = sb("kss", [128, 4], F32, esb)
                    kmn = sb("kmn", [128, 512], BF16, esb)
                    for mt in range(2):
                        pb = PS()
                        for kc in range(8):
                            MM(pb[:], memT[:, kc, mt * 128:(mt + 1) * 128], wK[:, kc, :], kc == 0, kc == 7, [memT, slK], [pb])
                        CP("act", kmraw[:], pb[:], [pb], [kmraw])
                        TT("dve", kmsq[:], kmraw[:], kmraw[:], ALU.mult, [kmraw], [kmsq])
                        op("dve", lambda E: E.tensor_reduce(out=kss[:], in_=kmsq[:].rearrange("p (h d) -> p h d", h=4), axis=AX.X, op=ALU.add), [kmsq], [kss])
                        rstd_of(kss[:], 128, [kss])
                        TT("dve", kmraw[:].rearrange("p (h d) -> p h d", h=4), kmraw[:].rearrange("p (h d) -> p h d", h=4),
                           kss[:].unsqueeze(2).to_broadcast([128, 4, 128]), ALU.mult, [kmraw, kss], [kmraw])
                        TT("dve", kmn[:].rearrange("p (h d) -> p h d", h=4), kmraw[:].rearrange("p (h d) -> p h d", h=4),
                           gkm[:].unsqueeze(1).to_broadcast([128, 4, 128]), ALU.mult, [kmraw, gkm], [kmn])
                        pb2 = PS()
                        pbv = pb2[:].bitcast(BF16)
                        for h in range(4):
                            TR(pbv[:, h * 128:(h + 1) * 128], kmn[:, h * 128:(h + 1) * 128], identb[:], [kmn, identb], [pb2])
                        CP("act", kmT[:, :, mt * 128:(mt + 1) * 128], pbv[:, 0:512].rearrange("p (h n) -> p h n", h=4), [pb2], [kmT])
                        pb3 = PS()
                        for kc in range(8):
                            MM(pb3[:], memT[:, kc, mt * 128:(mt + 1) * 128], wV[:, kc, :], kc == 0, kc == 7, [memT, slV], [pb3])
                        CP("act", vmaug[:, mt, :, 0:128], pb3[:].rearrange("p (h d) -> p h d", h=4), [pb3], [vmaug])
                    ringA.rel(hK); ringA.rel(hV)
                    fw.barrier()

            def attn_AG(b, st, A, esa):
                tok0 = st * 512
                hT = A["hT"]
                ydsaT, ymemT, yssmT, rst = A["ydsaT"], A["ymemT"], A["yssmT"], A["rst"]
                exa = ExitStack()
                xt = [sb("xt%d" % i, [128, D], F32, exa) for i in range(2)]
                hb = sb("hb", [128, D], BF16, exa)
                PSa = lambda: PSs("A")
                for t in range(4):
                    x_ = xt[t % 2]
                    fw.dma(x_[:], x_d[b, tok0 + t * 128:tok0 + (t + 1) * 128, :], writes=[x_], q=QA)
                    ACT(hb[:], x_[:], AF.Square, [x_], [hb, rst], accum=rst[:, t:t + 1])
                    yield 0.4
                    rstd_of(rst[:, t:t + 1], D, [rst])
                    TS("dve", hb[:], x_[:], rst[:, t:t + 1], None, ALU.mult, None, [x_, rst], [hb])
                    pb = PSa(); pbv = pb[:].bitcast(BF16)
                    for kc in range(8):
                        TR(pbv[:, kc * 128:(kc + 1) * 128], hb[:, kc * 128:(kc + 1) * 128], identb[:], [hb, identb], [pb])
                    TT("dve", hT[:, :, t * 128:(t + 1) * 128], pbv[:, 0:1024].rearrange("p (k n) -> p k n", k=8),
                       gmixT[:].unsqueeze(2).to_broadcast([128, 8, 128]), ALU.mult, [pb, gmixT], [hT])
                    yield 0.4
                fence(xt + [hb])
                exa.close()
                e2a = ExitStack(); e2b = ExitStack()
                uT = sb("uT", [128, 4, 512], BF16, e2a)
                qT = sb("qT", [64, 8, 512], BF16, e2b); qiT = sb("qiT", [64, 4, 512], F32, e2b)
                qmT = sb("qmT", [128, 4, 512], BF16, e2b); wis = sb("wis", [128, 4, 4], F32, e2b)
                sl, w, hh = ringA.get(C_U, 8, 512)
                for fc in range(4):
                    pb = PSa()
                    for kc in range(8):
                        MM(pb[:], w[:, kc, fc * 128:(fc + 1) * 128], hT[:, kc, :], kc == 0, kc == 7, [sl, hT], [pb])
                    CP("act", uT[:, fc, :], pb[:], [pb], [uT])
                    yield 0.4
                ringA.rel(hh)
                with ExitStack() as est:
                    raws = [(sb("qraw", [128, 512], F32, est), sb("kviraw", [128, 452], F32, est), sb("qmraw", [128, 512], F32, est)) for i_ in range(4)]
                    sq = sb("sq", [128, 512], F32, est); s8 = sb("s8", [128, 12], F32, est)
                    qn = sb("qn", [128, 8, 64], F32, est); qr = sb("qr", [128, 8, 64], BF16, est); tq = sb("tq", [128, 8, 32], F32, est)
                    kn = sb("kn", [128, 1, 64], F32, est); kr = sb("kr", [128, 1, 64], BF16, est)
                    qir = sb("qir", [128, 5, 64], F32, est)
                    qmn = sb("qmn", [128, 4, 128], BF16, est)
                    dtiles = [x_ for r_ in raws for x_ in r_] + [sq, s8, qn, qr, tq, kn, kr, qir, qmn]
                    for ci, (cid, n) in enumerate(((C_Q, 512), (C_KVI, 452), (C_QM, 512))):
                        sl, w, hh = ringA.get(cid, 8, n)
                        for t in range(4):
                            tsl = slice(t * 128, (t + 1) * 128)
                            dst = raws[t][ci]
                            pb = PSa()
                            for kc in range(8):
                                MM(pb[:, 0:n], hT[:, kc, tsl], w[:, kc, :], kc == 0, kc == 7, [hT, sl], [pb])
                            CP("act", dst[:], pb[:, 0:n], [pb], [dst])
                            yield 0.4
                        ringA.rel(hh)
                    for t in range(4):
                        gt_ = st * 4 + t
                        tsl = slice(t * 128, (t + 1) * 128)
                        cos_t = cosb[:, gt_, :]; sin_t = sinb[:, gt_, :]
                        qraw, kviraw, qmraw = raws[t]
                        q3 = qraw[:].rearrange("p (h d) -> p h d", h=8)
                        TT("pool", sq[:], qraw[:], qraw[:], ALU.mult, [qraw], [sq])
                        op("dve", lambda E: E.tensor_reduce(out=s8[:, 0:8], in_=sq[:].rearrange("p (h d) -> p h d", h=8), axis=AX.X, op=ALU.add), [sq], [s8])
                        TT("pool", sq[:, 0:64], kviraw[:, 0:64], kviraw[:, 0:64], ALU.mult, [kviraw, sq], [sq])
                        op("dve", lambda E: E.tensor_reduce(out=s8[:, 8:9], in_=sq[:, 0:64], axis=AX.X, op=ALU.add), [sq], [s8])
                        rstd_of(s8[:, 0:9], 64, [s8])
                        TT("dve", qn[:], q3, s8[:, 0:8].unsqueeze(2).to_broadcast([128, 8, 64]), ALU.mult, [qraw, s8], [qn])
                        TT("pool", qn[:], qn[:], gq[:].unsqueeze(1).to_broadcast([128, 8, 64]), ALU.mult, [qn, gq], [qn])
                        cb = cos_t.unsqueeze(1).to_broadcast([128, 8, 32]); sbb = sin_t.unsqueeze(1).to_broadcast([128, 8, 32])
                        rope(qr, qn, cb, sbb, tq[:], [qn, cosb, sinb], [qr, tq], eng="pool")
                        yield 0.4
                        pb = PSa(); pbv = pb[:].bitcast(BF16)
                        for h in range(8):
                            TR(pbv[0:64, h * 128:(h + 1) * 128], qr[:, h, :], identb[:], [qr, identb], [pb])
                        CP("act", qT[:, :, tsl], pbv[0:64, 0:1024].rearrange("p (h n) -> p h n", h=8), [pb], [qT])
                        yield 0.4
                        TS("dve", kn[:, 0, :], kviraw[:, 0:64], s8[:, 8:9], None, ALU.mult, None, [kviraw, s8], [kn])
                        TT("pool", kn[:, 0, :], kn[:, 0, :], gk[:], ALU.mult, [kn, gk], [kn])
                        c1b = cos_t.unsqueeze(1); s1b = sin_t.unsqueeze(1)
                        rope(kr, kn, c1b, s1b, tq[:, 0:1, :], [kn, cosb, sinb], [kr, tq], eng="pool")
                        yield 0.4
                        pb = PSa(); pbv = pb[:].bitcast(BF16)
                        TR(pbv[0:64, 0:128], kr[:, 0, :], identb[:], [kr, identb], [pb])
                        CP("act", kT[:, gt_ * 128:(gt_ + 1) * 128], pbv[0:64, 0:128], [pb], [kT])
                        yield 0.4
                        CP("pool", vaug[:, gt_, 0:64], kviraw[:, 64:128], [kviraw], [vaug])
                        yield 0.4
                        qi3 = kviraw[:, 128:448].rearrange("p (h d) -> p h d", h=5)
                        cb5 = cos_t.unsqueeze(1).to_broadcast([128, 5, 32]); sb5 = sin_t.unsqueeze(1).to_broadcast([128, 5, 32])
                        rope(qir, qi3, cb5, sb5, tq[:, 0:5, :], [kviraw, cosb, sinb], [qir, tq], eng="pool")
                        yield 0.4
                        pb = PSa()
                        for h in range(4):
                            TR(pb[0:64, h * 128:(h + 1) * 128], qir[:, h, :], ident[:], [qir, ident], [pb])
                        CP("act", qiT[:, :, tsl], pb[0:64, :].rearrange("p (h n) -> p h n", h=4), [pb], [qiT])
                        yield 0.4
                        pb = PSa()
                        TR(pb[0:64, 0:128], qir[:, 4, :], ident[:], [qir, ident], [pb])
                        CP("act", kiT[:, gt_ * 128:(gt_ + 1) * 128], pb[0:64, 0:128], [pb], [kiT])
                        yield 0.4
                        TS("dve", wis[:, t, :], kviraw[:, 448:452], 0.5 * 0.125, None, ALU.mult, None, [kviraw], [wis])
                        yield 0.4
                        TT("pool", sq[:], qmraw[:], qmraw[:], ALU.mult, [qmraw], [sq])
                        op("dve", lambda E: E.tensor_reduce(out=s8[:, 0:4], in_=sq[:].rearrange("p (h d) -> p h d", h=4), axis=AX.X, op=ALU.add), [sq], [s8])
                        rstd_of(s8[:, 0:4], 128, [s8])
                        TT("dve", qmraw[:].rearrange("p (h d) -> p h d", h=4), qmraw[:].rearrange("p (h d) -> p h d", h=4),
                           s8[:, 0:4].unsqueeze(2).to_broadcast([128, 4, 128]), ALU.mult, [qmraw, s8], [qmraw])
                        TT("pool", qmn[:], qmraw[:].rearrange("p (h d) -> p h d", h=4), gqm[:].unsqueeze(1).to_broadcast([128, 4, 128]), ALU.mult, [qmraw, gqm], [qmn])
                        pb = PSa(); pbv = pb[:].bitcast(BF16)
                        for h in range(4):
                            TR(pbv[:, h * 128:(h + 1) * 128], qmn[:, h, :], identb[:], [qmn, identb], [pb])
                        CP("act", qmT[:, :, tsl], pbv[:, 0:512].rearrange("p (h n) -> p h n", h=4), [pb], [qmT])
                        yield 0.4
                    fence(dtiles)

                estC = ExitStack()
                fences = []

                def genE(est):
                    pm = [sb("pm%d" % i, [128, 128], BF16, est) for i in range(2)]
                    ymem = sb("ymem", [128, 4, 128], BF16, est)
                    rden = sb("rden", [128, 4], F32, est)
                    pmi = 0
                    for t in range(4):
                        tsl = slice(t * 128, (t + 1) * 128)
                        for hp in range(2):
                            acc = pbank[ps_reserve("A")]
                            for h2 in range(2):
                                h = 2 * hp + h2
                                for mh in range(2):
                                    pb = PSa()
                                    MM(pb[:, 0:128], kmT[:, h, mh * 128:(mh + 1) * 128], qmT[:, h, tsl], True, True, [kmT, qmT], [pb])
                                    p_ = pm[pmi % 2]; pmi += 1
                                    ACT(p_[:], pb[:, 0:128], AF.Exp, [pb], [p_], scale=128 ** -0.5)
                                    yield 0.4
                                    MM(acc[:, h2 * 129:h2 * 129 + 129], p_[:], vmaug[:, mh, h, :], mh == 0, mh == 1, [p_, vmaug], [acc])
                            a3 = acc[:, 0:258].rearrange("p (h d) -> p h d", h=2)
                            op("dve", lambda E: E.reciprocal(out=rden[:, 2 * hp:2 * hp + 2], in_=a3[:, :, 128]), [acc], [rden])
                            TT("dve", ymem[:, 2 * hp:2 * hp + 2, :], a3[:, :, 0:128], rden[:, 2 * hp:2 * hp + 2].unsqueeze(2).to_broadcast([128, 2, 128]),
                               ALU.mult, [acc, rden], [ymem])
                            ps_release("A", pbank.index(acc))
                            yield 0.4
                        pb = PSa(); pbv = pb[:].bitcast(BF16)
                        for h in range(4):
                            TR(pbv[:, h * 128:(h + 1) * 128], ymem[:, h, :], identb[:], [ymem, identb], [pb])
                        CP("act", ymemT[:, :, tsl], pbv[:, 0:512].rearrange("p (h n) -> p h n", h=4), [pb], [ymemT])
                        yield 0.4
                    fences.append(pm + [ymem, rden])

                def genF(est, tiles):
                    isc = sb("isc", [128, SEQ], F32, est)
                    mneg = sb("mneg", [128, SEQ], BF16, est)
                    rl = [sb("rl%d" % i, [128, 512], F32, est) for i in range(2)]
                    PT = [sb("PT%d" % i, [128, 1024], BF16, est) for i in range(2)]
                    m8 = sb("m8", [128, 8], F32, est)
                    ydsa = sb("ydsa", [128, 8, 64], BF16, est)
                    rden8 = sb("rden8", [128, 8], F32, est)
                    oacc = sb("oacc", [128, 2, 260], F32, est)
                    rli = 0; pti = 0
                    for t in tiles:
                        gt_ = st * 4 + t
                        nk = (gt_ + 1) * 128
                        tsl = slice(t * 128, (t + 1) * 128)
                        if gt_ >= 2:
                            for c0 in range(0, nk, 512):
                                cw = min(512, nk - c0)
                                for h in range(4):
                                    pb = PSa()
                                    MM(pb[:, 0:cw], qiT[:, h, tsl], kiT[:, c0:c0 + cw], True, True, [qiT, kiT], [pb])
                                    r_ = rl[rli % 2]; rli += 1
                                    ACT(r_[:, 0:cw], pb[:, 0:cw], AF.Relu, [pb], [r_])
                                    yield 0.4
                                    if h == 0:
                                        TS("dve", isc[:, c0:c0 + cw], r_[:, 0:cw], wis[:, t, 0:1], None, ALU.mult, None, [r_, wis], [isc])
                                    else:
                                        STT(isc[:, c0:c0 + cw], r_[:, 0:cw], wis[:, t, h:h + 1], isc[:, c0:c0 + cw], ALU.mult, ALU.add, [r_, wis, isc], [isc])
                                yield 0.4
                            TT("dve", isc[:, nk - 128:nk], isc[:, nk - 128:nk], causal[:], ALU.add, [isc, causal], [isc])
                            for r in range(32):
                                op("dve", lambda E: E.max(out=m8[:], in_=isc[:, 0:nk]), [isc], [m8])
                                yield nk / 960.0
                                op("dve", lambda E: E.match_replace(out=isc[:, 0:nk], in_to_replace=m8[:], in_values=isc[:, 0:nk], imm_value=-3.0e38), [isc, m8], [isc])
                                yield nk / 960.0
                            TS("dve", mneg[:, 0:nk], isc[:, 0:nk], -1.0e35, -30000.0, ALU.is_gt, ALU.mult, [isc], [mneg])
                            yield 0.4
                        for j in range(gt_ + 1):
                            ksl = slice(j * 128, (j + 1) * 128)
                            need_mask = (gt_ >= 2) or (j == gt_)
                            p_ = PT[pti % 2]; pti += 1
                            for half in range(2):
                                pb = PSa()
                                MM(pb[:], kT[:, ksl], qT[:, 4 * half:4 * half + 4, tsl], True, not need_mask, [kT, qT], [pb])
                                if need_mask:
                                    ml = mneg[:, ksl] if gt_ >= 2 else causalb[:]
                                    MM(pb[:], ml, irep[:], False, True, [mneg, causalb, irep], [pb])
                                ACT(p_[:, half * 512:(half + 1) * 512], pb[:], AF.Exp, [pb], [p_], scale=0.125)
                                yield 0.4
                            for hp in range(2):
                                acc = PSa()
                                for h4 in range(4):
                                    h = 4 * hp + h4
                                    MM(acc[:, h4 * 65:h4 * 65 + 65], p_[:, h * 128:(h + 1) * 128], vaug[:, j, :], True, True, [p_, vaug], [acc])
                                if j == 0:
                                    CP("act", oacc[:, hp, :], acc[:, 0:260], [acc], [oacc])
                                    yield 0.4
                                else:
                                    TT("dve", oacc[:, hp, :], oacc[:, hp, :], acc[:, 0:260], ALU.add, [acc, oacc], [oacc])
                            yield 0.4
                        a3 = oacc[:].rearrange("p a (h d) -> p (a h) d", h=4)
                        op("dve", lambda E: E.reciprocal(out=rden8[:], in_=a3[:, :, 64]), [oacc], [rden8])
                        TT("dve", ydsa[:], a3[:, :, 0:64], rden8[:].unsqueeze(2).to_broadcast([128, 8, 64]), ALU.mult, [oacc, rden8], [ydsa])
                        pb = PSa(); pbv = pb[:].bitcast(BF16)
                        for c in range(4):
                            TR(pbv[:, c * 128:(c + 1) * 128], ydsa[:, 2 * c:2 * c + 2, :].rearrange("p h d -> p (h d)"), identb[:], [ydsa, identb], [pb])
                        CP("act", ydsaT[:, :, tsl], pbv[:, 0:512].rearrange("p (c n) -> p c n", c=4), [pb], [ydsaT])
                        yield 0.4
                    fences.append([isc, mneg, m8, ydsa, rden8, oacc] + rl + PT)

                yg = sb("yg", [128, 4, 512], BF16, estC)
                sz = sb("sz", [128, 512], BF16, estC)

                def gluG():
                    sl, w, hh = ringA.get(C_GLU, 4, 512)
                    for fo in range(4):
                        pb = PSa()
                        for fc in range(4):
                            MM(pb[:], w[:, fc, fo * 128:(fo + 1) * 128], yg[:, fc, :], fc == 0, fc == 3, [sl, yg], [pb])
                        ACT(sz[:], pb[:], AF.Sigmoid, [pb], [sz])
                        yield 0.4
                        TT("dve", yssmT[:, fo, :], yg[:, fo, :], sz[:], ALU.mult, [yg, sz], [yssmT])
                        yield 0.4
                    ringA.rel(hh)

                def genG(est, fcs, do_glu):
                    sets = []
                    t1_ = sb("t1", [128, 4, 128], F32, est); t2_ = sb("t2", [128, 4, 128], F32, est)
                    t3_ = sb("t3", [128, 4, 128], F32, est); t4_ = sb("t4", [128, 4, 128], F32, est)
                    for i_ in range(1):
                        sets.append(dict(
                            wre=sb("wre", [128, 4, 128], F32, est), wim=sb("wim", [128, 4, 128], F32, est),
                            t1=t1_, t2=t2_, t3=t3_, t4=t4_,
                            wsr=sb("wsr", [128, 4, 128], F32, est), wsi=sb("wsi", [128, 4, 128], F32, est),
                            sre=sb("sre", [128, 4, 128], BF16, est), sim=sb("sim", [128, 4, 128], BF16, est),
                            ini=sb("ini", [128, 2], F32, est), tiny=sb("tiny", [128, 2], F32, est)))
                    ypre = sb("ypre", [128, 512], F32, est)
                    for fc in fcs:
                        ypb = pbank[ps_reserve("A")]
                        for r in range(4):
                            pc = 4 * fc + r
                            S_ = sets[pc % len(sets)]
                            wre, wim, t1, t2, t3, t4 = S_["wre"], S_["wim"], S_["t1"], S_["t2"], S_["t3"], S_["t4"]
                            wsr, wsi, sre, sim, ini, tiny = S_["wsr"], S_["wsi"], S_["sre"], S_["sim"], S_["ini"], S_["tiny"]
                            psl = slice(32 * r, 32 * r + 32) if r < 3 else slice(64, 128)
                            osl = slice(64 * (r // 2), 64 * (r // 2) + 64)
                            pre = PSa(); pim = PSa()
                            MM(pre[:], BTre[psl, pc, :], uT[psl, fc, :], True, True, [BTre, uT], [pre])
                            MM(pim[:], BTim[psl, pc, :], uT[psl, fc, :], True, True, [BTim, uT], [pim])
                            Cb = tabc[:, pc:pc + 1, :].to_broadcast([128, 4, 128]); Sb = tabs[:, pc:pc + 1, :].to_broadcast([128, 4, 128])
                            pre3 = pre[:].rearrange("p (k n) -> p k n", k=4); pim3 = pim[:].rearrange("p (k n) -> p k n", k=4)
                            TT("dve", t1[:], pre3, Cb, ALU.mult, [pre, tabc], [t1])
                            TT("dve", t2[:], pim3, Sb, ALU.mult, [pim, tabs], [t2])
                            TT("dve", wre[:], t1[:], t2[:], ALU.add, [t1, t2], [wre])
                            TT("dve", t1[:], pim3, Cb, ALU.mult, [pim, tabc], [t1])
                            TT("dve", t2[:], pre3, Sb, ALU.mult, [pre, tabs], [t2])
                            TT("dve", wim[:], t1[:], t2[:], ALU.subtract, [t1, t2], [wim])
                            yield 3.0
                            magb = s5_mag[:, pc:pc + 1].to_broadcast([128, 128])
                            for k in range(4):
                                if k == 0:
                                    i_re = carr[:, pc, 0:1]; i_im = carr[:, pc, 1:2]; ib = carr
                                else:
                                    i_re = ini[:, 0:1]; i_im = ini[:, 1:2]; ib = ini
                                op("dve", lambda E: E.tensor_tensor_scan(out=wsr[:, k, :], data0=magb, data1=wre[:, k, :], initial=i_re, op0=ALU.mult, op1=ALU.add), [s5_mag, wre, ib], [wsr])
                                op("dve", lambda E: E.tensor_tensor_scan(out=wsi[:, k, :], data0=magb, data1=wim[:, k, :], initial=i_im, op0=ALU.mult, op1=ALU.add), [s5_mag, wim, ib], [wsi])
                                dst = ini if k < 3 else carr
                                d_re = ini[:, 0:1] if k < 3 else carr[:, pc, 0:1]
                                d_im = ini[:, 1:2] if k < 3 else carr[:, pc, 1:2]
                                TS("dve", tiny[:, 0:1], wsr[:, k, 127:128], s5_c128[:, pc:pc + 1], None, ALU.mult, None, [wsr, s5_c128], [tiny])
                                TS("dve", tiny[:, 1:2], wsi[:, k, 127:128], s5_c128[:, pc:pc + 1], None, ALU.mult, None, [wsi, s5_c128], [tiny])
                                STT(d_re, wsi[:, k, 127:128], s5_s128n[:, pc:pc + 1], tiny[:, 0:1], ALU.mult, ALU.add, [wsi, s5_s128n, tiny], [dst])
                                STT(d_im, wsr[:, k, 127:128], s5_s128[:, pc:pc + 1], tiny[:, 1:2], ALU.mult, ALU.add, [wsr, s5_s128, tiny], [dst])
                                yield 1.5
                            TT("pool", t3[:], wsr[:], Cb, ALU.mult, [wsr, tabc], [t3])
                            TT("pool", t4[:], wsi[:], Sb, ALU.mult, [wsi, tabs], [t4])
                            TT("pool", sre[:], t3[:], t4[:], ALU.subtract, [t3, t4], [sre])
                            TT("pool", t3[:], wsi[:], Cb, ALU.mult, [wsi, tabc], [t3])
                            TT("pool", t4[:], wsr[:], Sb, ALU.mult, [wsr, tabs], [t4])
                            TT("pool", sim[:], t3[:], t4[:], ALU.add, [t3, t4], [sim])
                            yield 0.4
                            MM(ypb[osl, :], Cwre[:, pc, :], sre[:].rearrange("p k n -> p (k n)"), r % 2 == 0, False, [Cwre, sre], [ypb])
                            MM(ypb[osl, :], Cwim[:, pc, :], sim[:].rearrange("p k n -> p (k n)"), False, r % 2 == 1, [Cwim, sim], [ypb])
                            yield 5.0
                        STT(ypre[:], uT[:, fc, :], dsk[:, fc:fc + 1], ypb[:], ALU.mult, ALU.add, [uT, dsk, ypb], [ypre])
                        ACT(yg[:, fc, :], ypre[:], AF.Gelu_apprx_tanh, [ypre], [yg])
                        yield 0.4
                        ps_release("A", pbank.index(ypb))
                    if do_glu:
                        yield from gluG()
                    fences.append([x_ for S2 in sets for x_ in S2.values()] + [ypre])

                fences.append([yg, sz])

                def genCast():
                    exp_ids = [C_EXP + 3 * e_ + j_ for e_ in range(NEXP) for j_ in range(3)]
                    n_st = NB * NST
                    per = (len(exp_ids) + n_st - 1) // n_st
                    for cid_ in exp_ids[(b * NST + st) * per:(b * NST + st + 1) * per]:
                        src_, kc_, n_ = srcs[cid_]
                        fw.dma(wsc[cid_][:, 0:kc_ * n_].rearrange("p (k n) -> p k n", k=kc_), src_, writes=[chunk_buf[cid_]], q="pool")
                        yield 50.0

                if st == 0:
                    gens = [genG(estC, (0, 2), False), genG(estC, (1, 3), False), genF(estC, (0, 1, 2, 3)), genE(estC), genCast()]
                else:
                    gens = [genG(estC, (0, 1, 2, 3), True), genF(estC, (0, 2)), genF(estC, (1, 3)), genE(estC), genCast()]
                acc_t = [0.0] * len(gens)
                live = [True] * len(gens)
                while any(live):
                    gi = min((k_ for k_ in range(len(gens)) if live[k_]), key=lambda k_: acc_t[k_])
                    try:
                        w_ = next(gens[gi])
                        acc_t[gi] += (w_ if w_ is not None else 0.4)
                    except StopIteration:
                        live[gi] = False
                if st == 0:
                    for _ in gluG():
                        pass
                for f_ in fences:
                    fence(f_)
                estC.close()
                fence([qT, qiT, qmT, wis])
                e2b.close()
                fence([uT])
                e2a.close()
                yield 0.4

            def stage_H(b, st, A):
                tok0 = st * 512
                hT, ydsaT, ymemT, yssmT = A["hT"], A["ydsaT"], A["ymemT"], A["yssmT"]
                PSa = lambda: PSs("A")
                esx = ExitStack()
                Xs = sb("Xs", [128, 4, D], F32, esx)
                with ExitStack() as est:
                    gsb = sb("gsb", [128, 4, 1024], BF16, est)
                    mg = sb("mg", [128, 4, D], F32, est)
                    mgb = sb("mgb", [128, 4, D], BF16, est)
                    mT = sb("mT", [128, 4, 8, 128], BF16, est)
                    fw.dma(Xs[:], x_d[b, tok0:tok0 + 512, :].rearrange("(t p) d -> p t d", p=128), writes=XsAll, q=QA)
                    for bi, (cid, yT_) in enumerate(((C_UPS, yssmT), (C_UPD, ydsaT), (C_UPM, ymemT))):
                        for i2 in range(2):
                            sl, w, hh = ringA.get(C_G0 + 2 * bi + i2, 8, 512)
                            for t in range(4):
                                tsl = slice(t * 128, (t + 1) * 128)
                                pb = PSa()
                                for kc in range(8):
                                    MM(pb[:], hT[:, kc, tsl], w[:, kc, :], kc == 0, kc == 7, [hT, sl], [pb])
                                ACT(gsb[:, t, i2 * 512:(i2 + 1) * 512], pb[:], AF.Sigmoid, [pb], [gsb])
                            ringA.rel(hh)
                        sl, w, hh = ringA.get(cid, 4, 1024)
                        for t in range(4):
                            tsl = slice(t * 128, (t + 1) * 128)
                            for half in range(2):
                                pb = PSa()
                                for fc in range(4):
                                    MM(pb[:], yT_[:, fc, tsl], w[:, fc, half * 512:(half + 1) * 512], fc == 0, fc == 3, [yT_, sl], [pb])
                                gsl = gsb[:, t, half * 512:(half + 1) * 512]
                                msl = mg[:, t, half * 512:(half + 1) * 512]
                                if bi == 0:
                                    TT("dve", msl, pb[:], gsl, ALU.mult, [pb, gsb], [mg])
                                else:
                                    TT("dve", pb[:], pb[:], gsl, ALU.mult, [pb, gsb], [pb])
                                    if bi == 1:
                                        TT("dve", msl, msl, pb[:], ALU.add, [pb, mg], [mg])
                                    else:
                                        TT("dve", mgb[:, t, half * 512:(half + 1) * 512], msl, pb[:], ALU.add, [pb, mg], [mgb])
                        ringA.rel(hh)
                    for t in range(4):
                        pb = PSa(); pbv = pb[:].bitcast(BF16)
                        for kc in range(8):
                            TR(pbv[:, kc * 128:(kc + 1) * 128], mgb[:, t, kc * 128:(kc + 1) * 128], identb[:], [mgb, identb], [pb])
                        CP("act", mT[:, t, :, :], pbv[:, 0:1024].rearrange("p (k n) -> p k n", k=8), [pb], [mT])
                    for half, cid in enumerate((C_OUT0, C_OUT1)):
                        sl, w, hh = ringA.get(cid, 8, 512)
                        for t in range(4):
                            pb = PSa()
                            for kc in range(8):
                                MM(pb[:], mT[:, t, kc, :], w[:, kc, :], kc == 0, kc == 7, [mT, sl], [pb])
                            xs_ = Xs[:, t, half * 512:(half + 1) * 512]
                            TT("dve", xs_, xs_, pb[:], ALU.add, [XsB[t][half], pb], [XsB[t][half]])
                        ringA.rel(hh)
                    fw.barrier()
                with ExitStack() as est:
                    hn = sb("hn", [128, D], F32, est); hnb = sb("hnb", [128, D], BF16, est)
                    hnb2 = [sb("hnb2_%d" % i_, [128, D], BF16, est) for i_ in range(2)]
                    ohs = sb("ohs", [128, 32], F32, est); rk = sb("rk", [128, 32], F32, est); rkm = sb("rkm", [128, 32], F32, est)
                    hnT32 = sb("hnT32", [128, 8, 128], F32, est)
                    rst2 = sb("rst2", [128, 4], F32, est)
                    lg = sb("lg", [128, 36], F32, est)
                    r4 = sb("r4", [128, 16], F32, est)
                    ohg = sb("ohg", [128, 4], F32, est)
                    le = sb("le", [128, 4, 8], F32, est); el = sb("el", [128, 8], F32, est); ee = sb("ee", [128, 8], F32, est)
                    oh1 = sb("oh1", [128, 8], F32, est); oh2 = sb("oh2", [128, 8], F32, est); e2 = sb("e2", [128, 8], F32, est)
                    for t in range(4):
                        ACT(hnb[:], Xs[:, t, :], AF.Square, XsB[t], [hnb, rst2], accum=rst2[:, t:t + 1])
                    rstd_of(rst2[:], D, [rst2])
                    for t in range(4):
                        tsl = slice(t * 128, (t + 1) * 128)
                        TS("dve", hn[:], Xs[:, t, :], rst2[:, t:t + 1], None, ALU.mult, None, XsB[t] + [rst2], [hn])
                        tt = (b * NST + st) * 4 + t
                        hb_ = hnb2[t % 2]
                        CP("pool", hb_[:], hn[:], [hn], [hb_])
                        fw.dma(HN[tt * 128:(tt + 1) * 128, :], hb_[:], reads=[hb_], writes=[HNb])
                        for q4 in range(2):
                            pb = PSa()
                            for c in range(4):
                                kc = 4 * q4 + c
                                TR(pb[:, c * 128:(c + 1) * 128], hn[:, kc * 128:(kc + 1) * 128], ident[:], [hn, ident], [pb])
                            TT("dve", hnT32[:, 4 * q4:4 * q4 + 4, :], pb[:].rearrange("p (k n) -> p k n", k=4),
                               gffnT[:, 4 * q4:4 * q4 + 4].unsqueeze(2).to_broadcast([128, 4, 128]), ALU.mult, [pb, gffnT], [hnT32])
                        pb = PSa()
                        for kc in range(8):
                            MM(pb[:, 0:36], hnT32[:, kc, :], wr32[:, kc, :], kc == 0, kc == 7, [hnT32, wr32], [pb])
                        TT("dve", lg[:], pb[:, 0:36], rbias[:], ALU.add, [pb, rbias], [lg])
                        op("dve", lambda E: E.tensor_reduce(out=r4[:, 0:1], in_=lg[:, 0:4], axis=AX.X, op=ALU.max), [lg], [r4])
                        TS("dve", ohg[:], lg[:, 0:4], r4[:, 0:1], None, ALU.is_equal, None, [lg, r4], [ohg])
                        TS("dve", r4[:, 1:2], r4[:, 0:1], -1.0, None, ALU.mult, None, [r4], [r4])
                        ACT(r4[:, 4:8], lg[:, 0:4], AF.Exp, [lg, r4], [r4], bias=r4[:, 1:2])
                        op("dve", lambda E: E.tensor_reduce(out=r4[:, 2:3], in_=r4[:, 4:8], axis=AX.X, op=ALU.add), [r4], [r4])
                        op("dve", lambda E: E.reciprocal(out=r4[:, 2:3], in_=r4[:, 2:3]), [r4], [r4])
                        TT("dve", le[:], lg[:, 4:36].rearrange("p (g e) -> p g e", g=4), ohg[:].unsqueeze(2).to_broadcast([128, 4, 8]), ALU.mult, [lg, ohg], [le])
                        op("dve", lambda E: E.tensor_reduce(out=el[:], in_=le[:].rearrange("p g e -> p e g"), axis=AX.X, op=ALU.add), [le], [el])
                        op("dve", lambda E: E.tensor_reduce(out=r4[:, 3:4], in_=el[:], axis=AX.X, op=ALU.max), [el], [r4])
                        TS("dve", r4[:, 8:9], r4[:, 3:4], -1.0, None, ALU.mult, None, [r4], [r4])
                        ACT(ee[:], el[:], AF.Exp, [el, r4], [ee], bias=r4[:, 8:9])
                        op("dve", lambda E: E.tensor_reduce(out=r4[:, 9:10], in_=ee[:], axis=AX.X, op=ALU.max), [ee], [r4])
                        TS("dve", oh1[:], ee[:], r4[:, 9:10], None, ALU.is_equal, None, [ee, r4], [oh1])
                        STT(e2[:], oh1[:], -4.0, ee[:], ALU.mult, ALU.add, [oh1, ee], [e2])
                        op("dve", lambda E: E.tensor_reduce(out=r4[:, 10:11], in_=e2[:], axis=AX.X, op=ALU.max), [e2], [r4])
                        TS("dve", oh2[:], e2[:], r4[:, 10:11], None, ALU.is_equal, None, [e2, r4], [oh2])
                        TT("dve", r4[:, 11:12], r4[:, 9:10], r4[:, 10:11], ALU.add, [r4], [r4])
                        op("dve", lambda E: E.reciprocal(out=r4[:, 11:12], in_=r4[:, 11:12]), [r4], [r4])
                        TT("dve", r4[:, 11:12], r4[:, 11:12], r4[:, 2:3], ALU.mult, [r4], [r4])
                        TT("dve", r4[:, 12:13], r4[:, 9:10], r4[:, 11:12], ALU.mult, [r4], [r4])
                        TT("dve", r4[:, 13:14], r4[:, 10:11], r4[:, 11:12], ALU.mult, [r4], [r4])
                        CP("dve", W12[:, tt, :], r4[:, 12:14], [r4], [W12])
                        TT("dve", OH1[:, tt, :].rearrange("p (g e) -> p g e", g=4), oh1[:].unsqueeze(1).to_broadcast([128, 4, 8]),
                           ohg[:].unsqueeze(2).to_broadcast([128, 4, 8]), ALU.mult, [oh1, ohg], [OH1])
                        TT("dve", OH2[:, tt, :].rearrange("p (g e) -> p g e", g=4), oh2[:].unsqueeze(1).to_broadcast([128, 4, 8]),
                           ohg[:].unsqueeze(2).to_broadcast([128, 4, 8]), ALU.mult, [oh2, ohg], [OH2])
                        TT("dve", ohs[:], OH1[:, tt, :], OH2[:, tt, :], ALU.add, [OH1, OH2], [ohs])
                        pb = PSa()
                        MM(pb[:, 0:32], ltri[:], ohs[:], True, True, [ltri, ohs], [pb])
                        MM(pb[:, 32:64], ones[:], ohs[:], True, True, [ones, ohs], [pb])
                        TT("dve", rk[:], pb[:, 0:32], run[:], ALU.add, [pb, run], [rk])
                        TT("dve", run[:], run[:], pb[:, 32:64], ALU.add, [pb, run], [run])
                        TT("dve", rkm[:], rk[:], OH1[:, tt, :], ALU.mult, [rk, OH1], [rkm])
                        op("dve", lambda E: E.tensor_reduce(out=RNK[:, tt, 0:1], in_=rkm[:], axis=AX.X, op=ALU.add), [rkm], [RNK])
                        TT("dve", rkm[:], rk[:], OH2[:, tt, :], ALU.mult, [rk, OH2], [rkm])
                        op("dve", lambda E: E.tensor_reduce(out=RNK[:, tt, 1:2], in_=rkm[:], axis=AX.X, op=ALU.add), [rkm], [RNK])
                    g0 = (b * NST + st) * 512
                    fw.dma(XM[g0:g0 + 512, :].rearrange("(t p) d -> p t d", p=128), Xs[:], reads=XsAll, writes=[XMb])
                    fw.barrier()
                esx.close()

            def drive(ga):
                for _ in ga:
                    pass

            for b in range(NB):
                batch_prep(b)
                for st in range(NST):
                    with ExitStack() as esa:
                        A = {
                            "hT": sb("hT", [128, 8, 512], BF16, esa),
                            "ydsaT": sb("ydsaT", [128, 4, 512], BF16, esa), "ymemT": sb("ymemT", [128, 4, 512], BF16, esa),
                            "yssmT": sb("yssmT", [128, 4, 512], BF16, esa), "rst": sb("rst", [128, 4], F32, esa),
                        }
                        drive(attn_AG(b, st, A, esa))
                        fw.barrier()
                        stage_H(b, st, A)
                    fw.barrier()
            fw.barrier()
            esP1.close()

            PS8 = lambda: PSs("A")
            with ExitStack() as e15:
                cmp = sb("cmp", [128, 32, 64], F32, e15)
                ltc = sb("ltc", [128, 1024], F32, e15)
                fw.dma(ltc[:], ltc_d.partition_broadcast(128), writes=[ltc])
                kt = sb("kt", [128, 32], F32, e15); tsv = sb("tsv", [128, 32], F32, e15); te = sb("te", [128, 32], F32, e15)
                ts128 = sb("ts128", [128, 32], F32, e15)
                cm2 = sb("cm2", [128, 32, 32], F32, e15)
                msk = sb("msk", [128, NTILE, 32], F32, e15)
                texp = sb("texp", [128, NTILE], F32, e15); wf = sb("wf", [128, NTILE, 3], F32, e15)
                big = sb("big", [128, NTT, 32], F32, e15); dpf = sb("dpf", [128, NTT], F32, e15)
                TT("dve", cmp[:], run[:].unsqueeze(2).to_broadcast([128, 32, 64]), thr[:].unsqueeze(1).to_broadcast([128, 32, 64]), ALU.is_gt, [run, thr], [cmp])
                op("dve", lambda E: E.tensor_reduce(out=kt[:], in_=cmp[:], axis=AX.X, op=ALU.add), [cmp], [kt])
                TT("dve", cm2[:], ltc[:].rearrange("p (a c) -> p a c", a=32), kt[:].unsqueeze(1).to_broadcast([128, 32, 32]), ALU.mult, [ltc, kt], [cm2])
                op("dve", lambda E: E.tensor_reduce(out=tsv[:], in_=cm2[:], axis=AX.X, op=ALU.add), [cm2], [tsv])
                TT("dve", te[:], tsv[:], kt[:], ALU.add, [tsv, kt], [te])
                TS("dve", ts128[:], tsv[:], 128.0, None, ALU.mult, None, [tsv], [ts128])
                TT("dve", msk[:], te[:].unsqueeze(1).to_broadcast([128, NTILE, 32]), iot[:].unsqueeze(2).to_broadcast([128, NTILE, 32]), ALU.is_le, [te, iot], [msk])
                op("dve", lambda E: E.tensor_reduce(out=texp[:], in_=msk[:], axis=AX.X, op=ALU.add), [msk], [texp])
                TS("dve", texp[:], texp[:], 31.0, None, ALU.min, None, [texp], [texp])
                for j in range(3):
                    TS("dve", wf[:, :, j], texp[:], 384.0, float((C_EXP + j) * 128), ALU.mult, ALU.add, [texp], [wf])
                TS("dve", wf[:], wf[:], pcol[:, 0:1], None, ALU.add, None, [wf, pcol], [wf])
                CP("dve", widx[:], wf[:], [wf], [widx])
                for c, OH in enumerate((OH1, OH2)):
                    TT("dve", big[:], OH[:], ts128[:].unsqueeze(1).to_broadcast([128, NTT, 32]), ALU.mult, [OH, ts128], [big])
                    op("dve", lambda E: E.tensor_reduce(out=dpf[:], in_=big[:], axis=AX.X, op=ALU.add), [big], [dpf])
                    TT("dve", dpf[:], dpf[:], RNK[:, :, c], ALU.add, [dpf, RNK], [dpf])
                    CP("dve", dpi[c][:], dpf[:], [dpf], [dpi[c]])
                hnl = [sb("hnl%d" % i_, [128, D], BF16, e15) for i_ in range(4)]
                for tt in range(NTT):
                    h_ = hnl[tt % 4]
                    fw.dma(h_[:], HN[tt * 128:(tt + 1) * 128, :], reads=[HNb], writes=[h_])
                    for c in range(2):
                        fw.idma(HS, bass.IndirectOffsetOnAxis(ap=dpi[c][:, tt:tt + 1], axis=0), h_[:], None, NTILE * 128 - 1,
                                reads=[h_, dpi[c]], writes=[HSb])
                fw.barrier()
            fw.barrier()

            wscf = wsc.rearrange("c p n -> (c p) n")
            with ExitStack() as e2:
                wgu = [[ring[0], ring[1]]] + [[sb("wgu%d_%d" % (i_, j_), [128, 4096], BF16, e2) for j_ in range(2)] for i_ in range(2)]
                wdn = [sb("wdn%d" % i_, [128, 4096], BF16, e2) for i_ in range(3)]
                hsl = [sb("hsl%d" % i_, [128, D], BF16, e2) for i_ in range(3)]
                hsT = [sb("hsT%d" % i_, [128, 8, 128], BF16, e2) for i_ in range(2)]
                sg2 = [sb("sg2_%d" % i_, [128, 512], F32, e2) for i_ in range(2)]
                hidb = [sb("hidb%d" % i_, [128, 512], BF16, e2) for i_ in range(2)]
                hidT = [sb("hidT%d" % i_, [128, 4, 128], BF16, e2) for i_ in range(2)]
                ysb = [sb("ysb%d" % i_, [128, D], F32, e2) for i_ in range(3)]

                def load_gu(i):
                    for j in range(2):
                        sl = wgu[i % 3][j]
                        fw.idma(sl[:], None, wscf, bass.IndirectOffsetOnAxis(ap=widx[:, i, j:j + 1], axis=0), NCH * 128 - 1, reads=[widx], writes=[sl])

                def load_dn(i):
                    sl = wdn[i % 3]
                    fw.idma(sl[:], None, wscf, bass.IndirectOffsetOnAxis(ap=widx[:, i, 2:3], axis=0), NCH * 128 - 1, reads=[widx], writes=[sl])

                def load_hs(i):
                    fw.dma(hsl[i % 3][:], HS[i * 128:(i + 1) * 128, :], reads=[HSb], writes=[hsl[i % 3]])

                def S1(i):
                    h_ = hsl[i % 3]; hT_ = hsT[i % 2]
                    pb = PS8(); pbv = pb[:].bitcast(BF16)
                    for kc in range(8):
                        TR(pbv[:, kc * 128:(kc + 1) * 128], h_[:, kc * 128:(kc + 1) * 128], identb[:], [h_, identb], [pb])
                    TT("dve", hT_[:], pbv[:, 0:1024].rearrange("p (k n) -> p k n", k=8),
                       gffnT[:].unsqueeze(2).to_broadcast([128, 8, 128]), ALU.mult, [pb, gffnT], [hT_])

                def S2(i):
                    hT_ = hsT[i % 2]
                    slg, slu = wgu[i % 3]
                    wg = slg[:].rearrange("p (k n) -> p k n", k=8); wu = slu[:].rearrange("p (k n) -> p k n", k=8)
                    pg = PS8(); pu = PS8()
                    for kc in range(8):
                        MM(pg[:], hT_[:, kc, :], wg[:, kc, :], kc == 0, kc == 7, [hT_, slg], [pg])
                    for kc in range(8):
                        MM(pu[:], hT_[:, kc, :], wu[:, kc, :], kc == 0, kc == 7, [hT_, slu], [pu])
                    ACT(sg2[i % 2][:], pg[:], AF.Silu, [pg], [sg2[i % 2]])
                    TT("dve", hidb[i % 2][:], pu[:], sg2[i % 2][:], ALU.mult, [pu, sg2[i % 2]], [hidb[i % 2]])

                def S3(i):
                    pb = PS8(); pbv = pb[:].bitcast(BF16)
                    for fc in range(4):
                        TR(pbv[:, fc * 128:(fc + 1) * 128], hidb[i % 2][:, fc * 128:(fc + 1) * 128], identb[:], [hidb[i % 2], identb], [pb])
                    CP("act", hidT[i % 2][:], pbv[:, 0:512].rearrange("p (k n) -> p k n", k=4), [pb], [hidT[i % 2]])

                def S4(i):
                    sld = wdn[i % 3]
                    wd = sld[:].rearrange("p (k n) -> p k n", k=4)
                    y_ = ysb[i % 3]
                    for half in range(2):
                        po = PS8()
                        for fc in range(4):
                            MM(po[:], hidT[i % 2][:, fc, :], wd[:, fc, half * 512:(half + 1) * 512], fc == 0, fc == 3, [hidT[i % 2], sld], [po])
                        CP("act" if half == 0 else "dve", y_[:, half * 512:(half + 1) * 512], po[:], [po], [y_])
                    fw.dma(YS[i * 128:(i + 1) * 128, :], y_[:], reads=[y_], writes=[YSb])

                for i in range(min(3, NTILE)):
                    load_hs(i); load_gu(i); load_dn(i)
                S1(0)
                if NTILE > 3:
                    load_hs(3)
                for i in range(NTILE + 1):
                    if i < NTILE:
                        S2(i)
                        if i + 3 < NTILE:
                            load_gu(i + 3)
                    if i + 1 < NTILE:
                        S1(i + 1)
                        if i + 4 < NTILE:
                            load_hs(i + 4)
                    if i >= 1:
                        S4(i - 1)
                        if i + 2 < NTILE:
                            load_dn(i + 2)
                    if i < NTILE:
                        S3(i)
                fw.barrier()
            fw.barrier()

            out_f = out_d.rearrange("b s d -> (b s) d")
            with ExitStack() as e3:
                xm = [sb("xm%d" % i_, [128, D], F32, e3) for i_ in range(3)]
                y1 = [sb("y1_%d" % i_, [128, D], F32, e3) for i_ in range(3)]
                y2 = [sb("y2_%d" % i_, [128, D], F32, e3) for i_ in range(3)]
                for tt in range(NTT):
                    k_ = tt % 3
                    fw.dma(xm[k_][:], XM[tt * 128:(tt + 1) * 128, :], reads=[XMb], writes=[xm[k_]])
                    fw.idma(y1[k_][:], None, YS, bass.IndirectOffsetOnAxis(ap=dpi[0][:, tt:tt + 1], axis=0), NTILE * 128 - 1, reads=[YSb, dpi[0]], writes=[y1[k_]])
                    fw.idma(y2[k_][:], None, YS, bass.IndirectOffsetOnAxis(ap=dpi[1][:, tt:tt + 1], axis=0), NTILE * 128 - 1, reads=[YSb, dpi[1]], writes=[y2[k_]])
                    STT(xm[k_][:], y1[k_][:], W12[:, tt, 0:1], xm[k_][:], ALU.mult, ALU.add, [y1[k_], W12, xm[k_]], [xm[k_]])
                    STT(xm[k_][:], y2[k_][:], W12[:, tt, 1:2], xm[k_][:], ALU.mult, ALU.add, [y2[k_], W12, xm[k_]], [xm[k_]])
                    fw.dma(out_f[tt * 128:(tt + 1) * 128, :], xm[k_][:], reads=[xm[k_]], q="act")
                fw.barrier()
        except _Stop:
            pass
        fw.barrier()
        print("[build] instrs=%d waits=%d" % (fw.ninstr, fw.nwaits))
    return nc, dbg_t


def _consts():
    ident = np.eye(128, dtype=np.float32)
    q = np.arange(128)[:, None]; k = np.arange(128)[None, :]
    causal = np.where(k <= q, 0.0, -1.0e30).astype(np.float32)
    jj = np.broadcast_to(np.arange(1, 129, dtype=np.float32)[None, :], (128, 128)).copy()
    invf = (1.0 / (np.float32(10000.0) ** (np.arange(0, 64, 2, dtype=np.float32) / np.float32(64.0)))).astype(np.float32)[None, :]
    par = np.zeros((128, 2), np.float32)
    g8 = np.arange(128) // 16
    par[:, 0] = (g8 % 2 == 0); par[:, 1] = (g8 % 2 == 1)
    ltri = (np.arange(128)[:, None] < np.arange(128)[None, :]).astype(np.float32)
    thr = (128.0 * np.arange(64, dtype=np.float32))[None, :]
    lt = (np.arange(32)[None, :] < np.arange(32)[:, None]).astype(np.float32).reshape(1, 1024)
    iot = np.arange(NTILE, dtype=np.float32)[None, :]
    pcol = np.arange(128, dtype=np.float32)[:, None]
    return {"c_ident": ident, "c_causal": causal, "c_jj": jj, "c_invf": invf, "c_par": par,
            "c_ltri": ltri, "c_thr": thr, "c_lt": lt, "c_iot": iot, "c_pcol": pcol}


def _in_maps(inputs, NB, ncores):
    f = lambda a: np.ascontiguousarray(a)
    shared = {
        "g_mix": f(inputs["g_mix"]), "g_mem": f(inputs["g_mem"]), "g_ffn": f(inputs["g_ffn"]),
        "w_in": f(inputs["w_in"][0]), "lam_re": f(inputs["lam_re"][0]), "lam_im": f(inputs["lam_im"][0]), "log_dt": f(inputs["log_dt"]),
        "b_re": f(inputs["b_re"][0]), "b_im": f(inputs["b_im"][0]),
        "c_re": f(inputs["c_re"][0].reshape(512, 64)), "c_im": f(inputs["c_im"][0].reshape(512, 64)),
        "d_skip": f(inputs["d_skip"]), "w_glu": f(inputs["w_glu"][0]),
        "g_q": f(inputs["g_q"]), "g_k": f(inputs["g_k"]), "g_qm": f(inputs["g_qm"]), "g_km": f(inputs["g_km"]),
        "w_mem_kv": f(inputs["w_mem_kv"][0]), "w_up_ssm": f(inputs["w_up_ssm"][0]), "w_up_dsa": f(inputs["w_up_dsa"][0]),
        "w_up_mem": f(inputs["w_up_mem"][0]), "w_out": f(inputs["w_out"][0]),
        "w_group": f(inputs["w_group"][0]), "b_group": f(inputs["b_group"]), "w_expert": f(inputs["w_expert"][0]),
        "b_expert": f(inputs["b_expert"][0].reshape(1, 32)),
        "w_gate": f(inputs["w_gate"][0]), "w_up": f(inputs["w_up"][0]), "w_down": f(inputs["w_down"][0]),
    }
    shared.update(_consts())
    maps = []
    for c in range(ncores):
        m = dict(shared)
        m["x"] = f(inputs["x"][c * NB:(c + 1) * NB])
        m["mem"] = f(inputs["mem"][c * NB:(c + 1) * NB])
        m["positions"] = f(inputs["positions"][c * NB:(c + 1) * NB].astype(np.int32))
        maps.append(m)
    return maps


def kernel(**inputs):
    NB = 4
    nc, _ = build_program(NB=NB, NST=4, NEXP=32)
    maps = _in_maps(inputs, NB, NCORES)
    res = run_bass_kernel_spmd(nc, maps, core_ids=list(range(NCORES)))
    out = np.concatenate([np.asarray(r["out"]) for r in res.results], axis=0)
    return out.astype(np.float32, copy=False)
```
